# Optimizing a Trainium2 kernel written in Bass

```python
import jax, jax.numpy as jnp
from jax import lax
import numpy as np

D_MODEL = 1024
BATCH = 8
SEQ = 4096
DEPTH = 2

GRID_W = 64
CTX_LEN = 256
N_EVEN = (DEPTH + 1) // 2
N_ODD = DEPTH // 2
N_MOD = 6
EPS = 1e-6

FOURIER_HEADS = 4
FOURIER_HEAD_DIM = D_MODEL // 8
FOURIER_WIDTH = FOURIER_HEADS * FOURIER_HEAD_DIM
CONV_HEADS = 4
CONV_WIDTH = D_MODEL // 2
CONV_K = 3
EVEN_IN = FOURIER_WIDTH + 3 * CONV_WIDTH
EVEN_MIX = FOURIER_WIDTH + CONV_WIDTH

POOL_WINDOWS = (2, 4, 8, 16)
POOL_GROUPS = 4
POOL_GROUP_DIM = D_MODEL // 8
POOL_WIDTH = POOL_GROUPS * POOL_GROUP_DIM
HEAD_DIM = 64
N_Q_HEADS = (D_MODEL // 2) // HEAD_DIM
N_KV_HEADS = N_Q_HEADS // 4
Q_PER_KV = N_Q_HEADS // N_KV_HEADS
ATTN_WIDTH = N_Q_HEADS * HEAD_DIM
KV_WIDTH = N_KV_HEADS * HEAD_DIM
ODD_IN = POOL_WIDTH + ATTN_WIDTH + 2 * KV_WIDTH
ODD_MIX = POOL_WIDTH + ATTN_WIDTH
WINDOW = 128
BLOCK = 128
ROPE_THETA = 10000.0

N_EXPERTS = 16
N_GROUPS = 4
EXPERTS_PER_GROUP = N_EXPERTS // N_GROUPS
TOP_K = 2
D_EXPERT = D_MODEL // 2

kernel_name = "hybrid_fourier_conv_pool_swa_moe_dit"


def rmsnorm(x, g):
    xf = x.astype(jnp.float32)
    y = xf * lax.rsqrt(jnp.mean(xf * xf, axis=-1, keepdims=True) + EPS)
    return (y * g.astype(jnp.float32)).astype(x.dtype)


def modulate(h, shift, scale):
    return h * (1 + scale) + shift


def ada_mod(s, w, b):
    return jnp.split(s @ w + b, N_MOD, axis=-1)


def fourier_mix(u):
    f = jnp.fft.fft2(u.astype(jnp.float32), axes=(1, 3), norm="ortho")
    return jnp.real(f).astype(u.dtype)


def short_conv(u, w):
    n = u.shape[1]
    p = CONV_K // 2
    up = jnp.pad(u, ((0, 0), (p, p), (0, 0)))
    out = up[:, 0:n] * w[0]
    for k in range(1, CONV_K):
        out = out + up[:, k:k + n] * w[k]
    return out


def even_mixer(h, w_in, conv_w, w_out):
    b, n, _ = h.shape
    proj = h @ w_in
    u_f, g_b, g_c, u_c = jnp.split(
        proj, [FOURIER_WIDTH, FOURIER_WIDTH + CONV_WIDTH, FOURIER_WIDTH + 2 * CONV_WIDTH], axis=-1)
    y_f = fourier_mix(u_f.reshape(b, n, FOURIER_HEADS, FOURIER_HEAD_DIM)).reshape(b, n, FOURIER_WIDTH)
    y_c = g_b * short_conv(g_c * u_c, conv_w)
    return jnp.concatenate([y_f, y_c], axis=-1) @ w_out


def multiscale_pool(u):
    n = u.shape[1]
    uf = u.astype(jnp.float32)
    cs = jnp.pad(jnp.cumsum(uf, axis=1), ((0, 0), (1, 0), (0, 0)))
    t = jnp.arange(n)
    outs = []
    for gi, w in enumerate(POOL_WINDOWS):
        r = w // 2
        lo = jnp.clip(t - r, 0, n)
        hi = jnp.clip(t + r + 1, 0, n)
        sl = slice(gi * POOL_GROUP_DIM, (gi + 1) * POOL_GROUP_DIM)
        seg = cs[:, :, sl]
        cnt = (hi - lo).astype(jnp.float32)[None, :, None]
        outs.append((seg[:, hi] - seg[:, lo]) / cnt - uf[:, :, sl])
    return jnp.concatenate(outs, axis=-1).astype(u.dtype)


def pool_branch(u, pool_w, pool_scale):
    b, n, _ = u.shape
    p = multiscale_pool(u).reshape(b, n, POOL_GROUPS, POOL_GROUP_DIM)
    y = jnp.einsum('bngc,gcd->bngd', p, pool_w).reshape(b, n, POOL_WIDTH)
    return y * pool_scale


def axial_rope_tables(row_ids, col_ids):
    quarter = HEAD_DIM // 4
    inv = ROPE_THETA ** (-jnp.arange(quarter, dtype=jnp.float32) / quarter)
    ang = jnp.stack([row_ids.astype(jnp.float32)[:, None] * inv,
                     col_ids.astype(jnp.float32)[:, None] * inv], axis=1)
    return jnp.cos(ang), jnp.sin(ang)


def apply_rope(x, cos, sin):
    xf = x.astype(jnp.float32).reshape(*x.shape[:-1], 2, 2, HEAD_DIM // 4)
    x1, x2 = xf[..., 0, :], xf[..., 1, :]
    c = cos[None, :, None]
    s = sin[None, :, None]
    out = jnp.stack([x1 * c - x2 * s, x2 * c + x1 * s], axis=-2)
    return out.reshape(x.shape).astype(x.dtype)


def latent_attention(q, k, v, k_ctx, v_ctx, sink):
    b, n, _, _ = q.shape
    nb = n // BLOCK
    span = 3 * BLOCK
    scale = HEAD_DIM ** -0.5
    qb = q.reshape(b, nb, BLOCK, N_KV_HEADS, Q_PER_KV, HEAD_DIM)
    pad = ((0, 0), (BLOCK, BLOCK), (0, 0), (0, 0))
    kp, vp = jnp.pad(k, pad), jnp.pad(v, pad)

    def band(z):
        return jnp.concatenate(
            [z[:, i * BLOCK:i * BLOCK + n].reshape(b, nb, BLOCK, N_KV_HEADS, HEAD_DIM) for i in range(3)],
            axis=2)

    kb, vb = band(kp), band(vp)
    s_lat = jnp.einsum('bnqkgd,bnskd->bnkgqs', qb, kb).astype(jnp.float32) * scale
    qi = jnp.arange(BLOCK)[:, None]
    kj = jnp.arange(span)[None, :]
    rel = kj - BLOCK - qi
    key_pos = jnp.arange(nb)[:, None, None] * BLOCK - BLOCK + kj[None]
    valid = (jnp.abs(rel) <= WINDOW)[None] & (key_pos >= 0) & (key_pos < n)
    s_lat = jnp.where(valid[None, :, None, None], s_lat, -jnp.inf)
    s_ctx = jnp.einsum('bnqkgd,blkd->bnkgql', qb, k_ctx).astype(jnp.float32) * scale
    sink_b = jnp.broadcast_to(
        sink.astype(jnp.float32).reshape(N_KV_HEADS, Q_PER_KV)[None, None, :, :, None, None],
        (b, nb, N_KV_HEADS, Q_PER_KV, BLOCK, 1))
    p = jax.nn.softmax(jnp.concatenate([s_lat, s_ctx, sink_b], axis=-1), axis=-1)
    l = k_ctx.shape[1]
    p_lat = p[..., :span].astype(v.dtype)
    p_ctx = p[..., span:span + l].astype(v.dtype)
    o = (jnp.einsum('bnkgqs,bnskd->bnqkgd', p_lat, vb)
         + jnp.einsum('bnkgql,blkd->bnqkgd', p_ctx, v_ctx))
    return o.reshape(b, n, ATTN_WIDTH)


def context_attention(q, k, v, sink):
    b, l, _, _ = q.shape
    scale = HEAD_DIM ** -0.5
    qg = q.reshape(b, l, N_KV_HEADS, Q_PER_KV, HEAD_DIM)
    s = jnp.einsum('blkgd,bmkd->bkglm', qg, k).astype(jnp.float32) * scale
    sink_b = jnp.broadcast_to(
        sink.astype(jnp.float32).reshape(N_KV_HEADS, Q_PER_KV)[None, :, :, None, None],
        (b, N_KV_HEADS, Q_PER_KV, l, 1))
    p = jax.nn.softmax(jnp.concatenate([s, sink_b], axis=-1), axis=-1)[..., :l].astype(v.dtype)
    o = jnp.einsum('bkglm,bmkd->blkgd', p, v)
    return o.reshape(b, l, ATTN_WIDTH)


def odd_mixer(a, ac, cos, sin, w_in, pool_w, pool_scale, sink, w_out, need_ctx):
    b, n, _ = a.shape
    l = ac.shape[1]
    kv0 = POOL_WIDTH + ATTN_WIDTH
    proj = a @ w_in
    u_pool = proj[..., :POOL_WIDTH]
    q = apply_rope(proj[..., POOL_WIDTH:kv0].reshape(b, n, N_Q_HEADS, HEAD_DIM), cos, sin)
    k = apply_rope(proj[..., kv0:kv0 + KV_WIDTH].reshape(b, n, N_KV_HEADS, HEAD_DIM), cos, sin)
    v = proj[..., kv0 + KV_WIDTH:].reshape(b, n, N_KV_HEADS, HEAD_DIM)
    if need_ctx:
        proj_c = ac @ w_in
        kv_c = proj_c[..., kv0:]
    else:
        kv_c = ac @ w_in[:, kv0:]
    k_c = kv_c[..., :KV_WIDTH].reshape(b, l, N_KV_HEADS, HEAD_DIM)
    v_c = kv_c[..., KV_WIDTH:].reshape(b, l, N_KV_HEADS, HEAD_DIM)
    y = jnp.concatenate([pool_branch(u_pool, pool_w, pool_scale),
                         latent_attention(q, k, v, k_c, v_c, sink)], axis=-1) @ w_out
    if need_ctx:
        q_c = proj_c[..., POOL_WIDTH:kv0].reshape(b, l, N_Q_HEADS, HEAD_DIM)
        yc = jnp.concatenate([pool_branch(proj_c[..., :POOL_WIDTH], pool_w, pool_scale),
                              context_attention(q_c, k_c, v_c, sink)], axis=-1) @ w_out
        return y, yc
    return y, None


def grouped_moe(h, router_w, router_b, w_gate, w_up, w_down):
    t = h.shape[0]
    aff = jax.nn.sigmoid((h @ router_w).astype(jnp.float32))
    sel = aff + router_b.astype(jnp.float32)
    group_score = lax.top_k(sel.reshape(t, N_GROUPS, EXPERTS_PER_GROUP), 2)[0].sum(-1)
    best = jnp.argmax(group_score, axis=-1)
    in_group = (jnp.arange(N_EXPERTS) // EXPERTS_PER_GROUP)[None, :] == best[:, None]
    _, idx = lax.top_k(jnp.where(in_group, sel, -jnp.inf), TOP_K)
    w_sel = jnp.take_along_axis(aff, idx, axis=-1)
    w_sel = w_sel / jnp.sum(w_sel, axis=-1, keepdims=True)
    combine = jnp.sum(jax.nn.one_hot(idx, N_EXPERTS, dtype=jnp.float32) * w_sel[..., None], axis=1)
    combine = combine.astype(h.dtype)
    out = jnp.zeros_like(h)
    for e in range(N_EXPERTS):
        hid = jax.nn.silu(h @ w_gate[e]) * (h @ w_up[e])
        out = out + combine[:, e:e + 1] * (hid @ w_down[e])
    return out


def setup_inputs(seed: int = 0) -> dict:
    key = jax.random.key(seed)
    ks = jax.random.split(key, 24)
    f32 = jnp.float32
    nrm = lambda k, shape, s: jax.random.normal(k, shape, f32) * s
    D = D_MODEL
    return {
        "x": nrm(ks[0], (BATCH, SEQ, D), 1.0),
        "c": nrm(ks[1], (BATCH, D), 1.0),
        "ctx": nrm(ks[2], (BATCH, CTX_LEN, D), 1.0),
        "c_ctx": nrm(ks[3], (D,), 1.0),
        "ada_w": nrm(ks[4], (DEPTH, D, N_MOD * D), 0.5 * D ** -0.5),
        "ada_b": nrm(ks[5], (DEPTH, N_MOD * D), 0.02),
        "norm_mix_g": 1.0 + nrm(ks[6], (DEPTH, D), 0.02),
        "norm_ffn_g": 1.0 + nrm(ks[7], (DEPTH, D), 0.02),
        "even_w_in": nrm(ks[8], (N_EVEN, D, EVEN_IN), D ** -0.5),
        "even_conv_w": nrm(ks[9], (N_EVEN, CONV_K, CONV_WIDTH), CONV_K ** -0.5),
        "even_w_out": nrm(ks[10], (N_EVEN, EVEN_MIX, D), EVEN_MIX ** -0.5),
        "odd_w_in": nrm(ks[11], (N_ODD, D, ODD_IN), D ** -0.5),
        "odd_pool_w": nrm(ks[12], (N_ODD, POOL_GROUPS, POOL_GROUP_DIM, POOL_GROUP_DIM), POOL_GROUP_DIM ** -0.5),
        "odd_pool_scale": 1.0 + nrm(ks[13], (N_ODD, POOL_WIDTH), 0.02),
        "odd_sink": nrm(ks[14], (N_ODD, N_Q_HEADS), 0.5),
        "odd_w_out": nrm(ks[15], (N_ODD, ODD_MIX, D), ODD_MIX ** -0.5),
        "router_w": nrm(ks[16], (D, N_EXPERTS), D ** -0.5),
        "router_b": nrm(ks[17], (N_EXPERTS,), 0.01),
        "moe_w_gate": nrm(ks[18], (DEPTH, N_EXPERTS, D, D_EXPERT), D ** -0.5),
        "moe_w_up": nrm(ks[19], (DEPTH, N_EXPERTS, D, D_EXPERT), D ** -0.5),
        "moe_w_down": nrm(ks[20], (DEPTH, N_EXPERTS, D_EXPERT, D), D_EXPERT ** -0.5),
        "final_g": 1.0 + nrm(ks[21], (D,), 0.02),
    }


def reference(x, c, ctx, c_ctx, ada_w, ada_b, norm_mix_g, norm_ffn_g,
              even_w_in, even_conv_w, even_w_out,
              odd_w_in, odd_pool_w, odd_pool_scale, odd_sink, odd_w_out,
              router_w, router_b, moe_w_gate, moe_w_up, moe_w_down, final_g):
    b, n, d = x.shape
    l = ctx.shape[1]
    rows = n // GRID_W
    row_ids = jnp.repeat(jnp.arange(rows), GRID_W)
    col_ids = jnp.tile(jnp.arange(GRID_W), rows)
    cos, sin = axial_rope_tables(row_ids, col_ids)
    s_lat = jax.nn.silu(c)
    s_ctx = jax.nn.silu(c_ctx)
    h, hc = x, ctx
    for layer in range(DEPTH):
        last = layer == DEPTH - 1
        j = layer // 2
        m = [z[:, None, :] for z in ada_mod(s_lat, ada_w[layer], ada_b[layer])]
        mc = ada_mod(s_ctx, ada_w[layer], ada_b[layer])
        a = modulate(rmsnorm(h, norm_mix_g[layer]), m[0], m[1])
        if layer % 2 == 0:
            y = even_mixer(a, even_w_in[j], even_conv_w[j], even_w_out[j])
            if not last:
                ac = modulate(rmsnorm(hc, norm_mix_g[layer]), mc[0], mc[1])
                yc = even_mixer(ac, even_w_in[j], even_conv_w[j], even_w_out[j])
        else:
            ac = modulate(rmsnorm(hc, norm_mix_g[layer]), mc[0], mc[1])
            y, yc = odd_mixer(a, ac, cos, sin, odd_w_in[j], odd_pool_w[j], odd_pool_scale[j],
                              odd_sink[j], odd_w_out[j], not last)
        h = h + m[2] * y
        f = modulate(rmsnorm(h, norm_ffn_g[layer]), m[3], m[4])
        if not last:
            hc = hc + mc[2] * yc
            fc = modulate(rmsnorm(hc, norm_ffn_g[layer]), mc[3], mc[4])
            tokens = jnp.concatenate([f.reshape(b * n, d), fc.reshape(b * l, d)], axis=0)
            out = grouped_moe(tokens, router_w, router_b,
                              moe_w_gate[layer], moe_w_up[layer], moe_w_down[layer])
            h = h + m[5] * out[:b * n].reshape(b, n, d)
            hc = hc + mc[5] * out[b * n:].reshape(b, l, d)
        else:
            out = grouped_moe(f.reshape(b * n, d), router_w, router_b,
                              moe_w_gate[layer], moe_w_up[layer], moe_w_down[layer])
            h = h + m[5] * out.reshape(b, n, d)
    return rmsnorm(h, final_g)
```

```python
import numpy as np
import ml_dtypes
import concourse.bass as bass
import concourse.mybir as mybir
from concourse.bass_utils import run_bass_kernel_spmd

F32 = mybir.dt.float32
BF16 = mybir.dt.bfloat16
AF = mybir.ActivationFunctionType
ALU = mybir.AluOpType
AX = mybir.AxisListType

D = 1024
SEQ = 4096
LCTX = 256
NTOK = SEQ + LCTX
NT = NTOK // 128
NE = 16
EPS = 1e-6
BIG = 1.0e4


class Buf:
    __slots__ = ("name", "last_w", "readers")

    def __init__(self, name=""):
        self.name = name
        self.last_w = None
        self.readers = []


class Op:
    __slots__ = ("eng", "emit", "deps", "is_dma", "signal", "sem", "val")

    def __init__(self, eng, emit, is_dma):
        self.eng = eng
        self.emit = emit
        self.is_dma = is_dma
        self.deps = []
        self.signal = False
        self.sem = None
        self.val = None


ENGS = ("sync", "act", "dve", "pool", "pe")
NDMA_SEMS = {"sync": 16, "act": 8, "pool": 32}


class Prog:
    def __init__(self, nc):
        self.nc = nc
        self.ops = {e: [] for e in ENGS}
        self.ctx = []
        self.last_compute = {}
        self.dma_since = []
        self.pending = {}

    def enter(self, cm):
        v = cm.__enter__()
        self.ctx.append(cm)
        return v

    def sbuf(self, name, shape, dt):
        return self.enter(self.nc.sbuf_tensor(name, list(shape), dt))

    def psum(self, name, shape, dt):
        return self.enter(self.nc.psum_tensor(name, list(shape), dt))

    def close(self):
        for cm in reversed(self.ctx):
            cm.__exit__(None, None, None)
        self.ctx = []

    def op(self, eng, emit, reads=(), writes=(), dma=False):
        o = Op(eng, emit, dma)
        deps = {}
        for b in reads:
            if b.last_w is not None:
                deps[id(b.last_w)] = (b.last_w, True)
        for b in writes:
            if b.last_w is not None and id(b.last_w) not in deps:
                deps[id(b.last_w)] = (b.last_w, False)
            for r in b.readers:
                if id(r) not in deps:
                    deps[id(r)] = (r, False)
        if eng in self.pending:
            for d in self.pending.pop(eng):
                if id(d) not in deps and not (d.eng == eng and not d.is_dma):
                    deps[id(d)] = (d, True)
        for d, raw in deps.values():
            if d is o:
                continue
            if d.eng == o.eng and not d.is_dma:
                if raw and o.eng != "pe" and not o.is_dma:
                    o.deps.append(d)
                    d.signal = True
                elif o.is_dma:
                    o.deps.append(d)
                    d.signal = True
                continue
            o.deps.append(d)
            d.signal = True
        for b in reads:
            if not dma:
                b.readers = [r for r in b.readers if r.is_dma or r.eng != eng]
            b.readers.append(o)
        for b in writes:
            b.last_w = o
            b.readers = []
        self.ops[eng].append(o)
        if dma:
            self.dma_since.append(o)
        else:
            self.last_compute[eng] = o
        return o

    def barrier(self):
        pend = list(self.last_compute.values()) + list(self.dma_since)
        self.dma_since = []
        for e in ENGS:
            self.pending[e] = list(self.pending.get(e, [])) + pend

    def dma(self, eng, out, in_, reads=(), writes=(), **kw):
        return self.op(eng, lambda e: e.dma_start(out=out, in_=in_, **kw), reads, writes, dma=True)

    def finalize(self, final_wait_ops=()):
        nc = self.nc
        for o in final_wait_ops:
            o.signal = True
        eng_sem = {e: self.enter(nc.semaphore("c_" + e)) for e in ("act", "dve", "pool", "pe")}
        dma_sems = {e: [self.enter(nc.semaphore(f"d_{e}{i}")) for i in range(n)]
                    for e, n in NDMA_SEMS.items()}
        for e in ENGS:
            cnt = 0
            dcnt = 0
            per_sem_val = {}
            prev_on_sem = {}
            for o in self.ops[e]:
                if o.is_dma:
                    pool = dma_sems[e]
                    k = dcnt % len(pool)
                    dcnt += 1
                    o.sem = pool[k]
                    per_sem_val[k] = per_sem_val.get(k, 0) + 16
                    o.val = per_sem_val[k]
                    if k in prev_on_sem:
                        o.deps.append(prev_on_sem[k])
                    prev_on_sem[k] = o
                    o.signal = True
                elif o.signal:
                    cnt += 1
                    o.sem = eng_sem[e]
                    o.val = cnt
        block = self.enter(nc.Block())
        handles = {"sync": block.sync, "act": block.scalar, "dve": block.vector,
                   "pool": block.gpsimd, "pe": block.tensor}

        def make(e):
            ops = self.ops[e]

            def body(eng):
                waited = {}
                for o in ops:
                    need = {}
                    for d in o.deps:
                        key = id(d.sem)
                        if waited.get(key, 0) >= d.val:
                            continue
                        if key not in need or need[key][1] < d.val:
                            need[key] = (d.sem, d.val)
                    for key, (s, v) in need.items():
                        eng.wait_ge(s, v)
                        waited[key] = v
                    ins = o.emit(eng)
                    if o.signal:
                        ins.then_inc(o.sem, 16 if o.is_dma else 1)
                if e == "sync":
                    for o in final_wait_ops:
                        eng.wait_ge(o.sem, o.val)
            return body

        for e in ENGS:
            if self.ops[e] or e == "sync":
                handles[e](make(e))
        self.close()


_CONST = {}


def _consts():
    if _CONST:
        return _CONST
    bf = ml_dtypes.bfloat16
    N = SEQ
    s = np.arange(N, dtype=np.int64)
    prod = (s[:, None] * s[None, :]) % N
    ang = prod.astype(np.float64) * (2 * np.pi / N)
    cosm = (np.cos(ang) / 64.0).astype(np.float32)
    nsin = (-np.sin(ang) / 64.0).astype(np.float32)
    both = np.stack([cosm, nsin], 0).reshape(2, 32, 128, 8, 512)
    dftN = np.ascontiguousarray(both.transpose(3, 2, 0, 1, 4)).reshape(8, 128, 64, 512)
    _CONST["dftN"] = dftN.astype(bf)
    del prod, ang, cosm, nsin, both, dftN
    sc = np.arange(LCTX)
    angc = ((sc[:, None] * sc[None, :]) % LCTX) * (2 * np.pi / LCTX)
    cb = np.stack([np.cos(angc) / 16.0, -np.sin(angc) / 16.0], 0).reshape(2, 2, 128, 256)
    _CONST["dftC"] = np.ascontiguousarray(cb.transpose(2, 0, 1, 3)).reshape(128, 4, 256).astype(bf)
    j = np.arange(128)
    angd = ((j[:, None] * j[None, :]) % 128) * (2 * np.pi / 128)
    _CONST["dftD"] = np.concatenate([np.cos(angd), np.sin(angd)], 1).astype(np.float32) / np.sqrt(128.0)
    _CONST["dftD"] = _CONST["dftD"].astype(bf)
    quarter = 16
    inv = 10000.0 ** (-np.arange(quarter, dtype=np.float32) / quarter)
    t = np.arange(N)
    pos = np.stack([t // 64, t % 64], 0).astype(np.float32)
    p = np.arange(128)
    d = p % 64
    axis = d // 32
    i = d % 16
    angr = pos[axis, :] * inv[i][:, None]
    _CONST["rope"] = np.stack([np.cos(angr), np.sin(angr)], 1).astype(np.float32)
    half = (d % 32) // 16
    _CONST["rot_src"] = np.where(half == 0, d + 16, d - 16)[:64]
    _CONST["rot_sign"] = np.where(half == 0, -1.0, 1.0)[:64].astype(np.float32)
    pb = np.zeros((128, 20, 128), np.float32)
    for gi, w in enumerate((2, 4, 8, 16)):
        r = w // 2
        for kind in range(5):
            if kind == 0:
                tt, ss = np.arange(128, 256), np.arange(0, 128)
                base_t = 1280
            elif kind == 1:
                tt, ss = np.arange(128, 256), np.arange(128, 256)
                base_t = 1280
            elif kind == 2:
                tt, ss = np.arange(128, 256), np.arange(256, 384)
                base_t = 1280
            elif kind == 3:
                tt, ss = np.arange(0, 128), np.arange(0, 128)
                base_t = 0
            else:
                tt, ss = np.arange(N - 128, N), np.arange(N - 128, N)
                base_t = 0
            if kind < 3:
                tt = tt + base_t
                ss = ss + base_t
            lo = np.clip(tt - r, 0, N)
            hi = np.clip(tt + r + 1, 0, N)
            cnt = (hi - lo).astype(np.float32)
            m = ((ss[:, None] >= lo[None, :]) & (ss[:, None] < hi[None, :])).astype(np.float32) / cnt[None, :]
            m = m - (ss[:, None] == tt[None, :]).astype(np.float32)
            pb[:, gi * 5 + kind, :] = m
    _CONST["poolB"] = pb.astype(bf)
    q = np.arange(128)
    am = np.zeros((128, 2, 128), np.float32)
    am[:, 0, :] = np.where(q[None, :] >= q[:, None], 0.0, -30000.0)
    am[:, 1, :] = np.where(q[None, :] <= q[:, None], 0.0, -30000.0)
    _CONST["amask"] = am
    _CONST["tauc"] = np.tile((np.arange(64, dtype=np.float32) * 256.0)[None, :], (128, 1))
    _CONST["pidx"] = np.arange(128, dtype=np.float32).reshape(128, 1)
    return _CONST


def build(debug=False, stop_after=None):
    nc = bass.Bass("TRN2", target_bir_lowering=False)

    def din(name, shape, dt=F32):
        return nc.dram_tensor(name, list(shape), dt, kind="ExternalInput").ap()

    def dscr(name, shape, dt=F32):
        return nc.dram_tensor(name, list(shape), dt, kind="Internal").ap()

    x = din("x", [SEQ, D])
    ctx = din("ctx", [LCTX, D])
    c2 = din("c2", [2, D])
    ada_w = din("ada_w", [2, D, 6 * D])
    ada_b = din("ada_b", [2, 6 * D])
    norm_mix_g = din("norm_mix_g", [2, D])
    norm_ffn_g = din("norm_ffn_g", [2, D])
    even_w_in = din("even_w_in", [D, 2048])
    even_conv_w = din("even_conv_w", [3, 512])
    even_w_out = din("even_w_out", [D, D])
    odd_w_in = din("odd_w_in", [D, 1280])
    odd_pool_w = din("odd_pool_w", [4, 128, 128])
    odd_pool_scale = din("odd_pool_scale", [512])
    odd_sink = din("odd_sink", [8])
    odd_w_out = din("odd_w_out", [D, D])
    router_w = din("router_w", [D, NE])
    router_b = din("router_b", [NE])
    moe_w_gate = din("moe_w_gate", [2, NE, D, 512])
    moe_w_up = din("moe_w_up", [2, NE, D, 512])
    moe_w_down = din("moe_w_down", [2, NE, 512, D])
    final_g = din("final_g", [D])
    dftN = din("dftN", [8, 128, 64, 512], BF16)
    dftC = din("dftC", [128, 4, 256], BF16)
    dftD = din("dftD", [128, 256], BF16)
    rope = din("rope", [128, 2, SEQ])
    poolB = din("poolB", [128, 20, 128], BF16)
    amask = din("amask", [128, 2, 128])
    tauc = din("tauc", [128, 64])
    pidx = din("pidx", [128, 1])
    out = nc.dram_tensor("out", [SEQ, D], F32, kind="ExternalOutput").ap()

    Mscr = dscr("Mscr", [2, 2, 6 * D])
    Hm = dscr("Hm", [NTOK, D])
    H1 = dscr("H1", [NTOK, D])
    ZT = dscr("ZT", [4, 128, NTOK], BF16)
    GBT = dscr("GBT", [4, 128, NTOK], BF16)
    FT = dscr("FT", [8, 128, NTOK], BF16)
    CW = dscr("CW", [NTOK, NE])
    Fd = dscr("Fd", [NTOK, D], BF16)
    NSLOT = 50 * 256
    Xs = dscr("Xs", [NSLOT, D], BF16)
    Ys = dscr("Ys", [NSLOT, D])
    WGU = [dscr(f"WGU{l}", [NE * 128, 8 * 1024], BF16) for l in range(2)]
    WD = [dscr(f"WD{l}", [NE * 128, 4 * 1024], BF16) for l in range(2)]
    bFd = Buf()
    bWGU = [Buf(), Buf()]
    bWD = [Buf(), Buf()]
    I32 = mybir.dt.int32
    bM = [Buf(), Buf()]
    bHm, bH1, bZT, bGBT, bFT, bCW = Buf(), Buf(), Buf(), Buf(), Buf(), Buf()
    dbg = {}
    if debug:
        for nm, shp in (("d_hm0", [NTOK, D]), ("d_cw0", [NTOK, NE]), ("d_h1", [NTOK, D]),
                        ("d_hm1", [NTOK, D]), ("d_cw1", [NTOK, NE]), ("d_M", [2, 2, 6 * D])):
            dbg[nm] = nc.dram_tensor(nm, shp, F32, kind="ExternalOutput").ap()

    P = Prog(nc)
    ARENA_ELEMS = 101 * 1024
    arena = P.sbuf("arena", [128, ARENA_ELEMS], BF16)
    pers = P.sbuf("pers", [128, 2048], BF16)
    st = {"off": 0, "poff": 0}

    def _carve(base, off, shape, dt):
        n = int(np.prod(shape[1:]))
        esz = 2 if dt == BF16 else 4
        nb = n * esz
        ap = base[0:shape[0], off // 2:(off + nb) // 2]
        if dt != BF16:
            ap = ap.bitcast(dt)
        if len(shape) == 3:
            ap = ap.rearrange("p (a b) -> p a b", a=shape[1], b=shape[2])
        elif len(shape) == 4:
            ap = ap.rearrange("p (a b c) -> p a b c", a=shape[1], b=shape[2], c=shape[3])
        return ap, (nb + 31) // 32 * 32

    def T(shape, dt):
        ap, nb = _carve(arena, st["off"], shape, dt)
        st["off"] += nb
        assert st["off"] <= ARENA_ELEMS * 2, st["off"]
        return ap

    def TP(shape, dt):
        ap, nb = _carve(pers, st["poff"], shape, dt)
        st["poff"] += nb
        assert st["poff"] <= 4096, st["poff"]
        return ap

    def new_phase():
        P.barrier()
        st["off"] = 0

    class Rot:
        def __init__(self, n, shape, dt):
            self.t = [T(shape, dt) for _ in range(n)]
            self.b = [Buf() for _ in range(n)]
            self.i = 0

        def next(self):
            k = self.i % len(self.t)
            self.i += 1
            return self.t[k], self.b[k]

    pairs = [P.psum(f"pp{i}", [128, 1024], F32) for i in range(4)]
    sloi_t = P.sbuf("sloi_t", [128, NT], mybir.dt.int32)
    shii_t = P.sbuf("shii_t", [128, NT], mybir.dt.int32)
    widx_t = P.sbuf("widx_t", [128, 64], mybir.dt.int32)
    psb = [pairs[i // 2][:, (i % 2) * 512:(i % 2 + 1) * 512] for i in range(8)]
    bps = [Buf() for _ in range(8)]

    def ps_bf(i):
        return psb[i].bitcast(BF16)

    ident = TP([128, 128], BF16)
    b_ident = Buf()
    epsT = TP([128, 1], F32)
    b_eps = Buf()
    P.op("pool", lambda e: e.memset(ident, 0.0), writes=[b_ident])
    P.op("pool", lambda e: e.affine_select(out=ident, in_=ident, pattern=[[-1, 128]],
                                           compare_op=ALU.not_equal, fill=1.0, base=0,
                                           channel_multiplier=1), reads=[b_ident], writes=[b_ident])
    P.op("pool", lambda e: e.memset(epsT, EPS), writes=[b_eps])
    Wr = TP([128, 8, NE], BF16)
    b_Wr = Buf()
    P.dma("pool", Wr, router_w.rearrange("(k p) n -> p k n", p=128), writes=[b_Wr])
    rbT = TP([128, NE], F32)
    b_rb = Buf()
    P.dma("sync", rbT, router_b.partition_broadcast(128), writes=[b_rb])
    stats = TP([128, 64], F32)
    stat_i = [0]

    def ada_layer(l):
        c2raw = T([128, 2, 8], F32)
        b_c2 = Buf()
        for r in range(2):
            P.dma("sync", c2raw[:, r, :], c2[r].rearrange("(p k) -> p k", k=8), writes=[b_c2])
        sT = T([128, 8, 2], F32)
        b_sT = Buf()
        P.op("act", lambda e: e.activation(out=sT.rearrange("p k r -> p r k"), in_=c2raw, func=AF.Silu),
             reads=[b_c2], writes=[b_sT])
        CW_ = 256
        wA = Rot(2, [128, 8, CW_], F32)
        adabs = Rot(2, [2, CW_], F32)
        msbs = Rot(2, [2, CW_], F32)
        awv = ada_w[l].rearrange("(p k) n -> p k n", k=8)
        for j in range(6 * D // CW_):
            cs_ = slice(j * CW_, (j + 1) * CW_)
            wt, bw = wA.next()
            P.dma("sync", wt, awv[:, :, cs_], writes=[bw])
            ab, bab = adabs.next()
            P.dma("sync", ab, ada_b[l, cs_].partition_broadcast(2), writes=[bab])
            pb_i = j % 2

            def mm(e, wt=wt, pb_i=pb_i):
                for k in range(8):
                    ins = e.matmul(psb[pb_i][0:2, 0:CW_], lhsT=sT[:, k, :], rhs=wt[:, k, :],
                                   start=(k == 0), stop=(k == 7))
                return ins
            P.op("pe", mm, reads=[b_sT, bw], writes=[bps[pb_i]])
            mb, bmb = msbs.next()
            P.op("dve", lambda e, mb=mb, ab=ab, pb_i=pb_i: e.tensor_tensor(
                out=mb[0:2, :], in0=psb[pb_i][0:2, 0:CW_], in1=ab[0:2, :], op=ALU.add),
                reads=[bps[pb_i], bab], writes=[bmb])
            P.dma("act", Mscr[l][:, cs_], mb[0:2, :], reads=[bmb], writes=[bM[l]])
            if debug:
                dbg_ops.append(P.dma("act", dbg["d_M"][l][:, cs_], mb[0:2, :], reads=[bmb]))

    def phase0():
        ada_layer(0)

    dbg_ops = []
    phase0()

    _bregs = {}

    def breg(e, v):
        if v not in _bregs:
            _bregs[v] = e.to_reg(v)
        return _bregs[v]

    def conv_jobs():
        for l in range(2):
            for ex in range(NE):
                rows = slice(ex * 128, (ex + 1) * 128)
                gv = WGU[l][rows, :].rearrange("p (k n) -> p k n", k=8)
                yield (gv[:, :, 0:512], moe_w_gate[l, ex].rearrange("(k p) n -> p k n", p=128), bWGU[l])
                yield (gv[:, :, 512:1024], moe_w_up[l, ex].rearrange("(k p) n -> p k n", p=128), bWGU[l])
                yield (WD[l][rows, :].rearrange("p (k n) -> p k n", k=4),
                       moe_w_down[l, ex].rearrange("(k p) n -> p k n", p=128), bWD[l])
    conv_it = conv_jobs()
    conv_left = [96]

    def convert_upto(total):
        convert_some(max(0, conv_left[0] - (96 - total)))

    def convert_some(n):
        for _ in range(n):
            if conv_left[0] == 0:
                return
            o_, i_, _b = next(conv_it)
            conv_left[0] -= 1
            P.dma("pool", o_, i_, writes=[Buf()])

    def load_mod(l, row, which, dst, bdst):
        P.dma("sync", dst, Mscr[l, row, which * D:(which + 1) * D].partition_broadcast(128),
              reads=[bM[l]], writes=[bdst])

    def make_GS(l, gvec, shift_i, scale_i, rows=(0, 1)):
        gt = T([128, D], F32)
        bg = Buf()
        P.dma("sync", gt, gvec.partition_broadcast(128), writes=[bg])
        res = {}
        for row in rows:
            G = T([128, D], F32)
            S = T([128, D], F32)
            bG, bS = Buf(), Buf()
            load_mod(l, row, scale_i, G, bG)
            load_mod(l, row, shift_i, S, bS)
            P.op("dve", lambda e, G=G: e.scalar_tensor_tensor(out=G, in0=G, scalar=1.0, in1=gt,
                                                               op0=ALU.add, op1=ALU.mult),
                 reads=[bG, bg], writes=[bG])
            res[row] = (G, bG, S, bS)
        return res

    def norm_mod(ht, bh, G, bG, S, bS, tmp, btmp, a_out, ba):
        c = stat_i[0] % 32
        stat_i[0] += 1
        ss = stats[:, 2 * c:2 * c + 1]
        rs = stats[:, 2 * c + 1:2 * c + 2]
        bs = Buf()
        P.op("act", lambda e: e.activation(out=tmp, in_=ht, func=AF.Square, accum_out=ss),
             reads=[bh], writes=[btmp, bs])
        P.op("act", lambda e: e.activation(out=rs, in_=ss, func=AF.Ln, bias=epsT[:, 0:1], scale=1.0 / D),
             reads=[bs, b_eps], writes=[bs])
        P.op("act", lambda e: e.activation(out=rs, in_=rs, func=AF.Exp, scale=-0.5), reads=[bs], writes=[bs])
        if G is None:
            return rs, bs
        P.op("dve", lambda e: e.scalar_tensor_tensor(out=tmp, in0=ht, scalar=rs, in1=G,
                                                     op0=ALU.mult, op1=ALU.mult),
             reads=[bh, bs, bG], writes=[btmp])
        P.op("dve", lambda e: e.tensor_tensor(out=a_out, in0=tmp, in1=S, op=ALU.add),
             reads=[btmp, bS], writes=[ba])
        return rs, bs

    def transpose8(a_bf, ba, pbank, dstT, bdst, col0):
        pv = ps_bf(pbank).rearrange("p (k c) -> p k c", k=8)

        def tr(e):
            for k in range(8):
                ins = e.transpose(out=pv[:, k, :], in_=a_bf[:, k * 128:(k + 1) * 128], identity=ident)
            return ins
        P.op("pe", tr, reads=[ba, b_ident], writes=[bps[pbank]])
        P.op("act", lambda e: e.copy(out=dstT[:, :, col0:col0 + 128], in_=pv),
             reads=[bps[pbank]], writes=[bdst])

    def src_rows(i):
        if i < 32:
            return x[i * 128:(i + 1) * 128, :]
        return ctx[(i - 32) * 128:(i - 31) * 128, :]

    def routing(psR_bank, ntile, cwt, bcw, sc, bsc, aff_ready=False):
        n = ntile
        lg = psb[psR_bank][:, 0:n * 16]
        aff = sc[:, 0:n, 0:16]
        sel = sc[:, 0:n, 16:32]
        prs = sc[:, 0:n, 32:56]
        gs = sc[:, 0:n, 56:60]
        gmx = sc[:, 0:n, 60:61]
        gmk = sc[:, 0:n, 61:65]
        msel = sc[:, 0:n, 65:81]
        m1 = sc[:, 0:n, 81:82]
        tmp = sc[:, 0:n, 82:98]
        m2 = sc[:, 0:n, 98:99]
        wsum = sc[:, 0:n, 99:100]
        rd, wr = [bps[psR_bank], bsc, b_rb], [bsc]
        if not aff_ready:
            P.op("act", lambda e: e.activation(out=aff, in_=lg.rearrange("p (n e) -> p n e", e=16), func=AF.Sigmoid),
                 reads=rd, writes=wr)
        else:
            rd = [bsc, b_rb]
        P.op("dve", lambda e: e.tensor_tensor(out=sel, in0=aff, in1=rbT.unsqueeze(1).to_broadcast([128, n, 16]),
                                              op=ALU.add), reads=rd, writes=wr)
        sel4 = sel.rearrange("p n (g k) -> p n g k", k=4)
        prs4 = prs.rearrange("p n (g k) -> p n g k", k=6)
        pi = 0
        for a in range(4):
            for b in range(a + 1, 4):
                P.op("dve", lambda e, a=a, b=b, pi=pi: e.tensor_tensor(
                    out=prs4[:, :, :, pi], in0=sel4[:, :, :, a], in1=sel4[:, :, :, b], op=ALU.add),
                    reads=[bsc], writes=wr)
                pi += 1
        P.op("dve", lambda e: e.tensor_reduce(out=gs, in_=prs4, axis=AX.X, op=ALU.max), reads=[bsc], writes=wr)
        P.op("dve", lambda e: e.tensor_reduce(out=sc[:, 0:n, 60], in_=gs, axis=AX.X, op=ALU.max), reads=[bsc], writes=wr)
        P.op("dve", lambda e: e.tensor_tensor(out=gmk, in0=gs, in1=gmx.to_broadcast([128, n, 4]), op=ALU.is_ge),
             reads=[bsc], writes=wr)
        P.op("dve", lambda e: e.tensor_scalar(out=gmk, in0=gmk, scalar1=BIG, scalar2=-BIG, op0=ALU.mult, op1=ALU.add),
             reads=[bsc], writes=wr)
        P.op("dve", lambda e: e.tensor_tensor(out=msel.rearrange("p n (g k) -> p n g k", k=4), in0=sel4,
                                              in1=gmk.unsqueeze(3).to_broadcast([128, n, 4, 4]), op=ALU.add),
             reads=[bsc], writes=wr)
        P.op("dve", lambda e: e.tensor_reduce(out=sc[:, 0:n, 81], in_=msel, axis=AX.X, op=ALU.max), reads=[bsc], writes=wr)
        P.op("dve", lambda e: e.tensor_tensor(out=tmp, in0=msel, in1=m1.to_broadcast([128, n, 16]), op=ALU.is_ge),
             reads=[bsc], writes=wr)
        P.op("dve", lambda e: e.scalar_tensor_tensor(out=tmp, in0=tmp, scalar=-BIG, in1=msel,
                                                     op0=ALU.mult, op1=ALU.add), reads=[bsc], writes=wr)
        P.op("dve", lambda e: e.tensor_reduce(out=sc[:, 0:n, 98], in_=tmp, axis=AX.X, op=ALU.max), reads=[bsc], writes=wr)
        P.op("dve", lambda e: e.tensor_tensor(out=tmp, in0=msel, in1=m2.to_broadcast([128, n, 16]), op=ALU.is_ge),
             reads=[bsc], writes=wr)
        P.op("dve", lambda e: e.tensor_tensor(out=tmp, in0=tmp, in1=aff, op=ALU.mult), reads=[bsc], writes=wr)
        P.op("dve", lambda e: e.tensor_reduce(out=sc[:, 0:n, 99], in_=tmp, axis=AX.X, op=ALU.add), reads=[bsc], writes=wr)
        P.op("dve", lambda e: e.reciprocal(out=wsum, in_=wsum), reads=[bsc], writes=wr)
        P.op("dve", lambda e: e.tensor_tensor(out=cwt[:, 0:n, :], in0=tmp, in1=wsum.to_broadcast([128, n, 16]), op=ALU.mult),
             reads=[bsc], writes=[bcw])

    def layer0_A():
        new_phase()
        Vcs = T([128, NT, 1024], BF16)
        bV = Buf()
        keep = st["off"]
        Wcs = T([128, 8, 1024], BF16)
        bWcs = Buf()
        Wconv = T([128, 8, 1536], BF16)
        bWconv = Buf()
        P.dma("pool", Wconv, even_w_in[:, 512:2048].rearrange("(k p) n -> p k n", p=128), writes=[bWconv])
        Wf = T([128, 8, 512], BF16)
        bWf = Buf()
        P.dma("pool", Wf, even_w_in[:, 0:512].rearrange("(k p) n -> p k n", p=128), writes=[bWf])
        dD = T([128, 256], BF16)
        bdD = Buf()
        P.dma("sync", dD, dftD, writes=[bdD])
        WfT = T([128, 1024], BF16)
        bWfT = Buf()
        for h in range(4):
            pv = ps_bf(0).rearrange("p (k c) -> p k c", k=8)

            def tr(e, h=h, pv=pv):
                for k in range(8):
                    ins = e.transpose(out=pv[:, k, :], in_=Wf[:, k, h * 128:(h + 1) * 128], identity=ident)
                return ins
            P.op("pe", tr, reads=[bWf, b_ident], writes=[bps[0]])
            P.op("act", lambda e: e.copy(out=WfT, in_=ps_bf(0)), reads=[bps[0]], writes=[bWfT])
            for k in range(8):
                pbk = 1 + (k % 2)
                P.op("pe", lambda e, k=k, pbk=pbk: e.matmul(psb[pbk][:, 0:256], lhsT=WfT[:, k * 128:(k + 1) * 128],
                                                            rhs=dD, start=True, stop=True),
                     reads=[bWfT, bdD], writes=[bps[pbk]])
                P.op("dve", lambda e, k=k, h=h, pbk=pbk: e.tensor_copy(
                    out=Wcs[:, k, :].rearrange("p (two hh c) -> p two hh c", two=2, hh=4)[:, :, h, :],
                    in_=psb[pbk][:, 0:256].rearrange("p (two c) -> p two c", two=2)),
                    reads=[bps[pbk]], writes=[bWcs])
        GS = make_GS(0, norm_mix_g[0], 0, 1)
        hts = Rot(3, [128, D], F32)
        tmps = Rot(2, [128, D], F32)
        abf = Rot(2, [128, D], BF16)
        aT4s = Rot(2, [128, 8, 512], BF16)
        zts = Rot(2, [128, 4, 512], BF16)
        gbs = Rot(2, [128, 4, 512], BF16)
        gcs = Rot(2, [128, 512], F32)
        def l0_stage1(i):
            row = 0 if i < 32 else 1
            G, bG, S, bS = GS[row]
            ht, bh = hts.next()
            P.dma("sync", ht, src_rows(i), writes=[bh])
            tmp, btmp = tmps.next()
            a, ba = abf.next()
            norm_mod(ht, bh, G, bG, S, bS, tmp, btmp, a, ba)
            return a, ba

        def l0_stage2(i, j, a, ba, aT4, baT4):
            transpose8(a, ba, i % 2, aT4, baT4, j * 128)
            pb0 = 2 + 2 * (i % 2)

            def mmv(e):
                for hf in range(2):
                    for k in range(8):
                        ins = e.matmul(psb[pb0 + hf], lhsT=aT4[:, k, j * 128:(j + 1) * 128],
                                       rhs=Wcs[:, k, hf * 512:(hf + 1) * 512], start=(k == 0), stop=(k == 7))
                return ins
            P.op("pe", mmv, reads=[baT4, bWcs], writes=[bps[pb0], bps[pb0 + 1]])
            P.op("act", lambda e: e.copy(out=Vcs[:, i, 0:512], in_=psb[pb0]), reads=[bps[pb0]], writes=[bV])
            P.op("dve", lambda e: e.tensor_copy(out=Vcs[:, i, 512:1024], in_=psb[pb0 + 1]), reads=[bps[pb0 + 1]], writes=[bV])

        ngroups = 9
        pend = l0_stage1(0)
        for g in range(ngroups):
            ntile = 4 if g < 8 else 2
            gw = ntile * 128
            t0 = g * 512
            aT4, baT4 = aT4s.next()
            for j in range(ntile):
                i = g * 4 + j
                nxt = l0_stage1(i + 1) if i + 1 < NT else None
                l0_stage2(i, j, pend[0], pend[1], aT4, baT4)
                pend = nxt
            zt, bz = zts.next()
            gb, bgb = gbs.next()
            for cc in range(4):
                for part in (1, 0, 2):
                    c = part * 4 + cc
                    pbk = 6 + (c % 2)

                    def mmc(e, c=c, pbk=pbk, aT4=aT4, gw=gw):
                        for k in range(8):
                            ins = e.matmul(psb[pbk][:, 0:gw], lhsT=Wconv[:, k, c * 128:(c + 1) * 128],
                                           rhs=aT4[:, k, 0:gw], start=(k == 0), stop=(k == 7))
                        return ins
                    P.op("pe", mmc, reads=[baT4, bWconv], writes=[bps[pbk]])
                    if part == 1:
                        gc, bgc = gcs.next()
                        P.op("act", lambda e, gc=gc, pbk=pbk, gw=gw: e.copy(out=gc[:, 0:gw], in_=psb[pbk][:, 0:gw]),
                             reads=[bps[pbk]], writes=[bgc])
                    elif part == 0:
                        P.op("act", lambda e, gb=gb, cc=cc, pbk=pbk, gw=gw: e.copy(out=gb[:, cc, 0:gw], in_=psb[pbk][:, 0:gw]),
                             reads=[bps[pbk]], writes=[bgb])
                    else:
                        P.op("dve", lambda e, zt=zt, cc=cc, pbk=pbk, gc=gc, gw=gw: e.tensor_tensor(
                            out=zt[:, cc, 0:gw], in0=psb[pbk][:, 0:gw], in1=gc[:, 0:gw], op=ALU.mult),
                            reads=[bps[pbk], bgc], writes=[bz])
            convert_some(3)
            P.dma("act", ZT[:, :, t0:t0 + gw].rearrange("c p t -> p c t"), zt[:, :, 0:gw], reads=[bz], writes=[bZT])
            P.dma("act", GBT[:, :, t0:t0 + gw].rearrange("c p t -> p c t"), gb[:, :, 0:gw], reads=[bgb], writes=[bGBT])
        return Vcs, bV, keep

    def mixer_tail(l, g, ntile, yT, byT, Wout, bWout, gate1, GS2, hts, tmps, fbf, fT4, bfT4, hsrc_fn, psY0, psT_bank,
                   psR_bank, cwt, bcw, rsc, brsc, dbg_hm=None, filler=None, deferred=False):
        gw = ntile * 128
        t0 = g * 512

        def stA(j):
            i = g * 4 + j
            row = 0 if i < 32 else 1

            def mmo(e):
                for hf in range(2):
                    for k in range(8):
                        ins = e.matmul(psb[psY0 + hf], lhsT=yT[:, k, j * 128:(j + 1) * 128],
                                       rhs=Wout[:, k, hf * 512:(hf + 1) * 512], start=(k == 0), stop=(k == 7))
                return ins
            P.op("pe", mmo, reads=[byT, bWout], writes=[bps[psY0], bps[psY0 + 1]])
            ht, bh = hts.next()
            hsrc, hreads = hsrc_fn(i)
            P.dma("sync", ht, hsrc, reads=hreads, writes=[bh])
            tmp, btmp = tmps.next()
            g1, bg1 = gate1[row]
            for hf in range(2):
                P.op("dve", lambda e, hf=hf: e.tensor_tensor(
                    out=tmp[:, hf * 512:(hf + 1) * 512], in0=psb[psY0 + hf], in1=g1[:, hf * 512:(hf + 1) * 512],
                    op=ALU.mult), reads=[bps[psY0 + hf], bg1], writes=[btmp])
            P.op("dve", lambda e: e.tensor_tensor(out=ht, in0=tmp, in1=ht, op=ALU.add), reads=[btmp, bh], writes=[bh])
            P.dma("pool", Hm[i * 128:(i + 1) * 128, :], ht, reads=[bh], writes=[bHm])
            if dbg_hm is not None:
                dbg_ops.append(P.dma("pool", dbg_hm[i * 128:(i + 1) * 128, :], ht, reads=[bh]))
            G, bG, S, bS = GS2[row]
            f, bf_ = fbf.next()
            norm_mod(ht, bh, G, bG, S, bS, tmp, btmp, f, bf_)
            P.dma("act", Fd[i * 128:(i + 1) * 128, :], f, reads=[bf_], writes=[Buf()])
            return f, bf_

        def stB(j, f, bf_):
            transpose8(f, bf_, psT_bank, fT4, bfT4, j * 128)

            def mmr(e):
                for k in range(8):
                    ins = e.matmul(psb[psR_bank][:, j * 16:(j + 1) * 16], lhsT=fT4[:, k, j * 128:(j + 1) * 128],
                                   rhs=Wr[:, k, :], start=(k == 0), stop=(k == 7))
                return ins
            if deferred:
                def mmr0(e):
                    for k in range(8):
                        ins = e.matmul(psb[psR_bank][:, 0:16], lhsT=fT4[:, k, j * 128:(j + 1) * 128],
                                       rhs=Wr[:, k, :], start=(k == 0), stop=(k == 7))
                    return ins
                P.op("pe", mmr0, reads=[bfT4, b_Wr], writes=[bps[psR_bank]])
                P.op("act", lambda e: e.activation(out=rsc[:, j, 0:16], in_=psb[psR_bank][:, 0:16], func=AF.Sigmoid),
                     reads=[bps[psR_bank]], writes=[brsc])
            else:
                P.op("pe", mmr, reads=[bfT4, b_Wr], writes=[bps[psR_bank]])

        def finish():
            routing(psR_bank, ntile, cwt, bcw, rsc, brsc, aff_ready=deferred)
            P.dma("act", CW[t0:t0 + gw, :].rearrange("(n p) e -> p n e", p=128), cwt[:, 0:ntile, :], reads=[bcw], writes=[bCW])

        if deferred:
            return stA, stB, finish

        pend = stA(0)
        for j in range(ntile):
            if filler is not None:
                filler(1)
            nxt = stA(j + 1) if j + 1 < ntile else None
            if filler is not None:
                filler(1)
            stB(j, pend[0], pend[1])
            pend = nxt
        finish()

    def layer0_B(Vcs, bV, keep):
        P.barrier()
        st["off"] = keep
        Wout = T([128, 8, D], BF16)
        bWout = Buf()
        P.dma("pool", Wout, even_w_out.rearrange("(k p) n -> p k n", p=128), writes=[bWout])
        cwc = T([128, 3, 4], F32)
        bcwc = Buf()
        for kk in range(3):
            P.dma("sync", cwc[:, kk, :], even_conv_w[kk].rearrange("(c p) -> p c", p=128), writes=[bcwc],
                  allow_slow_non_contiguous=True)
        dC = T([128, 4, 256], BF16)
        bdC = Buf()
        P.dma("sync", dC, dftC, writes=[bdC])
        GS2 = make_GS(0, norm_ffn_g[0], 3, 4)
        gate1 = []
        for row in range(2):
            gt = T([128, D], F32)
            bg = Buf()
            load_mod(0, row, 2, gt, bg)
            gate1.append((gt, bg))
        ring = Rot(3, [128, 8, 512], BF16)
        yTs = Rot(2, [128, 8, 512], BF16)
        zin = Rot(1, [128, 4, 514], BF16)
        gbin = Rot(1, [128, 4, 512], BF16)
        cacc = Rot(2, [128, 512], F32)
        hts = Rot(2, [128, D], F32)
        tmps = Rot(2, [128, D], F32)
        fbf = Rot(2, [128, D], BF16)
        fT4s = Rot(1, [128, 8, 512], BF16)
        cws = Rot(2, [128, 4, NE], F32)
        rsc = T([128, 4, 128], F32)
        brsc = Buf()
        def fourier_ops(g, yT, byT):
            ops_ = []
            gw_ = 512 if g < 8 else 256
            if g < 8:
                for piece in range(8):
                    def one(piece=piece):
                        rt, brt = ring.next()
                        P.dma("sync", rt, dftN[g, :, piece * 8:(piece + 1) * 8, :], writes=[brt])

                        def mmf(e):
                            for s8_ in range(8):
                                stt = piece * 8 + s8_
                                base = 0 if stt < 32 else 512
                                for h in range(4):
                                    ins = e.matmul(psb[h], lhsT=Vcs[:, stt % 32, base + h * 128: base + (h + 1) * 128],
                                                   rhs=rt[:, s8_, :], start=(stt == 0), stop=(stt == 63))
                            return ins
                        P.op("pe", mmf, reads=[brt, bV], writes=[bps[0], bps[1], bps[2], bps[3]])
                    ops_.append(one)
            else:
                def onec():
                    def mmfc(e):
                        for jj in range(4):
                            base = 0 if jj < 2 else 512
                            for h in range(4):
                                ins = e.matmul(psb[h][:, 0:256], lhsT=Vcs[:, 32 + (jj % 2), base + h * 128: base + (h + 1) * 128],
                                               rhs=dC[:, jj, :], start=(jj == 0), stop=(jj == 3))
                        return ins
                    P.op("pe", mmfc, reads=[bdC, bV], writes=[bps[0], bps[1], bps[2], bps[3]])
                ops_.append(onec)

            def evac():
                for h in range(4):
                    P.op("act", lambda e, h=h: e.copy(out=yT[:, h, 0:gw_], in_=psb[h][:, 0:gw_]), reads=[bps[h]], writes=[byT])
            ops_.append(evac)
            return ops_

        yT_next = yTs.next()
        pending_f = fourier_ops(0, *yT_next)
        for g in range(9):
            ntile = 4 if g < 8 else 2
            gw = ntile * 128
            t0 = g * 512
            yT, byT = yT_next
            while pending_f:
                pending_f.pop(0)()
            if g + 1 < 9:
                yT_next = yTs.next()
                pending_f = fourier_ops(g + 1, *yT_next)

            def filler(n, pending_f=pending_f):
                for _ in range(n):
                    if pending_f:
                        pending_f.pop(0)()
            zt, bz = zin.next()
            gb, bgb = gbin.next()
            first = g in (0, 8)
            last = g in (7, 8)
            lo = t0 - (0 if first else 1)
            hi = t0 + gw + (0 if last else 1)
            c0 = 1 if first else 0
            if first:
                P.op("pool", lambda e, zt=zt: e.memset(zt[:, :, 0:1], 0.0), writes=[bz])
            if last:
                P.op("pool", lambda e, zt=zt, gw=gw: e.memset(zt[:, :, gw + 1:gw + 2], 0.0), writes=[bz])
            P.dma("sync", zt[:, :, c0:c0 + (hi - lo)], ZT[:, :, lo:hi].rearrange("c p t -> p c t"), reads=[bZT], writes=[bz])
            P.dma("sync", gb[:, :, 0:gw], GBT[:, :, t0:t0 + gw].rearrange("c p t -> p c t"), reads=[bGBT], writes=[bgb])
            for cc in range(4):
                ac, bac = cacc.next()
                P.op("dve", lambda e, ac=ac, zt=zt, cc=cc, gw=gw: e.tensor_scalar(
                    out=ac[:, 0:gw], in0=zt[:, cc, 0:gw], scalar1=cwc[:, 0, cc:cc + 1], scalar2=None, op0=ALU.mult),
                    reads=[bz, bcwc], writes=[bac])
                for kk in (1, 2):
                    P.op("dve", lambda e, ac=ac, zt=zt, cc=cc, gw=gw, kk=kk: e.scalar_tensor_tensor(
                        out=ac[:, 0:gw], in0=zt[:, cc, kk:kk + gw], scalar=cwc[:, kk, cc:cc + 1], in1=ac[:, 0:gw],
                        op0=ALU.mult, op1=ALU.add), reads=[bz, bcwc, bac], writes=[bac])
                P.op("dve", lambda e, ac=ac, gb=gb, cc=cc, gw=gw, yT=yT: e.tensor_tensor(
                    out=yT[:, 4 + cc, 0:gw], in0=ac[:, 0:gw], in1=gb[:, cc, 0:gw], op=ALU.mult),
                    reads=[bac, bgb], writes=[byT])
            convert_some(3)
            fT4, bfT4 = fT4s.next()
            cwt, bcw = cws.next()
            mixer_tail(0, g, ntile, yT, byT, Wout, bWout, gate1, GS2, hts, tmps, fbf, fT4, bfT4,
                       lambda i: (src_rows(i), []), 4, 6, 7, cwt, bcw, rsc, brsc, dbg.get("d_hm0"), filler=filler)

    def moe(l, ntok, Hout, bHout, final):
        new_phase()
        sgs = [(0, 2048), (2048, ntok - 2048)]
        accmax = max(n for _, n in sgs) // 128
        acc = T([128, accmax, D], F32)
        bacc = [Buf() for _ in range(accmax)]
        fTs = T([128, 8, accmax * 128], BF16)
        bfTs = Buf()
        cws = T([128, accmax, NE], F32)
        bcws = Buf()
        wgs = Rot(2, [128, 8, 512], BF16)
        wus = Rot(2, [128, 8, 512], BF16)
        wds = Rot(2, [128, 4, D], BF16)
        hid = Rot(2, [128, 4, 512], BF16)
        sgt = Rot(2, [128, 512], F32)
        g2 = []
        for row in range(2):
            gt = T([128, D], F32)
            bg = Buf()
            load_mod(l, row, 5, gt, bg)
            g2.append((gt, bg))
        hts = Rot(2, [128, D], F32)
        tmps = Rot(2, [128, D], F32)
        if final:
            fg = T([128, D], F32)
            bfg = Buf()
            P.dma("sync", fg, final_g.partition_broadcast(128), writes=[bfg])
        outs = []
        for (s0, sn) in sgs:
            ntl = sn // 128
            P.dma("sync", fTs[:, :, 0:sn], FT[:, :, s0:s0 + sn].rearrange("k p t -> p k t"), reads=[bFT], writes=[bfTs])
            P.dma("sync", cws[:, 0:ntl, :], CW[s0:s0 + sn, :].rearrange("(n p) e -> p n e", p=128), reads=[bCW], writes=[bcws])
            groups = [(q, min(512, sn - q)) for q in range(0, sn, 512)]
            for ex in range(NE):
                wg, bwg = wgs.next()
                wu, bwu = wus.next()
                wd, bwd = wds.next()
                P.dma("pool", wg, moe_w_gate[l, ex].rearrange("(k p) n -> p k n", p=128), writes=[bwg])
                P.dma("pool", wu, moe_w_up[l, ex].rearrange("(k p) n -> p k n", p=128), writes=[bwu])
                P.dma("pool", wd, moe_w_down[l, ex].rearrange("(k p) n -> p k n", p=128), writes=[bwd])
                for (q0, qn) in groups:
                    hd, bhd = hid.next()
                    for jc in range(4):
                        pg, pu = (0, 1) if jc % 2 == 0 else (2, 3)

                        def mmg(e, jc=jc, pg=pg, pu=pu, wg=wg, wu=wu, q0=q0, qn=qn):
                            for (pb_, w_) in ((pg, wg), (pu, wu)):
                                for k in range(8):
                                    ins = e.matmul(psb[pb_][:, 0:qn], lhsT=w_[:, k, jc * 128:(jc + 1) * 128],
                                                   rhs=fTs[:, k, q0:q0 + qn], start=(k == 0), stop=(k == 7))
                            return ins
                        P.op("pe", mmg, reads=[bwg, bwu, bfTs], writes=[bps[pg], bps[pu]])
                        sg, bsg = sgt.next()
                        P.op("act", lambda e, sg=sg, pg=pg, qn=qn: e.activation(out=sg[:, 0:qn], in_=psb[pg][:, 0:qn], func=AF.Silu),
                             reads=[bps[pg]], writes=[bsg])
                        P.op("dve", lambda e, sg=sg, pu=pu, hd=hd, jc=jc, qn=qn: e.tensor_tensor(
                            out=hd[:, jc, 0:qn], in0=psb[pu][:, 0:qn], in1=sg[:, 0:qn], op=ALU.mult),
                            reads=[bps[pu], bsg], writes=[bhd])
                    for jt in range(qn // 128):
                        tl = (q0 // 128) + jt
                        for hf in range(2):
                            pd = 4 + ((jt * 2 + hf) % 4)

                            def mmd(e, pd=pd, hd=hd, jt=jt, hf=hf, wd=wd):
                                for jc in range(4):
                                    ins = e.matmul(psb[pd], lhsT=hd[:, jc, jt * 128:(jt + 1) * 128],
                                                   rhs=wd[:, jc, hf * 512:(hf + 1) * 512], start=(jc == 0), stop=(jc == 3))
                                return ins
                            P.op("pe", mmd, reads=[bhd, bwd], writes=[bps[pd]])
                            av = acc[:, tl, hf * 512:(hf + 1) * 512]
                            if ex == 0:
                                P.op("dve", lambda e, av=av, pd=pd, tl=tl, ex=ex: e.tensor_scalar(
                                    out=av, in0=psb[pd], scalar1=cws[:, tl, ex:ex + 1], scalar2=None, op0=ALU.mult),
                                    reads=[bps[pd], bcws], writes=[bacc[tl]])
                            else:
                                P.op("dve", lambda e, av=av, pd=pd, tl=tl, ex=ex: e.scalar_tensor_tensor(
                                    out=av, in0=psb[pd], scalar=cws[:, tl, ex:ex + 1], in1=av,
                                    op0=ALU.mult, op1=ALU.add), reads=[bps[pd], bcws, bacc[tl]], writes=[bacc[tl]])
            for tl in range(ntl):
                i = s0 // 128 + tl
                row = 0 if i < 32 else 1
                ht, bh = hts.next()
                P.dma("sync", ht, Hm[i * 128:(i + 1) * 128, :], reads=[bHm], writes=[bh])
                gt, bg = g2[row]
                P.op("pool", lambda e, tl=tl, gt=gt: e.tensor_tensor(out=acc[:, tl, :], in0=acc[:, tl, :], in1=gt, op=ALU.mult),
                     reads=[bacc[tl], bg], writes=[bacc[tl]])
                P.op("pool", lambda e, tl=tl, ht=ht: e.tensor_tensor(out=ht, in0=acc[:, tl, :], in1=ht, op=ALU.add),
                     reads=[bacc[tl], bh], writes=[bh])
                if not final:
                    P.dma("pool", Hout[i * 128:(i + 1) * 128, :], ht, reads=[bh], writes=[bHout])
                    if debug:
                        dbg_ops.append(P.dma("pool", dbg["d_h1"][i * 128:(i + 1) * 128, :], ht, reads=[bh]))
                else:
                    tmp, btmp = tmps.next()
                    rs, bs = norm_mod(ht, bh, None, None, None, None, tmp, btmp, None, None)
                    P.op("dve", lambda e, tmp=tmp, ht=ht, rs=rs: e.scalar_tensor_tensor(
                        out=tmp, in0=ht, scalar=rs, in1=fg, op0=ALU.mult, op1=ALU.mult),
                        reads=[bh, bs, bfg], writes=[btmp])
                    outs.append(P.dma("act", out[i * 128:(i + 1) * 128, :], tmp, reads=[btmp]))
        return outs


    def layer1_A():
        new_phase()
        Up = T([128, 32, 512], BF16)
        qT = T([128, 4, SEQ], BF16)
        kT = T([128, 2, NTOK], BF16)
        Vt = T([128, NT, 128], BF16)
        bUp, bqT, bkT, bVt = Buf(), Buf(), Buf(), Buf()
        keep = st["off"]
        Wp = T([128, 8, 512], BF16)
        Wq = T([128, 8, 512], BF16)
        Wqr = T([128, 8, 512], BF16)
        Wk = T([128, 8, 256], BF16)
        Wkr = T([128, 8, 256], BF16)
        Wv = T([128, 8, 128], BF16)
        bWp, bWq, bWqr, bWk, bWkr, bWv = Buf(), Buf(), Buf(), Buf(), Buf(), Buf()
        wv = odd_w_in.rearrange("(k p) n -> p k n", p=128)
        P.dma("pool", Wp, wv[:, :, 0:512], writes=[bWp])
        P.dma("pool", Wq, wv[:, :, 512:1024], writes=[bWq])
        for a in range(4):
            P.dma("pool", Wk[:, :, a * 64:(a + 1) * 64], wv[:, :, 1024 + (a // 2) * 64:1024 + (a // 2 + 1) * 64], writes=[bWk])
        P.dma("pool", Wv, wv[:, :, 1152:1280], writes=[bWv])
        for (src, bsrc, dst, bdst, nblk) in ((Wq, bWq, Wqr, bWqr, 16), (Wk, bWk, Wkr, bWkr, 8)):
            sv = src.rearrange("p k (blk two i) -> p (k blk) two i", two=2, i=16)
            dv = dst.rearrange("p k (blk two i) -> p (k blk) two i", two=2, i=16)
            P.op("dve", lambda e, sv=sv, dv=dv: e.tensor_scalar(out=dv[:, :, 0, :], in0=sv[:, :, 1, :], scalar1=-1.0,
                                                              scalar2=None, op0=ALU.mult), reads=[bsrc], writes=[bdst])
            P.op("dve", lambda e, sv=sv, dv=dv: e.tensor_copy(out=dv[:, :, 1, :], in_=sv[:, :, 0, :]), reads=[bsrc], writes=[bdst])
        GS = make_GS(1, norm_mix_g[1], 0, 1)
        hts = Rot(2, [128, D], F32)
        tmps = Rot(2, [128, D], F32)
        abf = Rot(2, [128, D], BF16)
        aT4s = Rot(2, [128, 8, 512], BF16)
        ropes = Rot(2, [128, 2, 512], F32)
        rt1 = Rot(2, [128, 512], F32)
        rt2 = Rot(2, [128, 512], F32)
        fm_i = [0]

        def l1_stage1(i):
            row = 0 if i < 32 else 1
            G, bG, S, bS = GS[row]
            ht, bh = hts.next()
            P.dma("sync", ht, H1[i * 128:(i + 1) * 128, :], reads=[bH1], writes=[bh])
            tmp, btmp = tmps.next()
            a, ba = abf.next()
            norm_mod(ht, bh, G, bG, S, bS, tmp, btmp, a, ba)
            return a, ba

        def l1_stage2(i, j, a, ba, aT4, baT4):
            transpose8(a, ba, i % 2, aT4, baT4, j * 128)
            if i < 32:
                def mmp(e):
                    for k in range(8):
                        ins = e.matmul(psb[2], lhsT=aT4[:, k, j * 128:(j + 1) * 128], rhs=Wp[:, k, :],
                                       start=(k == 0), stop=(k == 7))
                    return ins
                P.op("pe", mmp, reads=[baT4, bWp], writes=[bps[2]])
                P.op("act", lambda e: e.copy(out=Up[:, i, :], in_=psb[2]), reads=[bps[2]], writes=[bUp])

            def mmvv(e):
                for k in range(8):
                    ins = e.matmul(psb[3][:, 0:128], lhsT=aT4[:, k, j * 128:(j + 1) * 128], rhs=Wv[:, k, :],
                                   start=(k == 0), stop=(k == 7))
                return ins
            P.op("pe", mmvv, reads=[baT4, bWv], writes=[bps[3]])
            P.op("dve", lambda e: e.tensor_copy(out=Vt[:, i, :], in_=psb[3][:, 0:128]), reads=[bps[3]], writes=[bVt])

        pend1 = [l1_stage1(0)]
        for g in range(9):
            ntile = 4 if g < 8 else 2
            gw = ntile * 128
            t0 = g * 512
            aT4, baT4 = aT4s.next()
            for j in range(ntile):
                i = g * 4 + j
                nxt1 = l1_stage1(i + 1) if i + 1 < NT else None
                l1_stage2(i, j, pend1[0][0], pend1[0][1], aT4, baT4)
                pend1[0] = nxt1
            convert_some(3)
            if g < 8:
                rp, brp = ropes.next()
                P.dma("sync", rp, rope[:, :, t0:t0 + 512], writes=[brp])
            jobs = [("k", jj) for jj in range(2)]
            if g < 8:
                jobs = [("q", cc) for cc in range(4)] + jobs
            for (kind, cc) in jobs:
                W, bW, Wr_, bWr_ = (Wq, bWq, Wqr, bWqr) if kind == "q" else (Wk, bWk, Wkr, bWkr)
                pA, pB = (4, 5) if fm_i[0] % 2 == 0 else (6, 7)
                fm_i[0] += 1
                rot = g < 8

                def mmq(e, W=W, Wr_=Wr_, cc=cc, pA=pA, pB=pB, aT4=aT4, gw=gw, rot=rot):
                    for (pb_, w_) in ((pA, W), (pB, Wr_)) if rot else ((pA, W),):
                        for k in range(8):
                            ins = e.matmul(psb[pb_][:, 0:gw], lhsT=w_[:, k, cc * 128:(cc + 1) * 128], rhs=aT4[:, k, 0:gw],
                                           start=(k == 0), stop=(k == 7))
                    return ins
                P.op("pe", mmq, reads=[baT4, bW, bWr_], writes=[bps[pA], bps[pB]])
                dst = qT[:, cc, t0:t0 + gw] if kind == "q" else kT[:, cc, t0:t0 + gw]
                bdst = bqT if kind == "q" else bkT
                if rot:
                    t1, bt1 = rt1.next()
                    t2, bt2 = rt2.next()
                    P.op("dve", lambda e, t1=t1, pA=pA, rp=rp: e.tensor_tensor(out=t1, in0=psb[pA], in1=rp[:, 0, :], op=ALU.mult),
                         reads=[bps[pA], brp], writes=[bt1])
                    P.op("dve", lambda e, t2=t2, pB=pB, rp=rp: e.tensor_tensor(out=t2, in0=psb[pB], in1=rp[:, 1, :], op=ALU.mult),
                         reads=[bps[pB], brp], writes=[bt2])
                    P.op("dve", lambda e, t1=t1, t2=t2, dst=dst: e.tensor_tensor(out=dst, in0=t1, in1=t2, op=ALU.add),
                         reads=[bt1, bt2], writes=[bdst])
                else:
                    P.op("act", lambda e, dst=dst, pA=pA, gw=gw: e.copy(out=dst, in_=psb[pA][:, 0:gw]), reads=[bps[pA]], writes=[bdst])
        return (Up, bUp, qT, bqT, kT, bkT, Vt, bVt), keep

    def layer1_B(pk, keep):
        Up, bUp, qT, bqT, kT, bkT, Vt, bVt = pk
        P.barrier()
        st["off"] = keep
        Wout = T([128, 8, D], BF16)
        bWout = Buf()
        P.dma("pool", Wout, odd_w_out.rearrange("(k p) n -> p k n", p=128), writes=[bWout])
        pB = T([128, 20, 128], BF16)
        bpB = Buf()
        P.dma("sync", pB, poolB, writes=[bpB])
        PW = T([128, 4, 128], BF16)
        bPW = Buf()
        P.dma("pool", PW, odd_pool_w.rearrange("g c d -> c g d"), writes=[bPW])
        psc = T([128, 4], F32)
        bpsc = Buf()
        P.dma("sync", psc, odd_pool_scale.rearrange("(g p) -> p g", p=128), writes=[bpsc], allow_slow_non_contiguous=True)
        mk = T([128, 2, 128], BF16)
        bmk = Buf()
        P.dma("pool", mk, amask, writes=[bmk])
        sk8 = T([128, 8], F32)
        bsk8 = Buf()
        P.dma("sync", sk8, odd_sink.partition_broadcast(128), writes=[bsk8])
        P.op("dve", lambda e: e.tensor_scalar(out=sk8, in0=sk8, scalar1=8.0, scalar2=None, op0=ALU.mult), reads=[bsk8], writes=[bsk8])
        GS2 = make_GS(1, norm_ffn_g[1], 3, 4, rows=(0,))
        gt = T([128, D], F32)
        bg = Buf()
        load_mod(1, 0, 2, gt, bg)
        gate1 = {0: (gt, bg)}
        yTs = Rot(2, [128, 8, 512], BF16)
        Ps = Rot(3, [128, 640], BF16)
        PTs = Rot(3, [128, 5, 128], BF16)
        pTs = Rot(1, [128, 4, 128], BF16)
        Osb = Rot(2, [128, 8, 64], BF16)
        st8 = Rot(2, [128, 8, 8], F32)
        st8h = [[Buf() for _ in range(8)] for _ in range(2)]
        st8b = [Buf(), Buf()]
        hts = Rot(2, [128, D], F32)
        tmps = Rot(2, [128, D], F32)
        fbf = Rot(2, [128, D], BF16)
        fT4s = Rot(1, [128, 8, 512], BF16)
        cws = Rot(2, [128, 4, NE], F32)
        rsc = T([128, 4, 128], F32)
        brsc = Buf()
        hcount = [0]
        def attn_block(i, cs, yT, byT):
            wb = [b_ for b_ in (i - 1, i, i + 1) if 0 <= b_ < 32]
            nw = len(wb) * 128
            w0 = wb[0] * 128
            c_lo = 512 - nw
            s8, bs8 = st8.next()
            osb, bosb = Osb.next()
            pO = psb[6].rearrange("p (h d) -> p h d", h=8)
            slot8 = (st8.i - 1) % 2
            hb = st8h[slot8]

            def st_qk(h):
                c, half, kvj = h // 2, h % 2, h // 4
                pr = slice(half * 64, (half + 1) * 64)
                pi = hcount[0] % 2
                hcount[0] += 1
                pair = pairs[pi]
                bpair = [bps[2 * pi], bps[2 * pi + 1]]

                def mmqk(e, pair=pair, pr=pr, c=c, kvj=kvj):
                    qv = qT[pr, c, i * 128:(i + 1) * 128]
                    e.matmul(pair[:, c_lo:512], lhsT=qv, rhs=kT[pr, kvj, w0:w0 + nw], start=True, stop=False,
                             skip_group_check=True)
                    if wb[0] == i - 1:
                        e.matmul(pair[:, c_lo:c_lo + 128], lhsT=ident, rhs=mk[:, 0, :], start=False, stop=False,
                                 skip_group_check=True)
                    if wb[-1] == i + 1:
                        e.matmul(pair[:, 384:512], lhsT=ident, rhs=mk[:, 1, :], start=False, stop=False,
                                 skip_group_check=True)
                    ins = e.matmul(pair[:, 512:768], lhsT=qv, rhs=kT[pr, kvj, SEQ:SEQ + 256], start=True, stop=True,
                                   skip_group_check=True)
                    return ins
                P.op("pe", mmqk, reads=[bqT, bkT, bmk, b_ident], writes=bpair)
                sv = pair[:, c_lo:768]
                nk = 768 - c_lo
                P.op("dve", lambda e: e.tensor_reduce(out=s8[:, 0, h:h + 1], in_=sv, axis=AX.X, op=ALU.max),
                     reads=bpair, writes=[hb[h]])
                P.op("dve", lambda e: e.tensor_scalar(out=s8[:, 2, h:h + 1], in0=s8[:, 0, h:h + 1], scalar1=sk8[:, h:h + 1],
                                                      scalar2=-0.125, op0=ALU.max, op1=ALU.mult),
                     reads=[hb[h], bsk8], writes=[hb[h]])
                Pt, bPt = Ps.next()
                P.op("act", lambda e: e.activation(
                    out=Pt[:, 0:nk], in_=sv, func=AF.Exp, bias=s8[:, 2, h:h + 1], scale=0.125, accum_out=s8[:, 3, h:h + 1]),
                    reads=bpair + [hb[h]], writes=[bPt, hb[h]])
                return (h, kvj, Pt, bPt, nk)

            def st_tr(stt):
                h, kvj, Pt, bPt, nk = stt
                nch = nk // 128
                ptb = 4 + (h % 2)
                ptv = ps_bf(ptb)[:, 0:640].rearrange("p (n q) -> p n q", n=5)

                def trp(e):
                    for n_ in range(nch):
                        ins = e.transpose(out=ptv[:, n_, :], in_=Pt[:, n_ * 128:(n_ + 1) * 128], identity=ident)
                    return ins
                P.op("pe", trp, reads=[bPt, b_ident], writes=[bps[ptb]])
                PT_, bPT = PTs.next()
                if h % 2 == 0:
                    P.op("act", lambda e: e.copy(out=PT_[:, 0:nch, :], in_=ptv[:, 0:nch, :]), reads=[bps[ptb]], writes=[bPT])
                else:
                    P.op("dve", lambda e: e.tensor_copy(out=PT_[:, 0:nch, :], in_=ptv[:, 0:nch, :]), reads=[bps[ptb]], writes=[bPT])
                return (h, kvj, PT_, bPT)

            def st_pv(stt):
                h, kvj, PT_, bPT = stt
                vt = wb + [32, 33]

                def mmpv(e):
                    for n_, tl in enumerate(vt):
                        ins = e.matmul(pO[:, h, :], lhsT=PT_[:, n_, :], rhs=Vt[:, tl, kvj * 64:(kvj + 1) * 64],
                                       start=(n_ == 0), stop=(n_ == len(vt) - 1))
                    return ins
                P.op("pe", mmpv, reads=[bPT, bVt], writes=[bps[6]])

            q_st = {0: st_qk(0)}
            t_st = {}
            for h in range(8):
                if h + 1 < 8:
                    q_st[h + 1] = st_qk(h + 1)
                t_st[h] = st_tr(q_st[h])
                if h >= 1:
                    st_pv(t_st[h - 1])
            st_pv(t_st[7])
            bs8 = st8b[slot8]
            P.op("dve", lambda e, s8=s8: e.scalar_tensor_tensor(out=s8[:, 4, :], in0=s8[:, 2, :], scalar=8.0, in1=sk8,
                                                                op0=ALU.mult, op1=ALU.add),
                 reads=[bs8, bsk8] + hb, writes=[bs8])
            P.op("act", lambda e, s8=s8: e.activation(out=s8[:, 4, :], in_=s8[:, 4, :], func=AF.Exp, scale=0.125),
                 reads=[bs8], writes=[bs8])
            P.op("dve", lambda e, s8=s8: e.tensor_tensor(out=s8[:, 5, :], in0=s8[:, 4, :], in1=s8[:, 3, :], op=ALU.add),
                 reads=[bs8] + hb, writes=[bs8])
            P.op("dve", lambda e, s8=s8: e.reciprocal(out=s8[:, 5, :], in_=s8[:, 5, :]), reads=[bs8], writes=[bs8])
            P.op("dve", lambda e, s8=s8, osb=osb: e.tensor_tensor(out=osb, in0=pO, in1=s8[:, 5, :].unsqueeze(2).to_broadcast([128, 8, 64]),
                                                                  op=ALU.mult), reads=[bps[6], bs8], writes=[bosb])
            otv = ps_bf(7)[:, 0:512].rearrange("p (n q) -> p n q", n=4)
            of = osb.rearrange("p h d -> p (h d)")

            def post():
                def tro(e):
                    for n_ in range(4):
                        ins = e.transpose(out=otv[:, n_, :], in_=of[:, n_ * 128:(n_ + 1) * 128], identity=ident)
                    return ins
                P.op("pe", tro, reads=[bosb, b_ident], writes=[bps[7]])
                P.op("act", lambda e: e.copy(out=yT[:, 4:8, cs], in_=otv), reads=[bps[7]], writes=[byT])
            return post

        prev_post = [None]
        prev_yT = None

        tail = [None]
        tail_f = {}

        def do_tail(g, yT, byT):
            convert_some(3)
            fT4, bfT4 = fT4s.next()
            cwt, bcw = cws.next()
            return mixer_tail(1, g, 4, yT, byT, Wout, bWout, gate1, GS2, hts, tmps, fbf, fT4, bfT4,
                              lambda i: (H1[i * 128:(i + 1) * 128, :], [bH1]), 0, 2, 3, cwt, bcw, rsc, brsc,
                              dbg.get("d_hm1"), deferred=True)

        for g in range(8):
            yT, byT = yTs.next()
            for j in range(4):
                i = g * 4 + j
                cs = slice(j * 128, (j + 1) * 128)
                srcs = []
                if i > 0:
                    srcs.append((i - 1, 0))
                srcs.append((i, 3 if i == 0 else (4 if i == 31 else 1)))
                if i < 31:
                    srcs.append((i + 1, 2))
                pv4 = psb[7].rearrange("p (g t) -> p g t", g=4)

                def mmpool(e, srcs=srcs):
                    for gi in range(4):
                        for n_, (s_, kind) in enumerate(srcs):
                            ins = e.matmul(pv4[:, gi, :], lhsT=Up[:, s_, gi * 128:(gi + 1) * 128], rhs=pB[:, gi * 5 + kind, :],
                                           start=(n_ == 0), stop=(n_ == len(srcs) - 1))
                    return ins
                P.op("pe", mmpool, reads=[bUp, bpB], writes=[bps[7]])
                pT_, bpT = pTs.next()
                P.op("act", lambda e, pT_=pT_: e.copy(out=pT_, in_=pv4), reads=[bps[7]], writes=[bpT])

                def mmpw(e, pT_=pT_):
                    for gi in range(4):
                        ins = e.matmul(pv4[:, gi, :], lhsT=PW[:, gi, :], rhs=pT_[:, gi, :], start=True, stop=True)
                    return ins
                P.op("pe", mmpw, reads=[bpT, bPW], writes=[bps[7]])
                P.op("dve", lambda e, yT=yT, cs=cs: e.tensor_tensor(out=yT[:, 0:4, cs], in0=pv4,
                                                                   in1=psc.unsqueeze(2).to_broadcast([128, 4, 128]), op=ALU.mult),
                     reads=[bps[7], bpsc], writes=[byT])
                if prev_post[0] is not None:
                    prev_post[0]()
                    prev_post[0] = None
                if g > 0:
                    if j == 0:
                        if tail[0] is not None:
                            tA, tB, tF = tail[0]
                            tB(3, *tail_f[3])
                            tF()
                        tail[0] = do_tail(g - 1, prev_yT[0], prev_yT[1])
                    tA, tB, tF = tail[0]
                    tail_f[j] = tA(j)
                    if j > 0:
                        tB(j - 1, *tail_f[j - 1])
                prev_post[0] = attn_block(i, cs, yT, byT)
            prev_yT = (yT, byT)
        prev_post[0]()
        if tail[0] is not None:
            tA, tB, tF = tail[0]
            tB(3, *tail_f[3])
            tF()
        tA, tB, tF = do_tail(7, prev_yT[0], prev_yT[1])
        pend = tA(0)
        for j in range(4):
            nxt = tA(j + 1) if j + 1 < 4 else None
            tB(j, *pend)
            pend = nxt
        tF()


    def moe_sparse(l, ntok, Hout, bHout, final):
        convert_upto(48 * (l + 1))
        new_phase()
        NTl = ntok // 128
        NTS = (2 * ntok) // 256 + NE - 1
        nsl = NTS * 256
        cwa = T([128, NTl, NE], F32)
        mka = T([128, NTl, NE], F32)
        mkb = T([128, NTl, NE], BF16)
        sp = T([128, NTl, NE], F32)
        v1 = T([128, NTl, NE], F32)
        v2 = T([128, NTl, NE], F32)
        tm = T([128, NTl, NE], F32)
        bR = Buf()
        Lt = T([128, 128], BF16)
        ones = T([128, 128], BF16)
        sm = T([128, 16, NE], F32)
        smi = T([128, NE], I32)
        slo = T([128, NTl], F32)
        shi = T([128, NTl], F32)
        clo = T([128, NTl], F32)
        chi = T([128, NTl], F32)
        sloi = sloi_t[:, 0:NTl]
        shii = shii_t[:, 0:NTl]
        tau = T([128, 64], F32)
        pix = T([128, 1], F32)
        cmpt = T([128, NTS, NE], F32)
        ecn = T([128, NTS], F32)
        widx = widx_t[:, 0:NTS]
        P.dma("sync", cwa, CW[0:ntok, :].rearrange("(n p) e -> p n e", p=128), reads=[bCW], writes=[bR])
        P.dma("sync", tau, tauc, writes=[bR])
        P.dma("sync", pix, pidx, writes=[bR])
        P.op("pool", lambda e: e.memset(ones, 1.0), writes=[bR])
        P.op("pool", lambda e: e.memset(Lt, 1.0), writes=[bR])
        P.op("pool", lambda e: e.affine_select(out=Lt, in_=Lt, pattern=[[1, 128]], compare_op=ALU.is_gt, fill=0.0,
                                               base=0, channel_multiplier=-1), reads=[bR], writes=[bR])
        P.op("dve", lambda e: e.tensor_scalar(out=mka, in0=cwa, scalar1=0.0, scalar2=None, op0=ALU.is_gt), reads=[bR], writes=[bR])
        P.op("dve", lambda e: e.tensor_copy(out=mkb, in_=mka), reads=[bR], writes=[bR])
        half = (NTl + 1) // 2

        def mmpos(e):
            for i in range(NTl):
                pbk, col = (0, i) if i < half else (1, i - half)
                o_ = psb[pbk][:, col * 16:(col + 1) * 16]
                for i2 in range(i):
                    e.matmul(o_, lhsT=ones, rhs=mkb[:, i2, :], start=(i2 == 0), stop=False)
                e.matmul(o_, lhsT=Lt, rhs=mkb[:, i, :], start=(i == 0), stop=True)
            for i in range(NTl):
                ins = e.matmul(psb[2][:, 0:16], lhsT=ones, rhs=mkb[:, i, :], start=(i == 0), stop=(i == NTl - 1))
            return ins
        P.op("pe", mmpos, reads=[bR], writes=[bps[0], bps[1], bps[2]])
        rd = [bR, bps[0], bps[1], bps[2]]
        P.op("dve", lambda e: e.tensor_scalar(out=sm[:, 0, :], in0=psb[2][:, 0:16], scalar1=255.0, scalar2=None, op0=ALU.add),
             reads=rd, writes=[bR])
        P.op("dve", lambda e: e.tensor_copy(out=smi, in_=sm[:, 0, :]), reads=[bR], writes=[bR])
        P.op("dve", lambda e: e.tensor_scalar(out=smi, in0=smi, scalar1=8, scalar2=8, op0=ALU.arith_shift_right,
                                              op1=ALU.logical_shift_left), reads=[bR], writes=[bR])
        P.op("dve", lambda e: e.tensor_copy(out=sm[:, 1, :], in_=smi), reads=[bR], writes=[bR])
        P.op("dve", lambda e: e.tensor_copy(out=sm[:, 2, :], in_=sm[:, 1, :]), reads=[bR], writes=[bR])
        cur, nxt = 2, 3
        for d_ in (1, 2, 4, 8):
            P.op("dve", lambda e, cur=cur, nxt=nxt, d_=d_: e.tensor_tensor(
                out=sm[:, nxt, d_:16], in0=sm[:, cur, d_:16], in1=sm[:, cur, 0:16 - d_], op=ALU.add), reads=[bR], writes=[bR])
            P.op("dve", lambda e, cur=cur, nxt=nxt, d_=d_: e.tensor_copy(out=sm[:, nxt, 0:d_], in_=sm[:, cur, 0:d_]),
                 reads=[bR], writes=[bR])
            cur, nxt = nxt, cur
        P.op("dve", lambda e, cur=cur: e.tensor_tensor(out=sm[:, 4, :], in0=sm[:, cur, :], in1=sm[:, 1, :], op=ALU.subtract),
             reads=[bR], writes=[bR])
        start = sm[:, 4, :]
        P.op("dve", lambda e: e.tensor_tensor(out=sp[:, 0:half, :], in0=psb[0][:, 0:half * 16].rearrange("p (n e) -> p n e", e=16),
                                              in1=start.unsqueeze(1).to_broadcast([128, half, NE]), op=ALU.add), reads=rd, writes=[bR])
        P.op("dve", lambda e: e.tensor_tensor(out=sp[:, half:NTl, :],
                                              in0=psb[1][:, 0:(NTl - half) * 16].rearrange("p (n e) -> p n e", e=16),
                                              in1=start.unsqueeze(1).to_broadcast([128, NTl - half, NE]), op=ALU.add), reads=rd, writes=[bR])
        P.op("dve", lambda e: e.tensor_scalar(out=tm, in0=mka, scalar1=-1.0e6, scalar2=1.0e6, op0=ALU.mult, op1=ALU.add),
             reads=[bR], writes=[bR])
        P.op("dve", lambda e: e.tensor_tensor(out=sp, in0=sp, in1=mka, op=ALU.mult), reads=[bR], writes=[bR])
        P.op("dve", lambda e: e.tensor_tensor(out=v1, in0=sp, in1=tm, op=ALU.add), reads=[bR], writes=[bR])
        P.op("dve", lambda e: e.tensor_tensor(out=v2, in0=sp, in1=tm, op=ALU.subtract), reads=[bR], writes=[bR])
        P.op("dve", lambda e: e.tensor_reduce(out=slo, in_=v1, axis=AX.X, op=ALU.min), reads=[bR], writes=[bR])
        P.op("dve", lambda e: e.tensor_reduce(out=shi, in_=v2, axis=AX.X, op=ALU.max), reads=[bR], writes=[bR])
        P.op("dve", lambda e: e.tensor_tensor(out=v1, in0=v1, in1=slo.unsqueeze(2).to_broadcast([128, NTl, NE]), op=ALU.is_equal),
             reads=[bR], writes=[bR])
        P.op("dve", lambda e: e.tensor_tensor(out=v1, in0=v1, in1=cwa, op=ALU.mult), reads=[bR], writes=[bR])
        P.op("dve", lambda e: e.tensor_reduce(out=clo, in_=v1, axis=AX.X, op=ALU.add), reads=[bR], writes=[bR])
        P.op("dve", lambda e: e.tensor_tensor(out=v2, in0=v2, in1=shi.unsqueeze(2).to_broadcast([128, NTl, NE]), op=ALU.is_equal),
             reads=[bR], writes=[bR])
        P.op("dve", lambda e: e.tensor_tensor(out=v2, in0=v2, in1=cwa, op=ALU.mult), reads=[bR], writes=[bR])
        P.op("dve", lambda e: e.tensor_reduce(out=chi, in_=v2, axis=AX.X, op=ALU.add), reads=[bR], writes=[bR])
        P.op("dve", lambda e: e.tensor_copy(out=sloi, in_=slo), reads=[bR], writes=[bR])
        P.op("dve", lambda e: e.tensor_copy(out=shii, in_=shi), reads=[bR], writes=[bR])
        P.op("dve", lambda e: e.tensor_tensor(out=cmpt, in0=start.unsqueeze(1).to_broadcast([128, NTS, NE]),
                                              in1=tau[:, 0:NTS].unsqueeze(2).to_broadcast([128, NTS, NE]), op=ALU.is_le),
             reads=[bR], writes=[bR])
        P.op("dve", lambda e: e.tensor_reduce(out=ecn, in_=cmpt, axis=AX.X, op=ALU.add), reads=[bR], writes=[bR])
        P.op("dve", lambda e: e.tensor_scalar(out=ecn, in0=ecn, scalar1=128.0, scalar2=-128.0, op0=ALU.mult, op1=ALU.add),
             reads=[bR], writes=[bR])
        P.op("dve", lambda e: e.tensor_scalar(out=ecn, in0=ecn, scalar1=pix[:, 0:1], scalar2=None, op0=ALU.add), reads=[bR], writes=[bR])
        P.op("dve", lambda e: e.tensor_copy(out=widx, in_=ecn), reads=[bR], writes=[bR])
        if l == 0:
            ada_layer(1)
        fts = Rot(3, [128, D], BF16)
        for i in range(NTl):
            ft, bft = fts.next()
            P.dma("sync", ft, Fd[i * 128:(i + 1) * 128, :], writes=[bft])
            for idx in (sloi, shii):
                P.op("pool", lambda e, ft=ft, idx=idx, i=i: e.indirect_dma_start(
                    out=Xs[:, :], out_offset=bass.IndirectOffsetOnAxis(ap=idx[:, i:i + 1], axis=0),
                    in_=ft, in_offset=None, bounds_check=breg(e, nsl - 1), oob_is_err=False),
                    reads=[bft, bR], writes=[Buf()], dma=True)
        P.barrier()
        mark_e = st["off"]
        xss = Rot(2, [128, 2, D], BF16)
        xTs = Rot(2, [128, 8, 256], BF16)
        nwb = 3
        wgus = Rot(nwb, [128, 8, 1024], BF16)
        wds = Rot(nwb, [128, 4, 1024], BF16)
        hids = Rot(2, [128, 4, 256], BF16)
        sgs_ = Rot(2, [128, 256], F32)
        yss = Rot(2, [128, 2, D], F32)
        for tq in range(NTS):
            xs_, bxs = xss.next()
            P.dma("sync", xs_, Xs[tq * 256:(tq + 1) * 256, :].rearrange("(j p) d -> p j d", p=128), writes=[bxs])
            wgu, bwgu = wgus.next()
            wd, bwd = wds.next()
            P.op("pool", lambda e, wgu=wgu, tq=tq: e.indirect_dma_start(
                out=wgu.rearrange("p k n -> p (k n)"), out_offset=None, in_=WGU[l][:, :],
                in_offset=bass.IndirectOffsetOnAxis(ap=widx[:, tq:tq + 1], axis=0), bounds_check=breg(e, NE * 128 - 1), oob_is_err=False),
                reads=[bR], writes=[bwgu], dma=True)
            P.op("pool", lambda e, wd=wd, tq=tq: e.indirect_dma_start(
                out=wd.rearrange("p k n -> p (k n)"), out_offset=None, in_=WD[l][:, :],
                in_offset=bass.IndirectOffsetOnAxis(ap=widx[:, tq:tq + 1], axis=0), bounds_check=breg(e, NE * 128 - 1), oob_is_err=False),
                reads=[bR], writes=[bwd], dma=True)
            xT, bxT = xTs.next()
            for j in range(2):
                transpose8(xs_[:, j, :], bxs, j, xT, bxT, j * 128)
            hd, bhd = hids.next()
            for jc in range(4):
                pg, pu = (2, 3) if jc % 2 == 0 else (4, 5)

                def mmg(e, jc=jc, pg=pg, pu=pu, wgu=wgu, xT=xT):
                    for (pb_, off) in ((pg, 0), (pu, 512)):
                        for k in range(8):
                            ins = e.matmul(psb[pb_][:, 0:256], lhsT=wgu[:, k, off + jc * 128: off + (jc + 1) * 128],
                                           rhs=xT[:, k, :], start=(k == 0), stop=(k == 7))
                    return ins
                P.op("pe", mmg, reads=[bwgu, bxT], writes=[bps[pg], bps[pu]])
                sg, bsg = sgs_.next()
                P.op("act", lambda e, sg=sg, pg=pg: e.activation(out=sg, in_=psb[pg][:, 0:256], func=AF.Silu),
                     reads=[bps[pg]], writes=[bsg])
                P.op("dve", lambda e, sg=sg, pu=pu, hd=hd, jc=jc: e.tensor_tensor(out=hd[:, jc, :], in0=psb[pu][:, 0:256], in1=sg, op=ALU.mult),
                     reads=[bps[pu], bsg], writes=[bhd])
            ys_, bys = yss.next()
            for j in range(2):
                for hf in range(2):
                    pd = 6 + ((j * 2 + hf) % 2)

                    def mmd(e, pd=pd, hd=hd, j=j, hf=hf, wd=wd):
                        for jc in range(4):
                            ins = e.matmul(psb[pd], lhsT=hd[:, jc, j * 128:(j + 1) * 128],
                                           rhs=wd[:, jc, hf * 512:(hf + 1) * 512], start=(jc == 0), stop=(jc == 3))
                        return ins
                    P.op("pe", mmd, reads=[bhd, bwd], writes=[bps[pd]])
                    if hf == 0:
                        P.op("act", lambda e, ys_=ys_, j=j, hf=hf, pd=pd: e.copy(out=ys_[:, j, hf * 512:(hf + 1) * 512], in_=psb[pd]),
                             reads=[bps[pd]], writes=[bys])
                    else:
                        P.op("dve", lambda e, ys_=ys_, j=j, hf=hf, pd=pd: e.tensor_copy(out=ys_[:, j, hf * 512:(hf + 1) * 512], in_=psb[pd]),
                             reads=[bps[pd]], writes=[bys])
            P.dma("act", Ys[tq * 256:(tq + 1) * 256, :].rearrange("(j p) d -> p j d", p=128), ys_, reads=[bys], writes=[Buf()])
        P.barrier()
        st["off"] = mark_e
        g2 = {}
        for row in ((0, 1) if ntok > SEQ else (0,)):
            gt = T([128, D], F32)
            bg = Buf()
            load_mod(l, row, 5, gt, bg)
            g2[row] = (gt, bg)
        if final:
            fg = T([128, D], F32)
            bfg = Buf()
            P.dma("sync", fg, final_g.partition_broadcast(128), writes=[bfg])
        ylos = Rot(3, [128, D], F32)
        yhis = Rot(3, [128, D], F32)
        hts = Rot(3, [128, D], F32)
        tmps = Rot(2, [128, D], F32)
        outs = []

        def cb_load(i):
            ylo, bylo = ylos.next()
            yhi, byhi = yhis.next()
            for (yt_, byt, idx) in ((ylo, bylo, sloi), (yhi, byhi, shii)):
                P.op("pool", lambda e, yt_=yt_, idx=idx: e.indirect_dma_start(
                    out=yt_, out_offset=None, in_=Ys[:, :], in_offset=bass.IndirectOffsetOnAxis(ap=idx[:, i:i + 1], axis=0),
                    bounds_check=breg(e, nsl - 1), oob_is_err=False), reads=[bR], writes=[byt], dma=True)
            ht, bh = hts.next()
            P.dma("sync", ht, Hm[i * 128:(i + 1) * 128, :], reads=[bHm], writes=[bh])
            return ylo, bylo, yhi, byhi, ht, bh

        def cb_comp(i, ylo, bylo, yhi, byhi, ht, bh):
            row = 0 if i < 32 else 1
            gt, bg = g2[row]
            P.op("act", lambda e: e.activation(out=ylo, in_=ylo, func=AF.Copy, scale=clo[:, i:i + 1]),
                 reads=[bylo, bR], writes=[bylo])
            P.op("dve", lambda e: e.scalar_tensor_tensor(out=ylo, in0=yhi, scalar=chi[:, i:i + 1], in1=ylo,
                                                         op0=ALU.mult, op1=ALU.add), reads=[bylo, byhi, bR], writes=[bylo])
            P.op("dve", lambda e: e.tensor_tensor(out=ylo, in0=ylo, in1=gt, op=ALU.mult), reads=[bylo, bg], writes=[bylo])
            P.op("dve", lambda e: e.tensor_tensor(out=ht, in0=ylo, in1=ht, op=ALU.add),
                 reads=[bylo, bh], writes=[bh])
            if not final:
                P.dma("act", Hout[i * 128:(i + 1) * 128, :], ht, reads=[bh], writes=[bHout])
                if debug:
                    dbg_ops.append(P.dma("act", dbg["d_h1"][i * 128:(i + 1) * 128, :], ht, reads=[bh]))
            else:
                tmp, btmp = tmps.next()
                rs, bs = norm_mod(ht, bh, None, None, None, None, tmp, btmp, None, None)
                P.op("dve", lambda e: e.scalar_tensor_tensor(
                    out=tmp, in0=ht, scalar=rs, in1=fg, op0=ALU.mult, op1=ALU.mult), reads=[bh, bs, bfg], writes=[btmp])
                outs.append(P.dma("act", out[i * 128:(i + 1) * 128, :], tmp, reads=[btmp]))

        pend = [cb_load(0), cb_load(1)]
        for i in range(NTl):
            if i + 2 < NTl:
                pend.append(cb_load(i + 2))
            cb_comp(i, *pend.pop(0))
        return outs

    Vcs, bV, keep = layer0_A()
    layer0_B(Vcs, bV, keep)
    if debug:
        new_phase()
        cwd = T([128, NT, NE], F32)
        bb = Buf()
        P.dma("sync", cwd, CW.rearrange("(n p) e -> p n e", p=128), reads=[bCW], writes=[bb])
        dbg_ops.append(P.dma("sync", dbg["d_cw0"].rearrange("(n p) e -> p n e", p=128), cwd, reads=[bb]))
    finals = []
    if stop_after != "mix0":
        moe_sparse(0, NTOK, H1, bH1, False)
    if stop_after not in ("mix0", "moe0"):
        pk, keep1 = layer1_A()
        layer1_B(pk, keep1)
        if debug:
            new_phase()
            cwd = T([128, 32, NE], F32)
            bb = Buf()
            P.dma("sync", cwd, CW[0:SEQ, :].rearrange("(n p) e -> p n e", p=128), reads=[bCW], writes=[bb])
            dbg_ops.append(P.dma("sync", dbg["d_cw1"][0:SEQ, :].rearrange("(n p) e -> p n e", p=128), cwd, reads=[bb]))
        if stop_after != "mix1":
            finals = moe_sparse(1, SEQ, None, None, True)
    P.finalize(finals + dbg_ops)
    return nc


_NC = {}


def kernel(**inputs):
    inp = {k: np.asarray(v) for k, v in inputs.items()}
    c = _consts()
    if "nc" not in _NC:
        _NC["nc"] = build(debug=False)
    nc = _NC["nc"]
    shared = {}
    for k in ("ada_w", "ada_b", "norm_mix_g", "norm_ffn_g", "router_w", "router_b",
              "moe_w_gate", "moe_w_up", "moe_w_down", "final_g"):
        shared[k] = np.ascontiguousarray(inp[k], dtype=np.float32)
    for k in ("even_w_in", "even_conv_w", "even_w_out", "odd_w_in", "odd_pool_w", "odd_pool_scale",
              "odd_sink", "odd_w_out"):
        shared[k] = np.ascontiguousarray(inp[k][0], dtype=np.float32)
    for k in ("dftN", "dftC", "dftD", "rope", "poolB", "amask", "tauc", "pidx"):
        shared[k] = c[k]
    nb = inp["x"].shape[0]
    in_maps = []
    for b in range(nb):
        m = dict(shared)
        m["x"] = np.ascontiguousarray(inp["x"][b], dtype=np.float32)
        m["ctx"] = np.ascontiguousarray(inp["ctx"][b], dtype=np.float32)
        m["c2"] = np.ascontiguousarray(np.stack([inp["c"][b], inp["c_ctx"]], 0), dtype=np.float32)
        in_maps.append(m)
    res = run_bass_kernel_spmd(nc, in_maps, core_ids=list(range(nb)))
    return np.stack([np.asarray(r["out"], dtype=np.float32) for r in res.results], 0)
```

```python
import numpy as np
import ml_dtypes
import concourse.bass as bass
import concourse.mybir as mybir
from concourse.bass_utils import run_bass_kernel_spmd

F32 = mybir.dt.float32
BF16 = mybir.dt.bfloat16
AF = mybir.ActivationFunctionType
ALU = mybir.AluOpType
AX = mybir.AxisListType

D = 1024
SEQ = 4096
LCTX = 256
NTOK = SEQ + LCTX
NT = NTOK // 128
NE = 16
EPS = 1e-6
BIG = 1.0e4


class Buf:
    __slots__ = ("name", "last_w", "readers")

    def __init__(self, name=""):
        self.name = name
        self.last_w = None
        self.readers = []


class Op:
    __slots__ = ("eng", "emit", "deps", "is_dma", "signal", "sem", "val")

    def __init__(self, eng, emit, is_dma):
        self.eng = eng
        self.emit = emit
        self.is_dma = is_dma
        self.deps = []
        self.signal = False
        self.sem = None
        self.val = None


ENGS = ("sync", "act", "dve", "pool", "pe")
NDMA_SEMS = {"sync": 16, "act": 8, "pool": 32}


class Prog:
    def __init__(self, nc):
        self.nc = nc
        self.ops = {e: [] for e in ENGS}
        self.ctx = []
        self.last_compute = {}
        self.dma_since = []
        self.pending = {}

    def enter(self, cm):
        v = cm.__enter__()
        self.ctx.append(cm)
        return v

    def sbuf(self, name, shape, dt):
        return self.enter(self.nc.sbuf_tensor(name, list(shape), dt))

    def psum(self, name, shape, dt):
        return self.enter(self.nc.psum_tensor(name, list(shape), dt))

    def close(self):
        for cm in reversed(self.ctx):
            cm.__exit__(None, None, None)
        self.ctx = []

    def op(self, eng, emit, reads=(), writes=(), dma=False):
        o = Op(eng, emit, dma)
        deps = {}
        for b in reads:
            if b.last_w is not None:
                deps[id(b.last_w)] = (b.last_w, True)
        for b in writes:
            if b.last_w is not None and id(b.last_w) not in deps:
                deps[id(b.last_w)] = (b.last_w, False)
            for r in b.readers:
                if id(r) not in deps:
                    deps[id(r)] = (r, False)
        if eng in self.pending:
            for d in self.pending.pop(eng):
                if id(d) not in deps and not (d.eng == eng and not d.is_dma):
                    deps[id(d)] = (d, True)
        for d, raw in deps.values():
            if d is o:
                continue
            if d.eng == o.eng and not d.is_dma:
                if raw and o.eng != "pe" and not o.is_dma:
                    o.deps.append(d)
                    d.signal = True
                elif o.is_dma:
                    o.deps.append(d)
                    d.signal = True
                continue
            o.deps.append(d)
            d.signal = True
        for b in reads:
            if not dma:
                b.readers = [r for r in b.readers if r.is_dma or r.eng != eng]
            b.readers.append(o)
        for b in writes:
            b.last_w = o
            b.readers = []
        self.ops[eng].append(o)
        if dma:
            self.dma_since.append(o)
        else:
            self.last_compute[eng] = o
        return o

    def barrier(self):
        pend = list(self.last_compute.values()) + list(self.dma_since)
        self.dma_since = []
        for e in ENGS:
            self.pending[e] = list(self.pending.get(e, [])) + pend

    def dma(self, eng, out, in_, reads=(), writes=(), **kw):
        return self.op(eng, lambda e: e.dma_start(out=out, in_=in_, **kw), reads, writes, dma=True)

    def finalize(self, final_wait_ops=()):
        nc = self.nc
        for o in final_wait_ops:
            o.signal = True
        eng_sem = {e: self.enter(nc.semaphore("c_" + e)) for e in ("act", "dve", "pool", "pe")}
        dma_sems = {e: [self.enter(nc.semaphore(f"d_{e}{i}")) for i in range(n)]
                    for e, n in NDMA_SEMS.items()}
        for e in ENGS:
            cnt = 0
            dcnt = 0
            per_sem_val = {}
            prev_on_sem = {}
            for o in self.ops[e]:
                if o.is_dma:
                    pool = dma_sems[e]
                    k = dcnt % len(pool)
                    dcnt += 1
                    o.sem = pool[k]
                    per_sem_val[k] = per_sem_val.get(k, 0) + 16
                    o.val = per_sem_val[k]
                    if k in prev_on_sem:
                        o.deps.append(prev_on_sem[k])
                    prev_on_sem[k] = o
                    o.signal = True
                elif o.signal:
                    cnt += 1
                    o.sem = eng_sem[e]
                    o.val = cnt
        block = self.enter(nc.Block())
        handles = {"sync": block.sync, "act": block.scalar, "dve": block.vector,
                   "pool": block.gpsimd, "pe": block.tensor}

        def make(e):
            ops = self.ops[e]

            def body(eng):
                waited = {}
                for o in ops:
                    need = {}
                    for d in o.deps:
                        key = id(d.sem)
                        if waited.get(key, 0) >= d.val:
                            continue
                        if key not in need or need[key][1] < d.val:
                            need[key] = (d.sem, d.val)
                    for key, (s, v) in need.items():
                        eng.wait_ge(s, v)
                        waited[key] = v
                    ins = o.emit(eng)
                    if o.signal:
                        ins.then_inc(o.sem, 16 if o.is_dma else 1)
                if e == "sync":
                    for o in final_wait_ops:
                        eng.wait_ge(o.sem, o.val)
            return body

        for e in ENGS:
            if self.ops[e] or e == "sync":
                handles[e](make(e))
        self.close()


_CONST = {}


def _consts():
    if _CONST:
        return _CONST
    bf = ml_dtypes.bfloat16
    N = SEQ
    s = np.arange(N, dtype=np.int64)
    prod = (s[:, None] * s[None, :]) % N
    ang = prod.astype(np.float64) * (2 * np.pi / N)
    cosm = (np.cos(ang) / 64.0).astype(np.float32)
    nsin = (-np.sin(ang) / 64.0).astype(np.float32)
    both = np.stack([cosm, nsin], 0).reshape(2, 32, 128, 8, 512)
    dftN = np.ascontiguousarray(both.transpose(3, 2, 0, 1, 4)).reshape(8, 128, 64, 512)
    _CONST["dftN"] = dftN.astype(bf)
    del prod, ang, cosm, nsin, both, dftN
    sc = np.arange(LCTX)
    angc = ((sc[:, None] * sc[None, :]) % LCTX) * (2 * np.pi / LCTX)
    cb = np.stack([np.cos(angc) / 16.0, -np.sin(angc) / 16.0], 0).reshape(2, 2, 128, 256)
    _CONST["dftC"] = np.ascontiguousarray(cb.transpose(2, 0, 1, 3)).reshape(128, 4, 256).astype(bf)
    j = np.arange(128)
    angd = ((j[:, None] * j[None, :]) % 128) * (2 * np.pi / 128)
    _CONST["dftD"] = np.concatenate([np.cos(angd), np.sin(angd)], 1).astype(np.float32) / np.sqrt(128.0)
    _CONST["dftD"] = _CONST["dftD"].astype(bf)
    quarter = 16
    inv = 10000.0 ** (-np.arange(quarter, dtype=np.float32) / quarter)
    t = np.arange(N)
    pos = np.stack([t // 64, t % 64], 0).astype(np.float32)
    p = np.arange(128)
    d = p % 64
    axis = d // 32
    i = d % 16
    angr = pos[axis, :] * inv[i][:, None]
    _CONST["rope"] = np.stack([np.cos(angr), np.sin(angr)], 1).astype(np.float32)
    half = (d % 32) // 16
    _CONST["rot_src"] = np.where(half == 0, d + 16, d - 16)[:64]
    _CONST["rot_sign"] = np.where(half == 0, -1.0, 1.0)[:64].astype(np.float32)
    pb = np.zeros((128, 20, 128), np.float32)
    for gi, w in enumerate((2, 4, 8, 16)):
        r = w // 2
        for kind in range(5):
            if kind == 0:
                tt, ss = np.arange(128, 256), np.arange(0, 128)
                base_t = 1280
            elif kind == 1:
                tt, ss = np.arange(128, 256), np.arange(128, 256)
                base_t = 1280
            elif kind == 2:
                tt, ss = np.arange(128, 256), np.arange(256, 384)
                base_t = 1280
            elif kind == 3:
                tt, ss = np.arange(0, 128), np.arange(0, 128)
                base_t = 0
            else:
                tt, ss = np.arange(N - 128, N), np.arange(N - 128, N)
                base_t = 0
            if kind < 3:
                tt = tt + base_t
                ss = ss + base_t
            lo = np.clip(tt - r, 0, N)
            hi = np.clip(tt + r + 1, 0, N)
            cnt = (hi - lo).astype(np.float32)
            m = ((ss[:, None] >= lo[None, :]) & (ss[:, None] < hi[None, :])).astype(np.float32) / cnt[None, :]
            m = m - (ss[:, None] == tt[None, :]).astype(np.float32)
            pb[:, gi * 5 + kind, :] = m
    _CONST["poolB"] = pb.astype(bf)
    q = np.arange(128)
    am = np.zeros((128, 2, 128), np.float32)
    am[:, 0, :] = np.where(q[None, :] >= q[:, None], 0.0, -30000.0)
    am[:, 1, :] = np.where(q[None, :] <= q[:, None], 0.0, -30000.0)
    _CONST["amask"] = am
    _CONST["tauc"] = np.tile((np.arange(64, dtype=np.float32) * 256.0)[None, :], (128, 1))
    _CONST["pidx"] = np.arange(128, dtype=np.float32).reshape(128, 1)
    return _CONST


def build(debug=False, stop_after=None):
    nc = bass.Bass("TRN2", target_bir_lowering=False)

    def din(name, shape, dt=F32):
        return nc.dram_tensor(name, list(shape), dt, kind="ExternalInput").ap()

    def dscr(name, shape, dt=F32):
        return nc.dram_tensor(name, list(shape), dt, kind="Internal").ap()

    x = din("x", [SEQ, D])
    ctx = din("ctx", [LCTX, D])
    c2 = din("c2", [2, D])
    ada_w = din("ada_w", [2, D, 6 * D])
    ada_b = din("ada_b", [2, 6 * D])
    norm_mix_g = din("norm_mix_g", [2, D])
    norm_ffn_g = din("norm_ffn_g", [2, D])
    even_w_in = din("even_w_in", [D, 2048])
    even_conv_w = din("even_conv_w", [3, 512])
    even_w_out = din("even_w_out", [D, D])
    odd_w_in = din("odd_w_in", [D, 1280])
    odd_pool_w = din("odd_pool_w", [4, 128, 128])
    odd_pool_scale = din("odd_pool_scale", [512])
    odd_sink = din("odd_sink", [8])
    odd_w_out = din("odd_w_out", [D, D])
    router_w = din("router_w", [D, NE])
    router_b = din("router_b", [NE])
    moe_w_gate = din("moe_w_gate", [2, NE, D, 512])
    moe_w_up = din("moe_w_up", [2, NE, D, 512])
    moe_w_down = din("moe_w_down", [2, NE, 512, D])
    final_g = din("final_g", [D])
    dftN = din("dftN", [8, 128, 64, 512], BF16)
    dftC = din("dftC", [128, 4, 256], BF16)
    dftD = din("dftD", [128, 256], BF16)
    rope = din("rope", [128, 2, SEQ])
    poolB = din("poolB", [128, 20, 128], BF16)
    amask = din("amask", [128, 2, 128])
    tauc = din("tauc", [128, 64])
    pidx = din("pidx", [128, 1])
    out = nc.dram_tensor("out", [SEQ, D], F32, kind="ExternalOutput").ap()

    Mscr = dscr("Mscr", [2, 2, 6 * D])
    Hm = dscr("Hm", [NTOK, D])
    H1 = dscr("H1", [NTOK, D])
    ZT = dscr("ZT", [4, 128, NTOK], BF16)
    GBT = dscr("GBT", [4, 128, NTOK], BF16)
    FT = dscr("FT", [8, 128, NTOK], BF16)
    CW = dscr("CW", [NTOK, NE])
    Fd = dscr("Fd", [NTOK, D], BF16)
    NSLOT = 50 * 256
    Xs = dscr("Xs", [NSLOT, D], BF16)
    Ys = dscr("Ys", [NSLOT, D])
    WGU = [dscr(f"WGU{l}", [NE * 128, 8 * 1024], BF16) for l in range(2)]
    WD = [dscr(f"WD{l}", [NE * 128, 4 * 1024], BF16) for l in range(2)]
    bFd = Buf()
    bWGU = [Buf(), Buf()]
    bWD = [Buf(), Buf()]
    I32 = mybir.dt.int32
    bM = [Buf(), Buf()]
    bHm, bH1, bZT, bGBT, bFT, bCW = Buf(), Buf(), Buf(), Buf(), Buf(), Buf()
    dbg = {}
    if debug:
        for nm, shp in (("d_hm0", [NTOK, D]), ("d_cw0", [NTOK, NE]), ("d_h1", [NTOK, D]),
                        ("d_hm1", [NTOK, D]), ("d_cw1", [NTOK, NE]), ("d_M", [2, 2, 6 * D])):
            dbg[nm] = nc.dram_tensor(nm, shp, F32, kind="ExternalOutput").ap()

    P = Prog(nc)
    ARENA_ELEMS = 101 * 1024
    arena = P.sbuf("arena", [128, ARENA_ELEMS], BF16)
    pers = P.sbuf("pers", [128, 2048], BF16)
    st = {"off": 0, "poff": 0}

    def _carve(base, off, shape, dt):
        n = int(np.prod(shape[1:]))
        esz = 2 if dt == BF16 else 4
        nb = n * esz
        ap = base[0:shape[0], off // 2:(off + nb) // 2]
        if dt != BF16:
            ap = ap.bitcast(dt)
        if len(shape) == 3:
            ap = ap.rearrange("p (a b) -> p a b", a=shape[1], b=shape[2])
        elif len(shape) == 4:
            ap = ap.rearrange("p (a b c) -> p a b c", a=shape[1], b=shape[2], c=shape[3])
        return ap, (nb + 31) // 32 * 32

    def T(shape, dt):
        ap, nb = _carve(arena, st["off"], shape, dt)
        st["off"] += nb
        assert st["off"] <= ARENA_ELEMS * 2, st["off"]
        return ap

    def TP(shape, dt):
        ap, nb = _carve(pers, st["poff"], shape, dt)
        st["poff"] += nb
        assert st["poff"] <= 4096, st["poff"]
        return ap

    def new_phase():
        P.barrier()
        st["off"] = 0

    class Rot:
        def __init__(self, n, shape, dt):
            self.t = [T(shape, dt) for _ in range(n)]
            self.b = [Buf() for _ in range(n)]
            self.i = 0

        def next(self):
            k = self.i % len(self.t)
            self.i += 1
            return self.t[k], self.b[k]

    pairs = [P.psum(f"pp{i}", [128, 1024], F32) for i in range(4)]
    sloi_t = P.sbuf("sloi_t", [128, NT], mybir.dt.int32)
    shii_t = P.sbuf("shii_t", [128, NT], mybir.dt.int32)
    widx_t = P.sbuf("widx_t", [128, 64], mybir.dt.int32)
    psb = [pairs[i // 2][:, (i % 2) * 512:(i % 2 + 1) * 512] for i in range(8)]
    bps = [Buf() for _ in range(8)]

    def ps_bf(i):
        return psb[i].bitcast(BF16)

    ident = TP([128, 128], BF16)
    b_ident = Buf()
    epsT = TP([128, 1], F32)
    b_eps = Buf()
    P.op("pool", lambda e: e.memset(ident, 0.0), writes=[b_ident])
    P.op("pool", lambda e: e.affine_select(out=ident, in_=ident, pattern=[[-1, 128]],
                                           compare_op=ALU.not_equal, fill=1.0, base=0,
                                           channel_multiplier=1), reads=[b_ident], writes=[b_ident])
    P.op("pool", lambda e: e.memset(epsT, EPS), writes=[b_eps])
    Wr = TP([128, 8, NE], BF16)
    b_Wr = Buf()
    P.dma("pool", Wr, router_w.rearrange("(k p) n -> p k n", p=128), writes=[b_Wr])
    rbT = TP([128, NE], F32)
    b_rb = Buf()
    P.dma("sync", rbT, router_b.partition_broadcast(128), writes=[b_rb])
    stats = TP([128, 64], F32)
    stat_i = [0]

    def ada_layer(l):
        c2raw = T([128, 2, 8], F32)
        b_c2 = Buf()
        for r in range(2):
            P.dma("sync", c2raw[:, r, :], c2[r].rearrange("(p k) -> p k", k=8), writes=[b_c2])
        sT = T([128, 8, 2], F32)
        b_sT = Buf()
        P.op("act", lambda e: e.activation(out=sT.rearrange("p k r -> p r k"), in_=c2raw, func=AF.Silu),
             reads=[b_c2], writes=[b_sT])
        CW_ = 256
        wA = Rot(2, [128, 8, CW_], F32)
        adabs = Rot(2, [2, CW_], F32)
        msbs = Rot(2, [2, CW_], F32)
        awv = ada_w[l].rearrange("(p k) n -> p k n", k=8)
        for j in range(6 * D // CW_):
            cs_ = slice(j * CW_, (j + 1) * CW_)
            wt, bw = wA.next()
            P.dma("sync", wt, awv[:, :, cs_], writes=[bw])
            ab, bab = adabs.next()
            P.dma("sync", ab, ada_b[l, cs_].partition_broadcast(2), writes=[bab])
            pb_i = j % 2

            def mm(e, wt=wt, pb_i=pb_i):
                for k in range(8):
                    ins = e.matmul(psb[pb_i][0:2, 0:CW_], lhsT=sT[:, k, :], rhs=wt[:, k, :],
                                   start=(k == 0), stop=(k == 7))
                return ins
            P.op("pe", mm, reads=[b_sT, bw], writes=[bps[pb_i]])
            mb, bmb = msbs.next()
            P.op("dve", lambda e, mb=mb, ab=ab, pb_i=pb_i: e.tensor_tensor(
                out=mb[0:2, :], in0=psb[pb_i][0:2, 0:CW_], in1=ab[0:2, :], op=ALU.add),
                reads=[bps[pb_i], bab], writes=[bmb])
            P.dma("act", Mscr[l][:, cs_], mb[0:2, :], reads=[bmb], writes=[bM[l]])
            if debug:
                dbg_ops.append(P.dma("act", dbg["d_M"][l][:, cs_], mb[0:2, :], reads=[bmb]))

    def phase0():
        ada_layer(0)

    dbg_ops = []
    phase0()

    _bregs = {}

    def breg(e, v):
        if v not in _bregs:
            _bregs[v] = e.to_reg(v)
        return _bregs[v]

    def conv_jobs():
        for l in range(2):
            for ex in range(NE):
                rows = slice(ex * 128, (ex + 1) * 128)
                gv = WGU[l][rows, :].rearrange("p (k n) -> p k n", k=8)
                yield (gv[:, :, 0:512], moe_w_gate[l, ex].rearrange("(k p) n -> p k n", p=128), bWGU[l])
                yield (gv[:, :, 512:1024], moe_w_up[l, ex].rearrange("(k p) n -> p k n", p=128), bWGU[l])
                yield (WD[l][rows, :].rearrange("p (k n) -> p k n", k=4),
                       moe_w_down[l, ex].rearrange("(k p) n -> p k n", p=128), bWD[l])
    conv_it = conv_jobs()
    conv_left = [96]

    def convert_upto(total):
        convert_some(max(0, conv_left[0] - (96 - total)))

    def convert_some(n):
        for _ in range(n):
            if conv_left[0] == 0:
                return
            o_, i_, _b = next(conv_it)
            conv_left[0] -= 1
            P.dma("pool", o_, i_, writes=[Buf()])

    def load_mod(l, row, which, dst, bdst):
        P.dma("sync", dst, Mscr[l, row, which * D:(which + 1) * D].partition_broadcast(128),
              reads=[bM[l]], writes=[bdst])

    def make_GS(l, gvec, shift_i, scale_i, rows=(0, 1)):
        gt = T([128, D], F32)
        bg = Buf()
        P.dma("sync", gt, gvec.partition_broadcast(128), writes=[bg])
        res = {}
        for row in rows:
            G = T([128, D], F32)
            S = T([128, D], F32)
            bG, bS = Buf(), Buf()
            load_mod(l, row, scale_i, G, bG)
            load_mod(l, row, shift_i, S, bS)
            P.op("dve", lambda e, G=G: e.scalar_tensor_tensor(out=G, in0=G, scalar=1.0, in1=gt,
                                                               op0=ALU.add, op1=ALU.mult),
                 reads=[bG, bg], writes=[bG])
            res[row] = (G, bG, S, bS)
        return res

    def norm_mod(ht, bh, G, bG, S, bS, tmp, btmp, a_out, ba):
        c = stat_i[0] % 32
        stat_i[0] += 1
        ss = stats[:, 2 * c:2 * c + 1]
        rs = stats[:, 2 * c + 1:2 * c + 2]
        bs = Buf()
        P.op("act", lambda e: e.activation(out=tmp, in_=ht, func=AF.Square, accum_out=ss),
             reads=[bh], writes=[btmp, bs])
        P.op("act", lambda e: e.activation(out=rs, in_=ss, func=AF.Ln, bias=epsT[:, 0:1], scale=1.0 / D),
             reads=[bs, b_eps], writes=[bs])
        P.op("act", lambda e: e.activation(out=rs, in_=rs, func=AF.Exp, scale=-0.5), reads=[bs], writes=[bs])
        if G is None:
            return rs, bs
        P.op("dve", lambda e: e.scalar_tensor_tensor(out=tmp, in0=ht, scalar=rs, in1=G,
                                                     op0=ALU.mult, op1=ALU.mult),
             reads=[bh, bs, bG], writes=[btmp])
        P.op("dve", lambda e: e.tensor_tensor(out=a_out, in0=tmp, in1=S, op=ALU.add),
             reads=[btmp, bS], writes=[ba])
        return rs, bs

    def transpose8(a_bf, ba, pbank, dstT, bdst, col0):
        pv = ps_bf(pbank).rearrange("p (k c) -> p k c", k=8)

        def tr(e):
            for k in range(8):
                ins = e.transpose(out=pv[:, k, :], in_=a_bf[:, k * 128:(k + 1) * 128], identity=ident)
            return ins
        P.op("pe", tr, reads=[ba, b_ident], writes=[bps[pbank]])
        P.op("act", lambda e: e.copy(out=dstT[:, :, col0:col0 + 128], in_=pv),
             reads=[bps[pbank]], writes=[bdst])

    def src_rows(i):
        if i < 32:
            return x[i * 128:(i + 1) * 128, :]
        return ctx[(i - 32) * 128:(i - 31) * 128, :]

    def routing(psR_bank, ntile, cwt, bcw, sc, bsc, aff_ready=False):
        n = ntile
        lg = psb[psR_bank][:, 0:n * 16]
        aff = sc[:, 0:n, 0:16]
        sel = sc[:, 0:n, 16:32]
        prs = sc[:, 0:n, 32:56]
        gs = sc[:, 0:n, 56:60]
        gmx = sc[:, 0:n, 60:61]
        gmk = sc[:, 0:n, 61:65]
        msel = sc[:, 0:n, 65:81]
        m1 = sc[:, 0:n, 81:82]
        tmp = sc[:, 0:n, 82:98]
        m2 = sc[:, 0:n, 98:99]
        wsum = sc[:, 0:n, 99:100]
        rd, wr = [bps[psR_bank], bsc, b_rb], [bsc]
        if not aff_ready:
            P.op("act", lambda e: e.activation(out=aff, in_=lg.rearrange("p (n e) -> p n e", e=16), func=AF.Sigmoid),
                 reads=rd, writes=wr)
        else:
            rd = [bsc, b_rb]
        P.op("dve", lambda e: e.tensor_tensor(out=sel, in0=aff, in1=rbT.unsqueeze(1).to_broadcast([128, n, 16]),
                                              op=ALU.add), reads=rd, writes=wr)
        sel4 = sel.rearrange("p n (g k) -> p n g k", k=4)
        prs4 = prs.rearrange("p n (g k) -> p n g k", k=6)
        pi = 0
        for a in range(4):
            for b in range(a + 1, 4):
                P.op("dve", lambda e, a=a, b=b, pi=pi: e.tensor_tensor(
                    out=prs4[:, :, :, pi], in0=sel4[:, :, :, a], in1=sel4[:, :, :, b], op=ALU.add),
                    reads=[bsc], writes=wr)
                pi += 1
        P.op("dve", lambda e: e.tensor_reduce(out=gs, in_=prs4, axis=AX.X, op=ALU.max), reads=[bsc], writes=wr)
        P.op("dve", lambda e: e.tensor_reduce(out=sc[:, 0:n, 60], in_=gs, axis=AX.X, op=ALU.max), reads=[bsc], writes=wr)
        P.op("dve", lambda e: e.tensor_tensor(out=gmk, in0=gs, in1=gmx.to_broadcast([128, n, 4]), op=ALU.is_ge),
             reads=[bsc], writes=wr)
        P.op("dve", lambda e: e.tensor_scalar(out=gmk, in0=gmk, scalar1=BIG, scalar2=-BIG, op0=ALU.mult, op1=ALU.add),
             reads=[bsc], writes=wr)
        P.op("dve", lambda e: e.tensor_tensor(out=msel.rearrange("p n (g k) -> p n g k", k=4), in0=sel4,
                                              in1=gmk.unsqueeze(3).to_broadcast([128, n, 4, 4]), op=ALU.add),
             reads=[bsc], writes=wr)
        P.op("dve", lambda e: e.tensor_reduce(out=sc[:, 0:n, 81], in_=msel, axis=AX.X, op=ALU.max), reads=[bsc], writes=wr)
        P.op("dve", lambda e: e.tensor_tensor(out=tmp, in0=msel, in1=m1.to_broadcast([128, n, 16]), op=ALU.is_ge),
             reads=[bsc], writes=wr)
        P.op("dve", lambda e: e.scalar_tensor_tensor(out=tmp, in0=tmp, scalar=-BIG, in1=msel,
                                                     op0=ALU.mult, op1=ALU.add), reads=[bsc], writes=wr)
        P.op("dve", lambda e: e.tensor_reduce(out=sc[:, 0:n, 98], in_=tmp, axis=AX.X, op=ALU.max), reads=[bsc], writes=wr)
        P.op("dve", lambda e: e.tensor_tensor(out=tmp, in0=msel, in1=m2.to_broadcast([128, n, 16]), op=ALU.is_ge),
             reads=[bsc], writes=wr)
        P.op("dve", lambda e: e.tensor_tensor(out=tmp, in0=tmp, in1=aff, op=ALU.mult), reads=[bsc], writes=wr)
        P.op("dve", lambda e: e.tensor_reduce(out=sc[:, 0:n, 99], in_=tmp, axis=AX.X, op=ALU.add), reads=[bsc], writes=wr)
        P.op("dve", lambda e: e.reciprocal(out=wsum, in_=wsum), reads=[bsc], writes=wr)
        P.op("dve", lambda e: e.tensor_tensor(out=cwt[:, 0:n, :], in0=tmp, in1=wsum.to_broadcast([128, n, 16]), op=ALU.mult),
             reads=[bsc], writes=[bcw])

    def layer0_A():
        new_phase()
        Vcs = T([128, NT, 1024], BF16)
        bV = Buf()
        keep = st["off"]
        Wcs = T([128, 8, 1024], BF16)
        bWcs = Buf()
        Wconv = T([128, 8, 1536], BF16)
        bWconv = Buf()
        P.dma("pool", Wconv, even_w_in[:, 512:2048].rearrange("(k p) n -> p k n", p=128), writes=[bWconv])
        Wf = T([128, 8, 512], BF16)
        bWf = Buf()
        P.dma("pool", Wf, even_w_in[:, 0:512].rearrange("(k p) n -> p k n", p=128), writes=[bWf])
        dD = T([128, 256], BF16)
        bdD = Buf()
        P.dma("sync", dD, dftD, writes=[bdD])
        WfT = T([128, 1024], BF16)
        bWfT = Buf()
        for h in range(4):
            pv = ps_bf(0).rearrange("p (k c) -> p k c", k=8)

            def tr(e, h=h, pv=pv):
                for k in range(8):
                    ins = e.transpose(out=pv[:, k, :], in_=Wf[:, k, h * 128:(h + 1) * 128], identity=ident)
                return ins
            P.op("pe", tr, reads=[bWf, b_ident], writes=[bps[0]])
            P.op("act", lambda e: e.copy(out=WfT, in_=ps_bf(0)), reads=[bps[0]], writes=[bWfT])
            for k in range(8):
                pbk = 1 + (k % 2)
                P.op("pe", lambda e, k=k, pbk=pbk: e.matmul(psb[pbk][:, 0:256], lhsT=WfT[:, k * 128:(k + 1) * 128],
                                                            rhs=dD, start=True, stop=True),
                     reads=[bWfT, bdD], writes=[bps[pbk]])
                P.op("dve", lambda e, k=k, h=h, pbk=pbk: e.tensor_copy(
                    out=Wcs[:, k, :].rearrange("p (two hh c) -> p two hh c", two=2, hh=4)[:, :, h, :],
                    in_=psb[pbk][:, 0:256].rearrange("p (two c) -> p two c", two=2)),
                    reads=[bps[pbk]], writes=[bWcs])
        GS = make_GS(0, norm_mix_g[0], 0, 1)
        hts = Rot(3, [128, D], F32)
        tmps = Rot(2, [128, D], F32)
        abf = Rot(2, [128, D], BF16)
        aT4s = Rot(2, [128, 8, 512], BF16)
        zts = Rot(2, [128, 4, 512], BF16)
        gbs = Rot(2, [128, 4, 512], BF16)
        gcs = Rot(2, [128, 512], F32)
        def l0_stage1(i):
            row = 0 if i < 32 else 1
            G, bG, S, bS = GS[row]
            ht, bh = hts.next()
            P.dma("sync", ht, src_rows(i), writes=[bh])
            tmp, btmp = tmps.next()
            a, ba = abf.next()
            norm_mod(ht, bh, G, bG, S, bS, tmp, btmp, a, ba)
            return a, ba

        def l0_stage2(i, j, a, ba, aT4, baT4):
            transpose8(a, ba, i % 2, aT4, baT4, j * 128)
            pb0 = 2 + 2 * (i % 2)

            def mmv(e):
                for hf in range(2):
                    for k in range(8):
                        ins = e.matmul(psb[pb0 + hf], lhsT=aT4[:, k, j * 128:(j + 1) * 128],
                                       rhs=Wcs[:, k, hf * 512:(hf + 1) * 512], start=(k == 0), stop=(k == 7))
                return ins
            P.op("pe", mmv, reads=[baT4, bWcs], writes=[bps[pb0], bps[pb0 + 1]])
            P.op("act", lambda e: e.copy(out=Vcs[:, i, 0:512], in_=psb[pb0]), reads=[bps[pb0]], writes=[bV])
            P.op("dve", lambda e: e.tensor_copy(out=Vcs[:, i, 512:1024], in_=psb[pb0 + 1]), reads=[bps[pb0 + 1]], writes=[bV])

        ngroups = 9
        pend = l0_stage1(0)
        for g in range(ngroups):
            ntile = 4 if g < 8 else 2
            gw = ntile * 128
            t0 = g * 512
            aT4, baT4 = aT4s.next()
            for j in range(ntile):
                i = g * 4 + j
                nxt = l0_stage1(i + 1) if i + 1 < NT else None
                l0_stage2(i, j, pend[0], pend[1], aT4, baT4)
                pend = nxt
            zt, bz = zts.next()
            gb, bgb = gbs.next()
            for cc in range(4):
                for part in (1, 0, 2):
                    c = part * 4 + cc
                    pbk = 6 + (c % 2)

                    def mmc(e, c=c, pbk=pbk, aT4=aT4, gw=gw):
                        for k in range(8):
                            ins = e.matmul(psb[pbk][:, 0:gw], lhsT=Wconv[:, k, c * 128:(c + 1) * 128],
                                           rhs=aT4[:, k, 0:gw], start=(k == 0), stop=(k == 7))
                        return ins
                    P.op("pe", mmc, reads=[baT4, bWconv], writes=[bps[pbk]])
                    if part == 1:
                        gc, bgc = gcs.next()
                        P.op("act", lambda e, gc=gc, pbk=pbk, gw=gw: e.copy(out=gc[:, 0:gw], in_=psb[pbk][:, 0:gw]),
                             reads=[bps[pbk]], writes=[bgc])
                    elif part == 0:
                        P.op("act", lambda e, gb=gb, cc=cc, pbk=pbk, gw=gw: e.copy(out=gb[:, cc, 0:gw], in_=psb[pbk][:, 0:gw]),
                             reads=[bps[pbk]], writes=[bgb])
                    else:
                        P.op("dve", lambda e, zt=zt, cc=cc, pbk=pbk, gc=gc, gw=gw: e.tensor_tensor(
                            out=zt[:, cc, 0:gw], in0=psb[pbk][:, 0:gw], in1=gc[:, 0:gw], op=ALU.mult),
                            reads=[bps[pbk], bgc], writes=[bz])
            convert_some(3)
            P.dma("act", ZT[:, :, t0:t0 + gw].rearrange("c p t -> p c t"), zt[:, :, 0:gw], reads=[bz], writes=[bZT])
            P.dma("act", GBT[:, :, t0:t0 + gw].rearrange("c p t -> p c t"), gb[:, :, 0:gw], reads=[bgb], writes=[bGBT])
        return Vcs, bV, keep

    def mixer_tail(l, g, ntile, yT, byT, Wout, bWout, gate1, GS2, hts, tmps, fbf, fT4, bfT4, hsrc_fn, psY0, psT_bank,
                   psR_bank, cwt, bcw, rsc, brsc, dbg_hm=None, filler=None, deferred=False):
        gw = ntile * 128
        t0 = g * 512

        def stA(j):
            i = g * 4 + j
            row = 0 if i < 32 else 1

            def mmo(e):
                for hf in range(2):
                    for k in range(8):
                        ins = e.matmul(psb[psY0 + hf], lhsT=yT[:, k, j * 128:(j + 1) * 128],
                                       rhs=Wout[:, k, hf * 512:(hf + 1) * 512], start=(k == 0), stop=(k == 7))
                return ins
            P.op("pe", mmo, reads=[byT, bWout], writes=[bps[psY0], bps[psY0 + 1]])
            ht, bh = hts.next()
            hsrc, hreads = hsrc_fn(i)
            P.dma("sync", ht, hsrc, reads=hreads, writes=[bh])
            tmp, btmp = tmps.next()
            g1, bg1 = gate1[row]
            for hf in range(2):
                P.op("dve", lambda e, hf=hf: e.tensor_tensor(
                    out=tmp[:, hf * 512:(hf + 1) * 512], in0=psb[psY0 + hf], in1=g1[:, hf * 512:(hf + 1) * 512],
                    op=ALU.mult), reads=[bps[psY0 + hf], bg1], writes=[btmp])
            P.op("dve", lambda e: e.tensor_tensor(out=ht, in0=tmp, in1=ht, op=ALU.add), reads=[btmp, bh], writes=[bh])
            P.dma("pool", Hm[i * 128:(i + 1) * 128, :], ht, reads=[bh], writes=[bHm])
            if dbg_hm is not None:
                dbg_ops.append(P.dma("pool", dbg_hm[i * 128:(i + 1) * 128, :], ht, reads=[bh]))
            G, bG, S, bS = GS2[row]
            f, bf_ = fbf.next()
            norm_mod(ht, bh, G, bG, S, bS, tmp, btmp, f, bf_)
            P.dma("act", Fd[i * 128:(i + 1) * 128, :], f, reads=[bf_], writes=[Buf()])
            return f, bf_

        def stB(j, f, bf_):
            transpose8(f, bf_, psT_bank, fT4, bfT4, j * 128)

            def mmr(e):
                for k in range(8):
                    ins = e.matmul(psb[psR_bank][:, j * 16:(j + 1) * 16], lhsT=fT4[:, k, j * 128:(j + 1) * 128],
                                   rhs=Wr[:, k, :], start=(k == 0), stop=(k == 7))
                return ins
            if deferred:
                def mmr0(e):
                    for k in range(8):
                        ins = e.matmul(psb[psR_bank][:, 0:16], lhsT=fT4[:, k, j * 128:(j + 1) * 128],
                                       rhs=Wr[:, k, :], start=(k == 0), stop=(k == 7))
                    return ins
                P.op("pe", mmr0, reads=[bfT4, b_Wr], writes=[bps[psR_bank]])
                P.op("act", lambda e: e.activation(out=rsc[:, j, 0:16], in_=psb[psR_bank][:, 0:16], func=AF.Sigmoid),
                     reads=[bps[psR_bank]], writes=[brsc])
            else:
                P.op("pe", mmr, reads=[bfT4, b_Wr], writes=[bps[psR_bank]])

        def finish():
            routing(psR_bank, ntile, cwt, bcw, rsc, brsc, aff_ready=deferred)
            P.dma("act", CW[t0:t0 + gw, :].rearrange("(n p) e -> p n e", p=128), cwt[:, 0:ntile, :], reads=[bcw], writes=[bCW])

        if deferred:
            return stA, stB, finish

        pend = stA(0)
        for j in range(ntile):
            if filler is not None:
                filler(1)
            nxt = stA(j + 1) if j + 1 < ntile else None
            if filler is not None:
                filler(1)
            stB(j, pend[0], pend[1])
            pend = nxt
        finish()

    def layer0_B(Vcs, bV, keep):
        P.barrier()
        st["off"] = keep
        Wout = T([128, 8, D], BF16)
        bWout = Buf()
        P.dma("pool", Wout, even_w_out.rearrange("(k p) n -> p k n", p=128), writes=[bWout])
        cwc = T([128, 3, 4], F32)
        bcwc = Buf()
        for kk in range(3):
            P.dma("sync", cwc[:, kk, :], even_conv_w[kk].rearrange("(c p) -> p c", p=128), writes=[bcwc],
                  allow_slow_non_contiguous=True)
        dC = T([128, 4, 256], BF16)
        bdC = Buf()
        P.dma("sync", dC, dftC, writes=[bdC])
        GS2 = make_GS(0, norm_ffn_g[0], 3, 4)
        gate1 = []
        for row in range(2):
            gt = T([128, D], F32)
            bg = Buf()
            load_mod(0, row, 2, gt, bg)
            gate1.append((gt, bg))
        ring = Rot(3, [128, 8, 512], BF16)
        yTs = Rot(2, [128, 8, 512], BF16)
        zin = Rot(1, [128, 4, 514], BF16)
        gbin = Rot(1, [128, 4, 512], BF16)
        cacc = Rot(2, [128, 512], F32)
        hts = Rot(2, [128, D], F32)
        tmps = Rot(2, [128, D], F32)
        fbf = Rot(2, [128, D], BF16)
        fT4s = Rot(1, [128, 8, 512], BF16)
        cws = Rot(2, [128, 4, NE], F32)
        rsc = T([128, 4, 128], F32)
        brsc = Buf()
        def fourier_ops(g, yT, byT):
            ops_ = []
            gw_ = 512 if g < 8 else 256
            if g < 8:
                for piece in range(8):
                    def one(piece=piece):
                        rt, brt = ring.next()
                        P.dma("sync", rt, dftN[g, :, piece * 8:(piece + 1) * 8, :], writes=[brt])

                        def mmf(e):
                            for s8_ in range(8):
                                stt = piece * 8 + s8_
                                base = 0 if stt < 32 else 512
                                for h in range(4):
                                    ins = e.matmul(psb[h], lhsT=Vcs[:, stt % 32, base + h * 128: base + (h + 1) * 128],
                                                   rhs=rt[:, s8_, :], start=(stt == 0), stop=(stt == 63))
                            return ins
                        P.op("pe", mmf, reads=[brt, bV], writes=[bps[0], bps[1], bps[2], bps[3]])
                    ops_.append(one)
            else:
                def onec():
                    def mmfc(e):
                        for jj in range(4):
                            base = 0 if jj < 2 else 512
                            for h in range(4):
                                ins = e.matmul(psb[h][:, 0:256], lhsT=Vcs[:, 32 + (jj % 2), base + h * 128: base + (h + 1) * 128],
                                               rhs=dC[:, jj, :], start=(jj == 0), stop=(jj == 3))
                        return ins
                    P.op("pe", mmfc, reads=[bdC, bV], writes=[bps[0], bps[1], bps[2], bps[3]])
                ops_.append(onec)

            def evac():
                for h in range(4):
                    P.op("act", lambda e, h=h: e.copy(out=yT[:, h, 0:gw_], in_=psb[h][:, 0:gw_]), reads=[bps[h]], writes=[byT])
            ops_.append(evac)
            return ops_

        yT_next = yTs.next()
        pending_f = fourier_ops(0, *yT_next)
        for g in range(9):
            ntile = 4 if g < 8 else 2
            gw = ntile * 128
            t0 = g * 512
            yT, byT = yT_next
            while pending_f:
                pending_f.pop(0)()
            if g + 1 < 9:
                yT_next = yTs.next()
                pending_f = fourier_ops(g + 1, *yT_next)

            def filler(n, pending_f=pending_f):
                for _ in range(n):
                    if pending_f:
                        pending_f.pop(0)()
            zt, bz = zin.next()
            gb, bgb = gbin.next()
            first = g in (0, 8)
            last = g in (7, 8)
            lo = t0 - (0 if first else 1)
            hi = t0 + gw + (0 if last else 1)
            c0 = 1 if first else 0
            if first:
                P.op("pool", lambda e, zt=zt: e.memset(zt[:, :, 0:1], 0.0), writes=[bz])
            if last:
                P.op("pool", lambda e, zt=zt, gw=gw: e.memset(zt[:, :, gw + 1:gw + 2], 0.0), writes=[bz])
            P.dma("sync", zt[:, :, c0:c0 + (hi - lo)], ZT[:, :, lo:hi].rearrange("c p t -> p c t"), reads=[bZT], writes=[bz])
            P.dma("sync", gb[:, :, 0:gw], GBT[:, :, t0:t0 + gw].rearrange("c p t -> p c t"), reads=[bGBT], writes=[bgb])
            for cc in range(4):
                ac, bac = cacc.next()
                P.op("dve", lambda e, ac=ac, zt=zt, cc=cc, gw=gw: e.tensor_scalar(
                    out=ac[:, 0:gw], in0=zt[:, cc, 0:gw], scalar1=cwc[:, 0, cc:cc + 1], scalar2=None, op0=ALU.mult),
                    reads=[bz, bcwc], writes=[bac])
                for kk in (1, 2):
                    P.op("dve", lambda e, ac=ac, zt=zt, cc=cc, gw=gw, kk=kk: e.scalar_tensor_tensor(
                        out=ac[:, 0:gw], in0=zt[:, cc, kk:kk + gw], scalar=cwc[:, kk, cc:cc + 1], in1=ac[:, 0:gw],
                        op0=ALU.mult, op1=ALU.add), reads=[bz, bcwc, bac], writes=[bac])
                P.op("dve", lambda e, ac=ac, gb=gb, cc=cc, gw=gw, yT=yT: e.tensor_tensor(
                    out=yT[:, 4 + cc, 0:gw], in0=ac[:, 0:gw], in1=gb[:, cc, 0:gw], op=ALU.mult),
                    reads=[bac, bgb], writes=[byT])
            convert_some(3)
            fT4, bfT4 = fT4s.next()
            cwt, bcw = cws.next()
            mixer_tail(0, g, ntile, yT, byT, Wout, bWout, gate1, GS2, hts, tmps, fbf, fT4, bfT4,
                       lambda i: (src_rows(i), []), 4, 6, 7, cwt, bcw, rsc, brsc, dbg.get("d_hm0"), filler=filler)

    def moe(l, ntok, Hout, bHout, final):
        new_phase()
        sgs = [(0, 2048), (2048, ntok - 2048)]
        accmax = max(n for _, n in sgs) // 128
        acc = T([128, accmax, D], F32)
        bacc = [Buf() for _ in range(accmax)]
        fTs = T([128, 8, accmax * 128], BF16)
        bfTs = Buf()
        cws = T([128, accmax, NE], F32)
        bcws = Buf()
        wgs = Rot(2, [128, 8, 512], BF16)
        wus = Rot(2, [128, 8, 512], BF16)
        wds = Rot(2, [128, 4, D], BF16)
        hid = Rot(2, [128, 4, 512], BF16)
        sgt = Rot(2, [128, 512], F32)
        g2 = []
        for row in range(2):
            gt = T([128, D], F32)
            bg = Buf()
            load_mod(l, row, 5, gt, bg)
            g2.append((gt, bg))
        hts = Rot(2, [128, D], F32)
        tmps = Rot(2, [128, D], F32)
        if final:
            fg = T([128, D], F32)
            bfg = Buf()
            P.dma("sync", fg, final_g.partition_broadcast(128), writes=[bfg])
        outs = []
        for (s0, sn) in sgs:
            ntl = sn // 128
            P.dma("sync", fTs[:, :, 0:sn], FT[:, :, s0:s0 + sn].rearrange("k p t -> p k t"), reads=[bFT], writes=[bfTs])
            P.dma("sync", cws[:, 0:ntl, :], CW[s0:s0 + sn, :].rearrange("(n p) e -> p n e", p=128), reads=[bCW], writes=[bcws])
            groups = [(q, min(512, sn - q)) for q in range(0, sn, 512)]
            for ex in range(NE):
                wg, bwg = wgs.next()
                wu, bwu = wus.next()
                wd, bwd = wds.next()
                P.dma("pool", wg, moe_w_gate[l, ex].rearrange("(k p) n -> p k n", p=128), writes=[bwg])
                P.dma("pool", wu, moe_w_up[l, ex].rearrange("(k p) n -> p k n", p=128), writes=[bwu])
                P.dma("pool", wd, moe_w_down[l, ex].rearrange("(k p) n -> p k n", p=128), writes=[bwd])
                for (q0, qn) in groups:
                    hd, bhd = hid.next()
                    for jc in range(4):
                        pg, pu = (0, 1) if jc % 2 == 0 else (2, 3)

                        def mmg(e, jc=jc, pg=pg, pu=pu, wg=wg, wu=wu, q0=q0, qn=qn):
                            for (pb_, w_) in ((pg, wg), (pu, wu)):
                                for k in range(8):
                                    ins = e.matmul(psb[pb_][:, 0:qn], lhsT=w_[:, k, jc * 128:(jc + 1) * 128],
                                                   rhs=fTs[:, k, q0:q0 + qn], start=(k == 0), stop=(k == 7))
                            return ins
                        P.op("pe", mmg, reads=[bwg, bwu, bfTs], writes=[bps[pg], bps[pu]])
                        sg, bsg = sgt.next()
                        P.op("act", lambda e, sg=sg, pg=pg, qn=qn: e.activation(out=sg[:, 0:qn], in_=psb[pg][:, 0:qn], func=AF.Silu),
                             reads=[bps[pg]], writes=[bsg])
                        P.op("dve", lambda e, sg=sg, pu=pu, hd=hd, jc=jc, qn=qn: e.tensor_tensor(
                            out=hd[:, jc, 0:qn], in0=psb[pu][:, 0:qn], in1=sg[:, 0:qn], op=ALU.mult),
                            reads=[bps[pu], bsg], writes=[bhd])
                    for jt in range(qn // 128):
                        tl = (q0 // 128) + jt
                        for hf in range(2):
                            pd = 4 + ((jt * 2 + hf) % 4)

                            def mmd(e, pd=pd, hd=hd, jt=jt, hf=hf, wd=wd):
                                for jc in range(4):
                                    ins = e.matmul(psb[pd], lhsT=hd[:, jc, jt * 128:(jt + 1) * 128],
                                                   rhs=wd[:, jc, hf * 512:(hf + 1) * 512], start=(jc == 0), stop=(jc == 3))
                                return ins
                            P.op("pe", mmd, reads=[bhd, bwd], writes=[bps[pd]])
                            av = acc[:, tl, hf * 512:(hf + 1) * 512]
                            if ex == 0:
                                P.op("dve", lambda e, av=av, pd=pd, tl=tl, ex=ex: e.tensor_scalar(
                                    out=av, in0=psb[pd], scalar1=cws[:, tl, ex:ex + 1], scalar2=None, op0=ALU.mult),
                                    reads=[bps[pd], bcws], writes=[bacc[tl]])
                            else:
                                P.op("dve", lambda e, av=av, pd=pd, tl=tl, ex=ex: e.scalar_tensor_tensor(
                                    out=av, in0=psb[pd], scalar=cws[:, tl, ex:ex + 1], in1=av,
                                    op0=ALU.mult, op1=ALU.add), reads=[bps[pd], bcws, bacc[tl]], writes=[bacc[tl]])
            for tl in range(ntl):
                i = s0 // 128 + tl
                row = 0 if i < 32 else 1
                ht, bh = hts.next()
                P.dma("sync", ht, Hm[i * 128:(i + 1) * 128, :], reads=[bHm], writes=[bh])
                gt, bg = g2[row]
                P.op("pool", lambda e, tl=tl, gt=gt: e.tensor_tensor(out=acc[:, tl, :], in0=acc[:, tl, :], in1=gt, op=ALU.mult),
                     reads=[bacc[tl], bg], writes=[bacc[tl]])
                P.op("pool", lambda e, tl=tl, ht=ht: e.tensor_tensor(out=ht, in0=acc[:, tl, :], in1=ht, op=ALU.add),
                     reads=[bacc[tl], bh], writes=[bh])
                if not final:
                    P.dma("pool", Hout[i * 128:(i + 1) * 128, :], ht, reads=[bh], writes=[bHout])
                    if debug:
                        dbg_ops.append(P.dma("pool", dbg["d_h1"][i * 128:(i + 1) * 128, :], ht, reads=[bh]))
                else:
                    tmp, btmp = tmps.next()
                    rs, bs = norm_mod(ht, bh, None, None, None, None, tmp, btmp, None, None)
                    P.op("dve", lambda e, tmp=tmp, ht=ht, rs=rs: e.scalar_tensor_tensor(
                        out=tmp, in0=ht, scalar=rs, in1=fg, op0=ALU.mult, op1=ALU.mult),
                        reads=[bh, bs, bfg], writes=[btmp])
                    outs.append(P.dma("act", out[i * 128:(i + 1) * 128, :], tmp, reads=[btmp]))
        return outs


    def layer1_A():
        new_phase()
        Up = T([128, 32, 512], BF16)
        qT = T([128, 4, SEQ], BF16)
        kT = T([128, 2, NTOK], BF16)
        Vt = T([128, NT, 128], BF16)
        bUp, bqT, bkT, bVt = Buf(), Buf(), Buf(), Buf()
        keep = st["off"]
        Wp = T([128, 8, 512], BF16)
        Wq = T([128, 8, 512], BF16)
        Wqr = T([128, 8, 512], BF16)
        Wk = T([128, 8, 256], BF16)
        Wkr = T([128, 8, 256], BF16)
        Wv = T([128, 8, 128], BF16)
        bWp, bWq, bWqr, bWk, bWkr, bWv = Buf(), Buf(), Buf(), Buf(), Buf(), Buf()
        wv = odd_w_in.rearrange("(k p) n -> p k n", p=128)
        P.dma("pool", Wp, wv[:, :, 0:512], writes=[bWp])
        P.dma("pool", Wq, wv[:, :, 512:1024], writes=[bWq])
        for a in range(4):
            P.dma("pool", Wk[:, :, a * 64:(a + 1) * 64], wv[:, :, 1024 + (a // 2) * 64:1024 + (a // 2 + 1) * 64], writes=[bWk])
        P.dma("pool", Wv, wv[:, :, 1152:1280], writes=[bWv])
        for (src, bsrc, dst, bdst, nblk) in ((Wq, bWq, Wqr, bWqr, 16), (Wk, bWk, Wkr, bWkr, 8)):
            sv = src.rearrange("p k (blk two i) -> p (k blk) two i", two=2, i=16)
            dv = dst.rearrange("p k (blk two i) -> p (k blk) two i", two=2, i=16)
            P.op("dve", lambda e, sv=sv, dv=dv: e.tensor_scalar(out=dv[:, :, 0, :], in0=sv[:, :, 1, :], scalar1=-1.0,
                                                              scalar2=None, op0=ALU.mult), reads=[bsrc], writes=[bdst])
            P.op("dve", lambda e, sv=sv, dv=dv: e.tensor_copy(out=dv[:, :, 1, :], in_=sv[:, :, 0, :]), reads=[bsrc], writes=[bdst])
        GS = make_GS(1, norm_mix_g[1], 0, 1)
        hts = Rot(2, [128, D], F32)
        tmps = Rot(2, [128, D], F32)
        abf = Rot(2, [128, D], BF16)
        aT4s = Rot(2, [128, 8, 512], BF16)
        ropes = Rot(2, [128, 2, 512], F32)
        rt1 = Rot(2, [128, 512], F32)
        rt2 = Rot(2, [128, 512], F32)
        fm_i = [0]

        def l1_stage1(i):
            row = 0 if i < 32 else 1
            G, bG, S, bS = GS[row]
            ht, bh = hts.next()
            P.dma("sync", ht, H1[i * 128:(i + 1) * 128, :], reads=[bH1], writes=[bh])
            tmp, btmp = tmps.next()
            a, ba = abf.next()
            norm_mod(ht, bh, G, bG, S, bS, tmp, btmp, a, ba)
            return a, ba

        def l1_stage2(i, j, a, ba, aT4, baT4):
            transpose8(a, ba, i % 2, aT4, baT4, j * 128)
            if i < 32:
                def mmp(e):
                    for k in range(8):
                        ins = e.matmul(psb[2], lhsT=aT4[:, k, j * 128:(j + 1) * 128], rhs=Wp[:, k, :],
                                       start=(k == 0), stop=(k == 7))
                    return ins
                P.op("pe", mmp, reads=[baT4, bWp], writes=[bps[2]])
                P.op("act", lambda e: e.copy(out=Up[:, i, :], in_=psb[2]), reads=[bps[2]], writes=[bUp])

            def mmvv(e):
                for k in range(8):
                    ins = e.matmul(psb[3][:, 0:128], lhsT=aT4[:, k, j * 128:(j + 1) * 128], rhs=Wv[:, k, :],
                                   start=(k == 0), stop=(k == 7))
                return ins
            P.op("pe", mmvv, reads=[baT4, bWv], writes=[bps[3]])
            P.op("dve", lambda e: e.tensor_copy(out=Vt[:, i, :], in_=psb[3][:, 0:128]), reads=[bps[3]], writes=[bVt])

        pend1 = [l1_stage1(0)]
        for g in range(9):
            ntile = 4 if g < 8 else 2
            gw = ntile * 128
            t0 = g * 512
            aT4, baT4 = aT4s.next()
            for j in range(ntile):
                i = g * 4 + j
                nxt1 = l1_stage1(i + 1) if i + 1 < NT else None
                l1_stage2(i, j, pend1[0][0], pend1[0][1], aT4, baT4)
                pend1[0] = nxt1
            convert_some(3)
            if g < 8:
                rp, brp = ropes.next()
                P.dma("sync", rp, rope[:, :, t0:t0 + 512], writes=[brp])
            jobs = [("k", jj) for jj in range(2)]
            if g < 8:
                jobs = [("q", cc) for cc in range(4)] + jobs
            for (kind, cc) in jobs:
                W, bW, Wr_, bWr_ = (Wq, bWq, Wqr, bWqr) if kind == "q" else (Wk, bWk, Wkr, bWkr)
                pA, pB = (4, 5) if fm_i[0] % 2 == 0 else (6, 7)
                fm_i[0] += 1
                rot = g < 8

                def mmq(e, W=W, Wr_=Wr_, cc=cc, pA=pA, pB=pB, aT4=aT4, gw=gw, rot=rot):
                    for (pb_, w_) in ((pA, W), (pB, Wr_)) if rot else ((pA, W),):
                        for k in range(8):
                            ins = e.matmul(psb[pb_][:, 0:gw], lhsT=w_[:, k, cc * 128:(cc + 1) * 128], rhs=aT4[:, k, 0:gw],
                                           start=(k == 0), stop=(k == 7))
                    return ins
                P.op("pe", mmq, reads=[baT4, bW, bWr_], writes=[bps[pA], bps[pB]])
                dst = qT[:, cc, t0:t0 + gw] if kind == "q" else kT[:, cc, t0:t0 + gw]
                bdst = bqT if kind == "q" else bkT
                if rot:
                    t1, bt1 = rt1.next()
                    t2, bt2 = rt2.next()
                    P.op("dve", lambda e, t1=t1, pA=pA, rp=rp: e.tensor_tensor(out=t1, in0=psb[pA], in1=rp[:, 0, :], op=ALU.mult),
                         reads=[bps[pA], brp], writes=[bt1])
                    P.op("dve", lambda e, t2=t2, pB=pB, rp=rp: e.tensor_tensor(out=t2, in0=psb[pB], in1=rp[:, 1, :], op=ALU.mult),
                         reads=[bps[pB], brp], writes=[bt2])
                    P.op("dve", lambda e, t1=t1, t2=t2, dst=dst: e.tensor_tensor(out=dst, in0=t1, in1=t2, op=ALU.add),
                         reads=[bt1, bt2], writes=[bdst])
                else:
                    P.op("act", lambda e, dst=dst, pA=pA, gw=gw: e.copy(out=dst, in_=psb[pA][:, 0:gw]), reads=[bps[pA]], writes=[bdst])
        return (Up, bUp, qT, bqT, kT, bkT, Vt, bVt), keep

    def layer1_B(pk, keep):
        Up, bUp, qT, bqT, kT, bkT, Vt, bVt = pk
        P.barrier()
        st["off"] = keep
        Wout = T([128, 8, D], BF16)
        bWout = Buf()
        P.dma("pool", Wout, odd_w_out.rearrange("(k p) n -> p k n", p=128), writes=[bWout])
        pB = T([128, 20, 128], BF16)
        bpB = Buf()
        P.dma("sync", pB, poolB, writes=[bpB])
        PW = T([128, 4, 128], BF16)
        bPW = Buf()
        P.dma("pool", PW, odd_pool_w.rearrange("g c d -> c g d"), writes=[bPW])
        psc = T([128, 4], F32)
        bpsc = Buf()
        P.dma("sync", psc, odd_pool_scale.rearrange("(g p) -> p g", p=128), writes=[bpsc], allow_slow_non_contiguous=True)
        mk = T([128, 2, 128], BF16)
        bmk = Buf()
        P.dma("pool", mk, amask, writes=[bmk])
        sk8 = T([128, 8], F32)
        bsk8 = Buf()
        P.dma("sync", sk8, odd_sink.partition_broadcast(128), writes=[bsk8])
        P.op("dve", lambda e: e.tensor_scalar(out=sk8, in0=sk8, scalar1=8.0, scalar2=None, op0=ALU.mult), reads=[bsk8], writes=[bsk8])
        GS2 = make_GS(1, norm_ffn_g[1], 3, 4, rows=(0,))
        gt = T([128, D], F32)
        bg = Buf()
        load_mod(1, 0, 2, gt, bg)
        gate1 = {0: (gt, bg)}
        yTs = Rot(2, [128, 8, 512], BF16)
        Ps = Rot(3, [128, 640], BF16)
        PTs = Rot(3, [128, 5, 128], BF16)
        pTs = Rot(1, [128, 4, 128], BF16)
        Osb = Rot(2, [128, 8, 64], BF16)
        st8 = Rot(2, [128, 8, 8], F32)
        st8h = [[Buf() for _ in range(8)] for _ in range(2)]
        st8b = [Buf(), Buf()]
        hts = Rot(2, [128, D], F32)
        tmps = Rot(2, [128, D], F32)
        fbf = Rot(2, [128, D], BF16)
        fT4s = Rot(1, [128, 8, 512], BF16)
        cws = Rot(2, [128, 4, NE], F32)
        rsc = T([128, 4, 128], F32)
        brsc = Buf()
        hcount = [0]
        def attn_block(i, cs, yT, byT):
            wb = [b_ for b_ in (i - 1, i, i + 1) if 0 <= b_ < 32]
            nw = len(wb) * 128
            w0 = wb[0] * 128
            c_lo = 512 - nw
            s8, bs8 = st8.next()
            osb, bosb = Osb.next()
            pO = psb[6].rearrange("p (h d) -> p h d", h=8)
            slot8 = (st8.i - 1) % 2
            hb = st8h[slot8]

            def st_qk(h):
                c, half, kvj = h // 2, h % 2, h // 4
                pr = slice(half * 64, (half + 1) * 64)
                pi = hcount[0] % 2
                hcount[0] += 1
                pair = pairs[pi]
                bpair = [bps[2 * pi], bps[2 * pi + 1]]

                def mmqk(e, pair=pair, pr=pr, c=c, kvj=kvj):
                    qv = qT[pr, c, i * 128:(i + 1) * 128]
                    e.matmul(pair[:, c_lo:512], lhsT=qv, rhs=kT[pr, kvj, w0:w0 + nw], start=True, stop=False,
                             skip_group_check=True)
                    if wb[0] == i - 1:
                        e.matmul(pair[:, c_lo:c_lo + 128], lhsT=ident, rhs=mk[:, 0, :], start=False, stop=False,
                                 skip_group_check=True)
                    if wb[-1] == i + 1:
                        e.matmul(pair[:, 384:512], lhsT=ident, rhs=mk[:, 1, :], start=False, stop=False,
                                 skip_group_check=True)
                    ins = e.matmul(pair[:, 512:768], lhsT=qv, rhs=kT[pr, kvj, SEQ:SEQ + 256], start=True, stop=True,
                                   skip_group_check=True)
                    return ins
                P.op("pe", mmqk, reads=[bqT, bkT, bmk, b_ident], writes=bpair)
                sv = pair[:, c_lo:768]
                nk = 768 - c_lo
                P.op("dve", lambda e: e.tensor_reduce(out=s8[:, 0, h:h + 1], in_=sv, axis=AX.X, op=ALU.max),
                     reads=bpair, writes=[hb[h]])
                P.op("dve", lambda e: e.tensor_scalar(out=s8[:, 2, h:h + 1], in0=s8[:, 0, h:h + 1], scalar1=sk8[:, h:h + 1],
                                                      scalar2=-0.125, op0=ALU.max, op1=ALU.mult),
                     reads=[hb[h], bsk8], writes=[hb[h]])
                Pt, bPt = Ps.next()
                P.op("act", lambda e: e.activation(
                    out=Pt[:, 0:nk], in_=sv, func=AF.Exp, bias=s8[:, 2, h:h + 1], scale=0.125, accum_out=s8[:, 3, h:h + 1]),
                    reads=bpair + [hb[h]], writes=[bPt, hb[h]])
                return (h, kvj, Pt, bPt, nk)

            def st_tr(stt):
                h, kvj, Pt, bPt, nk = stt
                nch = nk // 128
                ptb = 4 + (h % 2)
                ptv = ps_bf(ptb)[:, 0:640].rearrange("p (n q) -> p n q", n=5)

                def trp(e):
                    for n_ in range(nch):
                        ins = e.transpose(out=ptv[:, n_, :], in_=Pt[:, n_ * 128:(n_ + 1) * 128], identity=ident)
                    return ins
                P.op("pe", trp, reads=[bPt, b_ident], writes=[bps[ptb]])
                PT_, bPT = PTs.next()
                if h % 2 == 0:
                    P.op("act", lambda e: e.copy(out=PT_[:, 0:nch, :], in_=ptv[:, 0:nch, :]), reads=[bps[ptb]], writes=[bPT])
                else:
                    P.op("dve", lambda e: e.tensor_copy(out=PT_[:, 0:nch, :], in_=ptv[:, 0:nch, :]), reads=[bps[ptb]], writes=[bPT])
                return (h, kvj, PT_, bPT)

            def st_pv(stt):
                h, kvj, PT_, bPT = stt
                vt = wb + [32, 33]

                def mmpv(e):
                    for n_, tl in enumerate(vt):
                        ins = e.matmul(pO[:, h, :], lhsT=PT_[:, n_, :], rhs=Vt[:, tl, kvj * 64:(kvj + 1) * 64],
                                       start=(n_ == 0), stop=(n_ == len(vt) - 1))
                    return ins
                P.op("pe", mmpv, reads=[bPT, bVt], writes=[bps[6]])

            q_st = {0: st_qk(0)}
            t_st = {}
            for h in range(8):
                if h + 1 < 8:
                    q_st[h + 1] = st_qk(h + 1)
                t_st[h] = st_tr(q_st[h])
                if h >= 1:
                    st_pv(t_st[h - 1])
            st_pv(t_st[7])
            bs8 = st8b[slot8]
            P.op("dve", lambda e, s8=s8: e.scalar_tensor_tensor(out=s8[:, 4, :], in0=s8[:, 2, :], scalar=8.0, in1=sk8,
                                                                op0=ALU.mult, op1=ALU.add),
                 reads=[bs8, bsk8] + hb, writes=[bs8])
            P.op("act", lambda e, s8=s8: e.activation(out=s8[:, 4, :], in_=s8[:, 4, :], func=AF.Exp, scale=0.125),
                 reads=[bs8], writes=[bs8])
            P.op("dve", lambda e, s8=s8: e.tensor_tensor(out=s8[:, 5, :], in0=s8[:, 4, :], in1=s8[:, 3, :], op=ALU.add),
                 reads=[bs8] + hb, writes=[bs8])
            P.op("dve", lambda e, s8=s8: e.reciprocal(out=s8[:, 5, :], in_=s8[:, 5, :]), reads=[bs8], writes=[bs8])
            P.op("dve", lambda e, s8=s8, osb=osb: e.tensor_tensor(out=osb, in0=pO, in1=s8[:, 5, :].unsqueeze(2).to_broadcast([128, 8, 64]),
                                                                  op=ALU.mult), reads=[bps[6], bs8], writes=[bosb])
            otv = ps_bf(7)[:, 0:512].rearrange("p (n q) -> p n q", n=4)
            of = osb.rearrange("p h d -> p (h d)")

            def post():
                def tro(e):
                    for n_ in range(4):
                        ins = e.transpose(out=otv[:, n_, :], in_=of[:, n_ * 128:(n_ + 1) * 128], identity=ident)
                    return ins
                P.op("pe", tro, reads=[bosb, b_ident], writes=[bps[7]])
                P.op("act", lambda e: e.copy(out=yT[:, 4:8, cs], in_=otv), reads=[bps[7]], writes=[byT])
            return post

        prev_post = [None]
        prev_yT = None

        tail = [None]
        tail_f = {}

        def do_tail(g, yT, byT):
            convert_some(3)
            fT4, bfT4 = fT4s.next()
            cwt, bcw = cws.next()
            return mixer_tail(1, g, 4, yT, byT, Wout, bWout, gate1, GS2, hts, tmps, fbf, fT4, bfT4,
                              lambda i: (H1[i * 128:(i + 1) * 128, :], [bH1]), 0, 2, 3, cwt, bcw, rsc, brsc,
                              dbg.get("d_hm1"), deferred=True)

        for g in range(8):
            yT, byT = yTs.next()
            for j in range(4):
                i = g * 4 + j
                cs = slice(j * 128, (j + 1) * 128)
                srcs = []
                if i > 0:
                    srcs.append((i - 1, 0))
                srcs.append((i, 3 if i == 0 else (4 if i == 31 else 1)))
                if i < 31:
                    srcs.append((i + 1, 2))
                pv4 = psb[7].rearrange("p (g t) -> p g t", g=4)

                def mmpool(e, srcs=srcs):
                    for gi in range(4):
                        for n_, (s_, kind) in enumerate(srcs):
                            ins = e.matmul(pv4[:, gi, :], lhsT=Up[:, s_, gi * 128:(gi + 1) * 128], rhs=pB[:, gi * 5 + kind, :],
                                           start=(n_ == 0), stop=(n_ == len(srcs) - 1))
                    return ins
                P.op("pe", mmpool, reads=[bUp, bpB], writes=[bps[7]])
                pT_, bpT = pTs.next()
                P.op("act", lambda e, pT_=pT_: e.copy(out=pT_, in_=pv4), reads=[bps[7]], writes=[bpT])

                def mmpw(e, pT_=pT_):
                    for gi in range(4):
                        ins = e.matmul(pv4[:, gi, :], lhsT=PW[:, gi, :], rhs=pT_[:, gi, :], start=True, stop=True)
                    return ins
                P.op("pe", mmpw, reads=[bpT, bPW], writes=[bps[7]])
                P.op("dve", lambda e, yT=yT, cs=cs: e.tensor_tensor(out=yT[:, 0:4, cs], in0=pv4,
                                                                   in1=psc.unsqueeze(2).to_broadcast([128, 4, 128]), op=ALU.mult),
                     reads=[bps[7], bpsc], writes=[byT])
                if prev_post[0] is not None:
                    prev_post[0]()
                    prev_post[0] = None
                if g > 0:
                    if j == 0:
                        if tail[0] is not None:
                            tA, tB, tF = tail[0]
                            tB(3, *tail_f[3])
                            tF()
                        tail[0] = do_tail(g - 1, prev_yT[0], prev_yT[1])
                    tA, tB, tF = tail[0]
                    tail_f[j] = tA(j)
                    if j > 0:
                        tB(j - 1, *tail_f[j - 1])
                prev_post[0] = attn_block(i, cs, yT, byT)
            prev_yT = (yT, byT)
        prev_post[0]()
        if tail[0] is not None:
            tA, tB, tF = tail[0]
            tB(3, *tail_f[3])
            tF()
        tA, tB, tF = do_tail(7, prev_yT[0], prev_yT[1])
        pend = tA(0)
        for j in range(4):
            nxt = tA(j + 1) if j + 1 < 4 else None
            tB(j, *pend)
            pend = nxt
        tF()


    def moe_sparse(l, ntok, Hout, bHout, final):
        convert_upto(48 * (l + 1))
        new_phase()
        NTl = ntok // 128
        NTS = (2 * ntok) // 256 + NE - 1
        nsl = NTS * 256
        cwa = T([128, NTl, NE], F32)
        mka = T([128, NTl, NE], F32)
        mkb = T([128, NTl, NE], BF16)
        sp = T([128, NTl, NE], F32)
        v1 = T([128, NTl, NE], F32)
        v2 = T([128, NTl, NE], F32)
        tm = T([128, NTl, NE], F32)
        bR = Buf()
        Lt = T([128, 128], BF16)
        ones = T([128, 128], BF16)
        sm = T([128, 16, NE], F32)
        smi = T([128, NE], I32)
        slo = T([128, NTl], F32)
        shi = T([128, NTl], F32)
        clo = T([128, NTl], F32)
        chi = T([128, NTl], F32)
        sloi = sloi_t[:, 0:NTl]
        shii = shii_t[:, 0:NTl]
        tau = T([128, 64], F32)
        pix = T([128, 1], F32)
        cmpt = T([128, NTS, NE], F32)
        ecn = T([128, NTS], F32)
        widx = widx_t[:, 0:NTS]
        P.dma("sync", cwa, CW[0:ntok, :].rearrange("(n p) e -> p n e", p=128), reads=[bCW], writes=[bR])
        P.dma("sync", tau, tauc, writes=[bR])
        P.dma("sync", pix, pidx, writes=[bR])
        P.op("pool", lambda e: e.memset(ones, 1.0), writes=[bR])
        P.op("pool", lambda e: e.memset(Lt, 1.0), writes=[bR])
        P.op("pool", lambda e: e.affine_select(out=Lt, in_=Lt, pattern=[[1, 128]], compare_op=ALU.is_gt, fill=0.0,
                                               base=0, channel_multiplier=-1), reads=[bR], writes=[bR])
        P.op("dve", lambda e: e.tensor_scalar(out=mka, in0=cwa, scalar1=0.0, scalar2=None, op0=ALU.is_gt), reads=[bR], writes=[bR])
        P.op("dve", lambda e: e.tensor_copy(out=mkb, in_=mka), reads=[bR], writes=[bR])
        half = (NTl + 1) // 2

        def mmpos(e):
            for i in range(NTl):
                pbk, col = (0, i) if i < half else (1, i - half)
                o_ = psb[pbk][:, col * 16:(col + 1) * 16]
                for i2 in range(i):
                    e.matmul(o_, lhsT=ones, rhs=mkb[:, i2, :], start=(i2 == 0), stop=False)
                e.matmul(o_, lhsT=Lt, rhs=mkb[:, i, :], start=(i == 0), stop=True)
            for i in range(NTl):
                ins = e.matmul(psb[2][:, 0:16], lhsT=ones, rhs=mkb[:, i, :], start=(i == 0), stop=(i == NTl - 1))
            return ins
        P.op("pe", mmpos, reads=[bR], writes=[bps[0], bps[1], bps[2]])
        rd = [bR, bps[0], bps[1], bps[2]]
        P.op("dve", lambda e: e.tensor_scalar(out=sm[:, 0, :], in0=psb[2][:, 0:16], scalar1=255.0, scalar2=None, op0=ALU.add),
             reads=rd, writes=[bR])
        P.op("dve", lambda e: e.tensor_copy(out=smi, in_=sm[:, 0, :]), reads=[bR], writes=[bR])
        P.op("dve", lambda e: e.tensor_scalar(out=smi, in0=smi, scalar1=8, scalar2=8, op0=ALU.arith_shift_right,
                                              op1=ALU.logical_shift_left), reads=[bR], writes=[bR])
        P.op("dve", lambda e: e.tensor_copy(out=sm[:, 1, :], in_=smi), reads=[bR], writes=[bR])
        P.op("dve", lambda e: e.tensor_copy(out=sm[:, 2, :], in_=sm[:, 1, :]), reads=[bR], writes=[bR])
        cur, nxt = 2, 3
        for d_ in (1, 2, 4, 8):
            P.op("dve", lambda e, cur=cur, nxt=nxt, d_=d_: e.tensor_tensor(
                out=sm[:, nxt, d_:16], in0=sm[:, cur, d_:16], in1=sm[:, cur, 0:16 - d_], op=ALU.add), reads=[bR], writes=[bR])
            P.op("dve", lambda e, cur=cur, nxt=nxt, d_=d_: e.tensor_copy(out=sm[:, nxt, 0:d_], in_=sm[:, cur, 0:d_]),
                 reads=[bR], writes=[bR])
            cur, nxt = nxt, cur
        P.op("dve", lambda e, cur=cur: e.tensor_tensor(out=sm[:, 4, :], in0=sm[:, cur, :], in1=sm[:, 1, :], op=ALU.subtract),
             reads=[bR], writes=[bR])
        start = sm[:, 4, :]
        P.op("dve", lambda e: e.tensor_tensor(out=sp[:, 0:half, :], in0=psb[0][:, 0:half * 16].rearrange("p (n e) -> p n e", e=16),
                                              in1=start.unsqueeze(1).to_broadcast([128, half, NE]), op=ALU.add), reads=rd, writes=[bR])
        P.op("dve", lambda e: e.tensor_tensor(out=sp[:, half:NTl, :],
                                              in0=psb[1][:, 0:(NTl - half) * 16].rearrange("p (n e) -> p n e", e=16),
                                              in1=start.unsqueeze(1).to_broadcast([128, NTl - half, NE]), op=ALU.add), reads=rd, writes=[bR])
        P.op("dve", lambda e: e.tensor_scalar(out=tm, in0=mka, scalar1=-1.0e6, scalar2=1.0e6, op0=ALU.mult, op1=ALU.add),
             reads=[bR], writes=[bR])
        P.op("dve", lambda e: e.tensor_tensor(out=sp, in0=sp, in1=mka, op=ALU.mult), reads=[bR], writes=[bR])
        P.op("dve", lambda e: e.tensor_tensor(out=v1, in0=sp, in1=tm, op=ALU.add), reads=[bR], writes=[bR])
        P.op("dve", lambda e: e.tensor_tensor(out=v2, in0=sp, in1=tm, op=ALU.subtract), reads=[bR], writes=[bR])
        P.op("dve", lambda e: e.tensor_reduce(out=slo, in_=v1, axis=AX.X, op=ALU.min), reads=[bR], writes=[bR])
        P.op("dve", lambda e: e.tensor_reduce(out=shi, in_=v2, axis=AX.X, op=ALU.max), reads=[bR], writes=[bR])
        P.op("dve", lambda e: e.tensor_tensor(out=v1, in0=v1, in1=slo.unsqueeze(2).to_broadcast([128, NTl, NE]), op=ALU.is_equal),
             reads=[bR], writes=[bR])
        P.op("dve", lambda e: e.tensor_tensor(out=v1, in0=v1, in1=cwa, op=ALU.mult), reads=[bR], writes=[bR])
        P.op("dve", lambda e: e.tensor_reduce(out=clo, in_=v1, axis=AX.X, op=ALU.add), reads=[bR], writes=[bR])
        P.op("dve", lambda e: e.tensor_tensor(out=v2, in0=v2, in1=shi.unsqueeze(2).to_broadcast([128, NTl, NE]), op=ALU.is_equal),
             reads=[bR], writes=[bR])
        P.op("dve", lambda e: e.tensor_tensor(out=v2, in0=v2, in1=cwa, op=ALU.mult), reads=[bR], writes=[bR])
        P.op("dve", lambda e: e.tensor_reduce(out=chi, in_=v2, axis=AX.X, op=ALU.add), reads=[bR], writes=[bR])
        P.op("dve", lambda e: e.tensor_copy(out=sloi, in_=slo), reads=[bR], writes=[bR])
        P.op("dve", lambda e: e.tensor_copy(out=shii, in_=shi), reads=[bR], writes=[bR])
        P.op("dve", lambda e: e.tensor_tensor(out=cmpt, in0=start.unsqueeze(1).to_broadcast([128, NTS, NE]),
                                              in1=tau[:, 0:NTS].unsqueeze(2).to_broadcast([128, NTS, NE]), op=ALU.is_le),
             reads=[bR], writes=[bR])
        P.op("dve", lambda e: e.tensor_reduce(out=ecn, in_=cmpt, axis=AX.X, op=ALU.add), reads=[bR], writes=[bR])
        P.op("dve", lambda e: e.tensor_scalar(out=ecn, in0=ecn, scalar1=128.0, scalar2=-128.0, op0=ALU.mult, op1=ALU.add),
             reads=[bR], writes=[bR])
        P.op("dve", lambda e: e.tensor_scalar(out=ecn, in0=ecn, scalar1=pix[:, 0:1], scalar2=None, op0=ALU.add), reads=[bR], writes=[bR])
        P.op("dve", lambda e: e.tensor_copy(out=widx, in_=ecn), reads=[bR], writes=[bR])
        if l == 0:
            ada_layer(1)
        fts = Rot(3, [128, D], BF16)
        for i in range(NTl):
            ft, bft = fts.next()
            P.dma("sync", ft, Fd[i * 128:(i + 1) * 128, :], writes=[bft])
            for idx in (sloi, shii):
                P.op("pool", lambda e, ft=ft, idx=idx, i=i: e.indirect_dma_start(
                    out=Xs[:, :], out_offset=bass.IndirectOffsetOnAxis(ap=idx[:, i:i + 1], axis=0),
                    in_=ft, in_offset=None, bounds_check=breg(e, nsl - 1), oob_is_err=False),
                    reads=[bft, bR], writes=[Buf()], dma=True)
        P.barrier()
        mark_e = st["off"]
        xss = Rot(2, [128, 2, D], BF16)
        xTs = Rot(2, [128, 8, 256], BF16)
        nwb = 3
        wgus = Rot(nwb, [128, 8, 1024], BF16)
        wds = Rot(nwb, [128, 4, 1024], BF16)
        hids = Rot(2, [128, 4, 256], BF16)
        sgs_ = Rot(2, [128, 256], F32)
        yss = Rot(2, [128, 2, D], F32)
        for tq in range(NTS):
            xs_, bxs = xss.next()
            P.dma("sync", xs_, Xs[tq * 256:(tq + 1) * 256, :].rearrange("(j p) d -> p j d", p=128), writes=[bxs])
            wgu, bwgu = wgus.next()
            wd, bwd = wds.next()
            P.op("pool", lambda e, wgu=wgu, tq=tq: e.indirect_dma_start(
                out=wgu.rearrange("p k n -> p (k n)"), out_offset=None, in_=WGU[l][:, :],
                in_offset=bass.IndirectOffsetOnAxis(ap=widx[:, tq:tq + 1], axis=0), bounds_check=breg(e, NE * 128 - 1), oob_is_err=False),
                reads=[bR], writes=[bwgu], dma=True)
            P.op("pool", lambda e, wd=wd, tq=tq: e.indirect_dma_start(
                out=wd.rearrange("p k n -> p (k n)"), out_offset=None, in_=WD[l][:, :],
                in_offset=bass.IndirectOffsetOnAxis(ap=widx[:, tq:tq + 1], axis=0), bounds_check=breg(e, NE * 128 - 1), oob_is_err=False),
                reads=[bR], writes=[bwd], dma=True)
            xT, bxT = xTs.next()
            for j in range(2):
                transpose8(xs_[:, j, :], bxs, j, xT, bxT, j * 128)
            hd, bhd = hids.next()
            for jc in range(4):
                pg, pu = (2, 3) if jc % 2 == 0 else (4, 5)

                def mmg(e, jc=jc, pg=pg, pu=pu, wgu=wgu, xT=xT):
                    for (pb_, off) in ((pg, 0), (pu, 512)):
                        for k in range(8):
                            ins = e.matmul(psb[pb_][:, 0:256], lhsT=wgu[:, k, off + jc * 128: off + (jc + 1) * 128],
                                           rhs=xT[:, k, :], start=(k == 0), stop=(k == 7))
                    return ins
                P.op("pe", mmg, reads=[bwgu, bxT], writes=[bps[pg], bps[pu]])
                sg, bsg = sgs_.next()
                P.op("act", lambda e, sg=sg, pg=pg: e.activation(out=sg, in_=psb[pg][:, 0:256], func=AF.Silu),
                     reads=[bps[pg]], writes=[bsg])
                P.op("dve", lambda e, sg=sg, pu=pu, hd=hd, jc=jc: e.tensor_tensor(out=hd[:, jc, :], in0=psb[pu][:, 0:256], in1=sg, op=ALU.mult),
                     reads=[bps[pu], bsg], writes=[bhd])
            ys_, bys = yss.next()
            for j in range(2):
                for hf in range(2):
                    pd = 6 + ((j * 2 + hf) % 2)

                    def mmd(e, pd=pd, hd=hd, j=j, hf=hf, wd=wd):
                        for jc in range(4):
                            ins = e.matmul(psb[pd], lhsT=hd[:, jc, j * 128:(j + 1) * 128],
                                           rhs=wd[:, jc, hf * 512:(hf + 1) * 512], start=(jc == 0), stop=(jc == 3))
                        return ins
                    P.op("pe", mmd, reads=[bhd, bwd], writes=[bps[pd]])
                    if hf == 0:
                        P.op("act", lambda e, ys_=ys_, j=j, hf=hf, pd=pd: e.copy(out=ys_[:, j, hf * 512:(hf + 1) * 512], in_=psb[pd]),
                             reads=[bps[pd]], writes=[bys])
                    else:
                        P.op("dve", lambda e, ys_=ys_, j=j, hf=hf, pd=pd: e.tensor_copy(out=ys_[:, j, hf * 512:(hf + 1) * 512], in_=psb[pd]),
                             reads=[bps[pd]], writes=[bys])
            P.dma("act", Ys[tq * 256:(tq + 1) * 256, :].rearrange("(j p) d -> p j d", p=128), ys_, reads=[bys], writes=[Buf()])
        P.barrier()
        st["off"] = mark_e
        g2 = {}
        for row in ((0, 1) if ntok > SEQ else (0,)):
            gt = T([128, D], F32)
            bg = Buf()
            load_mod(l, row, 5, gt, bg)
            g2[row] = (gt, bg)
        if final:
            fg = T([128, D], F32)
            bfg = Buf()
            P.dma("sync", fg, final_g.partition_broadcast(128), writes=[bfg])
        ylos = Rot(3, [128, D], F32)
        yhis = Rot(3, [128, D], F32)
        hts = Rot(3, [128, D], F32)
        tmps = Rot(2, [128, D], F32)
        outs = []

        def cb_load(i):
            ylo, bylo = ylos.next()
            yhi, byhi = yhis.next()
            for (yt_, byt, idx) in ((ylo, bylo, sloi), (yhi, byhi, shii)):
                P.op("pool", lambda e, yt_=yt_, idx=idx: e.indirect_dma_start(
                    out=yt_, out_offset=None, in_=Ys[:, :], in_offset=bass.IndirectOffsetOnAxis(ap=idx[:, i:i + 1], axis=0),
                    bounds_check=breg(e, nsl - 1), oob_is_err=False), reads=[bR], writes=[byt], dma=True)
            ht, bh = hts.next()
            P.dma("sync", ht, Hm[i * 128:(i + 1) * 128, :], reads=[bHm], writes=[bh])
            return ylo, bylo, yhi, byhi, ht, bh

        def cb_comp(i, ylo, bylo, yhi, byhi, ht, bh):
            row = 0 if i < 32 else 1
            gt, bg = g2[row]
            P.op("dve", lambda e: e.tensor_scalar(out=ylo, in0=ylo, scalar1=clo[:, i:i + 1], scalar2=None, op0=ALU.mult),
                 reads=[bylo, bR], writes=[bylo])
            P.op("dve", lambda e: e.scalar_tensor_tensor(out=ylo, in0=yhi, scalar=chi[:, i:i + 1], in1=ylo,
                                                         op0=ALU.mult, op1=ALU.add), reads=[bylo, byhi, bR], writes=[bylo])
            P.op("dve", lambda e: e.tensor_tensor(out=ylo, in0=ylo, in1=gt, op=ALU.mult), reads=[bylo, bg], writes=[bylo])
            P.op("dve", lambda e: e.tensor_tensor(out=ht, in0=ylo, in1=ht, op=ALU.add),
                 reads=[bylo, bh], writes=[bh])
            if not final:
                P.dma("act", Hout[i * 128:(i + 1) * 128, :], ht, reads=[bh], writes=[bHout])
                if debug:
                    dbg_ops.append(P.dma("act", dbg["d_h1"][i * 128:(i + 1) * 128, :], ht, reads=[bh]))
            else:
                tmp, btmp = tmps.next()
                rs, bs = norm_mod(ht, bh, None, None, None, None, tmp, btmp, None, None)
                P.op("dve", lambda e: e.scalar_tensor_tensor(
                    out=tmp, in0=ht, scalar=rs, in1=fg, op0=ALU.mult, op1=ALU.mult), reads=[bh, bs, bfg], writes=[btmp])
                outs.append(P.dma("act", out[i * 128:(i + 1) * 128, :], tmp, reads=[btmp]))

        pend = [cb_load(0), cb_load(1)]
        for i in range(NTl):
            if i + 2 < NTl:
                pend.append(cb_load(i + 2))
            cb_comp(i, *pend.pop(0))
        return outs

    Vcs, bV, keep = layer0_A()
    layer0_B(Vcs, bV, keep)
    if debug:
        new_phase()
        cwd = T([128, NT, NE], F32)
        bb = Buf()
        P.dma("sync", cwd, CW.rearrange("(n p) e -> p n e", p=128), reads=[bCW], writes=[bb])
        dbg_ops.append(P.dma("sync", dbg["d_cw0"].rearrange("(n p) e -> p n e", p=128), cwd, reads=[bb]))
    finals = []
    if stop_after != "mix0":
        moe_sparse(0, NTOK, H1, bH1, False)
    if stop_after not in ("mix0", "moe0"):
        pk, keep1 = layer1_A()
        layer1_B(pk, keep1)
        if debug:
            new_phase()
            cwd = T([128, 32, NE], F32)
            bb = Buf()
            P.dma("sync", cwd, CW[0:SEQ, :].rearrange("(n p) e -> p n e", p=128), reads=[bCW], writes=[bb])
            dbg_ops.append(P.dma("sync", dbg["d_cw1"][0:SEQ, :].rearrange("(n p) e -> p n e", p=128), cwd, reads=[bb]))
        if stop_after != "mix1":
            finals = moe_sparse(1, SEQ, None, None, True)
    P.finalize(finals + dbg_ops)
    return nc


_NC = {}


def kernel(**inputs):
    inp = {k: np.asarray(v) for k, v in inputs.items()}
    c = _consts()
    if "nc" not in _NC:
        _NC["nc"] = build(debug=False)
    nc = _NC["nc"]
    shared = {}
    for k in ("ada_w", "ada_b", "norm_mix_g", "norm_ffn_g", "router_w", "router_b",
              "moe_w_gate", "moe_w_up", "moe_w_down", "final_g"):
        shared[k] = np.ascontiguousarray(inp[k], dtype=np.float32)
    for k in ("even_w_in", "even_conv_w", "even_w_out", "odd_w_in", "odd_pool_w", "odd_pool_scale",
              "odd_sink", "odd_w_out"):
        shared[k] = np.ascontiguousarray(inp[k][0], dtype=np.float32)
    for k in ("dftN", "dftC", "dftD", "rope", "poolB", "amask", "tauc", "pidx"):
        shared[k] = c[k]
    nb = inp["x"].shape[0]
    in_maps = []
    for b in range(nb):
        m = dict(shared)
        m["x"] = np.ascontiguousarray(inp["x"][b], dtype=np.float32)
        m["ctx"] = np.ascontiguousarray(inp["ctx"][b], dtype=np.float32)
        m["c2"] = np.ascontiguousarray(np.stack([inp["c"][b], inp["c_ctx"]], 0), dtype=np.float32)
        in_maps.append(m)
    res = run_bass_kernel_spmd(nc, in_maps, core_ids=list(range(nb)))
    return np.stack([np.asarray(r["out"], dtype=np.float32) for r in res.results], 0)
```

```python
import numpy as np
import ml_dtypes
import concourse.bass as bass
import concourse.mybir as mybir
from concourse.bass_utils import run_bass_kernel_spmd

F32 = mybir.dt.float32
BF16 = mybir.dt.bfloat16
AF = mybir.ActivationFunctionType
ALU = mybir.AluOpType
AX = mybir.AxisListType

D = 1024
SEQ = 4096
LCTX = 256
NTOK = SEQ + LCTX
NT = NTOK // 128
NE = 16
EPS = 1e-6
BIG = 1.0e4


class Buf:
    __slots__ = ("name", "last_w", "readers")

    def __init__(self, name=""):
        self.name = name
        self.last_w = None
        self.readers = []


class Op:
    __slots__ = ("eng", "emit", "deps", "is_dma", "signal", "sem", "val")

    def __init__(self, eng, emit, is_dma):
        self.eng = eng
        self.emit = emit
        self.is_dma = is_dma
        self.deps = []
        self.signal = False
        self.sem = None
        self.val = None


ENGS = ("sync", "act", "dve", "pool", "pe")
NDMA_SEMS = {"sync": 16, "act": 8, "pool": 32}


class Prog:
    def __init__(self, nc):
        self.nc = nc
        self.ops = {e: [] for e in ENGS}
        self.ctx = []
        self.last_compute = {}
        self.dma_since = []
        self.pending = {}

    def enter(self, cm):
        v = cm.__enter__()
        self.ctx.append(cm)
        return v

    def sbuf(self, name, shape, dt):
        return self.enter(self.nc.sbuf_tensor(name, list(shape), dt))

    def psum(self, name, shape, dt):
        return self.enter(self.nc.psum_tensor(name, list(shape), dt))

    def close(self):
        for cm in reversed(self.ctx):
            cm.__exit__(None, None, None)
        self.ctx = []

    def op(self, eng, emit, reads=(), writes=(), dma=False):
        o = Op(eng, emit, dma)
        deps = {}
        for b in reads:
            if b.last_w is not None:
                deps[id(b.last_w)] = (b.last_w, True)
        for b in writes:
            if b.last_w is not None and id(b.last_w) not in deps:
                deps[id(b.last_w)] = (b.last_w, False)
            for r in b.readers:
                if id(r) not in deps:
                    deps[id(r)] = (r, False)
        if eng in self.pending:
            for d in self.pending.pop(eng):
                if id(d) not in deps and not (d.eng == eng and not d.is_dma):
                    deps[id(d)] = (d, True)
        for d, raw in deps.values():
            if d is o:
                continue
            if d.eng == o.eng and not d.is_dma:
                if raw and o.eng != "pe" and not o.is_dma:
                    o.deps.append(d)
                    d.signal = True
                elif o.is_dma:
                    o.deps.append(d)
                    d.signal = True
                continue
            o.deps.append(d)
            d.signal = True
        for b in reads:
            if not dma:
                b.readers = [r for r in b.readers if r.is_dma or r.eng != eng]
            b.readers.append(o)
        for b in writes:
            b.last_w = o
            b.readers = []
        self.ops[eng].append(o)
        if dma:
            self.dma_since.append(o)
        else:
            self.last_compute[eng] = o
        return o

    def barrier(self):
        pend = list(self.last_compute.values()) + list(self.dma_since)
        self.dma_since = []
        for e in ENGS:
            self.pending[e] = list(self.pending.get(e, [])) + pend

    def dma(self, eng, out, in_, reads=(), writes=(), **kw):
        return self.op(eng, lambda e: e.dma_start(out=out, in_=in_, **kw), reads, writes, dma=True)

    def finalize(self, final_wait_ops=()):
        nc = self.nc
        for o in final_wait_ops:
            o.signal = True
        eng_sem = {e: self.enter(nc.semaphore("c_" + e)) for e in ("act", "dve", "pool", "pe")}
        dma_sems = {e: [self.enter(nc.semaphore(f"d_{e}{i}")) for i in range(n)]
                    for e, n in NDMA_SEMS.items()}
        for e in ENGS:
            cnt = 0
            dcnt = 0
            per_sem_val = {}
            prev_on_sem = {}
            for o in self.ops[e]:
                if o.is_dma:
                    pool = dma_sems[e]
                    k = dcnt % len(pool)
                    dcnt += 1
                    o.sem = pool[k]
                    per_sem_val[k] = per_sem_val.get(k, 0) + 16
                    o.val = per_sem_val[k]
                    if k in prev_on_sem:
                        o.deps.append(prev_on_sem[k])
                    prev_on_sem[k] = o
                    o.signal = True
                elif o.signal:
                    cnt += 1
                    o.sem = eng_sem[e]
                    o.val = cnt
        block = self.enter(nc.Block())
        handles = {"sync": block.sync, "act": block.scalar, "dve": block.vector,
                   "pool": block.gpsimd, "pe": block.tensor}

        def make(e):
            ops = self.ops[e]

            def body(eng):
                waited = {}
                for o in ops:
                    need = {}
                    for d in o.deps:
                        key = id(d.sem)
                        if waited.get(key, 0) >= d.val:
                            continue
                        if key not in need or need[key][1] < d.val:
                            need[key] = (d.sem, d.val)
                    for key, (s, v) in need.items():
                        eng.wait_ge(s, v)
                        waited[key] = v
                    ins = o.emit(eng)
                    if o.signal:
                        ins.then_inc(o.sem, 16 if o.is_dma else 1)
                if e == "sync":
                    for o in final_wait_ops:
                        eng.wait_ge(o.sem, o.val)
            return body

        for e in ENGS:
            if self.ops[e] or e == "sync":
                handles[e](make(e))
        self.close()


_CONST = {}


def _consts():
    if _CONST:
        return _CONST
    bf = ml_dtypes.bfloat16
    N = SEQ
    s = np.arange(N, dtype=np.int64)
    prod = (s[:, None] * s[None, :]) % N
    ang = prod.astype(np.float64) * (2 * np.pi / N)
    cosm = (np.cos(ang) / 64.0).astype(np.float32)
    nsin = (-np.sin(ang) / 64.0).astype(np.float32)
    both = np.stack([cosm, nsin], 0).reshape(2, 32, 128, 8, 512)
    dftN = np.ascontiguousarray(both.transpose(3, 2, 0, 1, 4)).reshape(8, 128, 64, 512)
    _CONST["dftN"] = dftN.astype(bf)
    del prod, ang, cosm, nsin, both, dftN
    sc = np.arange(LCTX)
    angc = ((sc[:, None] * sc[None, :]) % LCTX) * (2 * np.pi / LCTX)
    cb = np.stack([np.cos(angc) / 16.0, -np.sin(angc) / 16.0], 0).reshape(2, 2, 128, 256)
    _CONST["dftC"] = np.ascontiguousarray(cb.transpose(2, 0, 1, 3)).reshape(128, 4, 256).astype(bf)
    j = np.arange(128)
    angd = ((j[:, None] * j[None, :]) % 128) * (2 * np.pi / 128)
    _CONST["dftD"] = np.concatenate([np.cos(angd), np.sin(angd)], 1).astype(np.float32) / np.sqrt(128.0)
    _CONST["dftD"] = _CONST["dftD"].astype(bf)
    quarter = 16
    inv = 10000.0 ** (-np.arange(quarter, dtype=np.float32) / quarter)
    t = np.arange(N)
    pos = np.stack([t // 64, t % 64], 0).astype(np.float32)
    p = np.arange(128)
    d = p % 64
    axis = d // 32
    i = d % 16
    angr = pos[axis, :] * inv[i][:, None]
    _CONST["rope"] = np.stack([np.cos(angr), np.sin(angr)], 1).astype(np.float32)
    half = (d % 32) // 16
    _CONST["rot_src"] = np.where(half == 0, d + 16, d - 16)[:64]
    _CONST["rot_sign"] = np.where(half == 0, -1.0, 1.0)[:64].astype(np.float32)
    pb = np.zeros((128, 20, 128), np.float32)
    for gi, w in enumerate((2, 4, 8, 16)):
        r = w // 2
        for kind in range(5):
            if kind == 0:
                tt, ss = np.arange(128, 256), np.arange(0, 128)
                base_t = 1280
            elif kind == 1:
                tt, ss = np.arange(128, 256), np.arange(128, 256)
                base_t = 1280
            elif kind == 2:
                tt, ss = np.arange(128, 256), np.arange(256, 384)
                base_t = 1280
            elif kind == 3:
                tt, ss = np.arange(0, 128), np.arange(0, 128)
                base_t = 0
            else:
                tt, ss = np.arange(N - 128, N), np.arange(N - 128, N)
                base_t = 0
            if kind < 3:
                tt = tt + base_t
                ss = ss + base_t
            lo = np.clip(tt - r, 0, N)
            hi = np.clip(tt + r + 1, 0, N)
            cnt = (hi - lo).astype(np.float32)
            m = ((ss[:, None] >= lo[None, :]) & (ss[:, None] < hi[None, :])).astype(np.float32) / cnt[None, :]
            m = m - (ss[:, None] == tt[None, :]).astype(np.float32)
            pb[:, gi * 5 + kind, :] = m
    _CONST["poolB"] = pb.astype(bf)
    q = np.arange(128)
    am = np.zeros((128, 2, 128), np.float32)
    am[:, 0, :] = np.where(q[None, :] >= q[:, None], 0.0, -30000.0)
    am[:, 1, :] = np.where(q[None, :] <= q[:, None], 0.0, -30000.0)
    _CONST["amask"] = am
    _CONST["tauc"] = np.tile((np.arange(64, dtype=np.float32) * 256.0)[None, :], (128, 1))
    _CONST["pidx"] = np.arange(128, dtype=np.float32).reshape(128, 1)
    return _CONST


def build(debug=False, stop_after=None):
    nc = bass.Bass("TRN2", target_bir_lowering=False)

    def din(name, shape, dt=F32):
        return nc.dram_tensor(name, list(shape), dt, kind="ExternalInput").ap()

    def dscr(name, shape, dt=F32):
        return nc.dram_tensor(name, list(shape), dt, kind="Internal").ap()

    x = din("x", [SEQ, D])
    ctx = din("ctx", [LCTX, D])
    c2 = din("c2", [2, D])
    ada_w = din("ada_w", [2, D, 6 * D])
    ada_b = din("ada_b", [2, 6 * D])
    norm_mix_g = din("norm_mix_g", [2, D])
    norm_ffn_g = din("norm_ffn_g", [2, D])
    even_w_in = din("even_w_in", [D, 2048])
    even_conv_w = din("even_conv_w", [3, 512])
    even_w_out = din("even_w_out", [D, D])
    odd_w_in = din("odd_w_in", [D, 1280])
    odd_pool_w = din("odd_pool_w", [4, 128, 128])
    odd_pool_scale = din("odd_pool_scale", [512])
    odd_sink = din("odd_sink", [8])
    odd_w_out = din("odd_w_out", [D, D])
    router_w = din("router_w", [D, NE])
    router_b = din("router_b", [NE])
    moe_w_gate = din("moe_w_gate", [2, NE, D, 512])
    moe_w_up = din("moe_w_up", [2, NE, D, 512])
    moe_w_down = din("moe_w_down", [2, NE, 512, D])
    final_g = din("final_g", [D])
    dftN = din("dftN", [8, 128, 64, 512], BF16)
    dftC = din("dftC", [128, 4, 256], BF16)
    dftD = din("dftD", [128, 256], BF16)
    rope = din("rope", [128, 2, SEQ])
    poolB = din("poolB", [128, 20, 128], BF16)
    amask = din("amask", [128, 2, 128])
    tauc = din("tauc", [128, 64])
    pidx = din("pidx", [128, 1])
    out = nc.dram_tensor("out", [SEQ, D], F32, kind="ExternalOutput").ap()

    Mscr = dscr("Mscr", [2, 2, 6 * D])
    Hm = dscr("Hm", [NTOK, D])
    H1 = dscr("H1", [NTOK, D])
    ZT = dscr("ZT", [4, 128, NTOK], BF16)
    GBT = dscr("GBT", [4, 128, NTOK], BF16)
    FT = dscr("FT", [8, 128, NTOK], BF16)
    CW = dscr("CW", [NTOK, NE])
    Fd = dscr("Fd", [NTOK, D], BF16)
    NSLOT = 50 * 256
    Xs = dscr("Xs", [NSLOT, D], BF16)
    Ys = dscr("Ys", [NSLOT, D])
    WGU = [dscr(f"WGU{l}", [NE * 128, 8 * 1024], BF16) for l in range(2)]
    WD = [dscr(f"WD{l}", [NE * 128, 4 * 1024], BF16) for l in range(2)]
    bFd = Buf()
    bWGU = [Buf(), Buf()]
    bWD = [Buf(), Buf()]
    I32 = mybir.dt.int32
    bM = [Buf(), Buf()]
    bHm, bH1, bZT, bGBT, bFT, bCW = Buf(), Buf(), Buf(), Buf(), Buf(), Buf()
    dbg = {}
    if debug:
        for nm, shp in (("d_hm0", [NTOK, D]), ("d_cw0", [NTOK, NE]), ("d_h1", [NTOK, D]),
                        ("d_hm1", [NTOK, D]), ("d_cw1", [NTOK, NE]), ("d_M", [2, 2, 6 * D])):
            dbg[nm] = nc.dram_tensor(nm, shp, F32, kind="ExternalOutput").ap()

    P = Prog(nc)
    ARENA_ELEMS = 101 * 1024
    arena = P.sbuf("arena", [128, ARENA_ELEMS], BF16)
    pers = P.sbuf("pers", [128, 2048], BF16)
    st = {"off": 0, "poff": 0}

    def _carve(base, off, shape, dt):
        n = int(np.prod(shape[1:]))
        esz = 2 if dt == BF16 else 4
        nb = n * esz
        ap = base[0:shape[0], off // 2:(off + nb) // 2]
        if dt != BF16:
            ap = ap.bitcast(dt)
        if len(shape) == 3:
            ap = ap.rearrange("p (a b) -> p a b", a=shape[1], b=shape[2])
        elif len(shape) == 4:
            ap = ap.rearrange("p (a b c) -> p a b c", a=shape[1], b=shape[2], c=shape[3])
        return ap, (nb + 31) // 32 * 32

    def T(shape, dt):
        ap, nb = _carve(arena, st["off"], shape, dt)
        st["off"] += nb
        assert st["off"] <= ARENA_ELEMS * 2, st["off"]
        return ap

    def TP(shape, dt):
        ap, nb = _carve(pers, st["poff"], shape, dt)
        st["poff"] += nb
        assert st["poff"] <= 4096, st["poff"]
        return ap

    def new_phase():
        P.barrier()
        st["off"] = 0

    class Rot:
        def __init__(self, n, shape, dt):
            self.t = [T(shape, dt) for _ in range(n)]
            self.b = [Buf() for _ in range(n)]
            self.i = 0

        def next(self):
            k = self.i % len(self.t)
            self.i += 1
            return self.t[k], self.b[k]

    pairs = [P.psum(f"pp{i}", [128, 1024], F32) for i in range(4)]
    sloi_t = P.sbuf("sloi_t", [128, NT], mybir.dt.int32)
    shii_t = P.sbuf("shii_t", [128, NT], mybir.dt.int32)
    widx_t = P.sbuf("widx_t", [128, 64], mybir.dt.int32)
    psb = [pairs[i // 2][:, (i % 2) * 512:(i % 2 + 1) * 512] for i in range(8)]
    bps = [Buf() for _ in range(8)]

    def ps_bf(i):
        return psb[i].bitcast(BF16)

    ident = TP([128, 128], BF16)
    b_ident = Buf()
    epsT = TP([128, 1], F32)
    b_eps = Buf()
    P.op("pool", lambda e: e.memset(ident, 0.0), writes=[b_ident])
    P.op("pool", lambda e: e.affine_select(out=ident, in_=ident, pattern=[[-1, 128]],
                                           compare_op=ALU.not_equal, fill=1.0, base=0,
                                           channel_multiplier=1), reads=[b_ident], writes=[b_ident])
    P.op("pool", lambda e: e.memset(epsT, EPS), writes=[b_eps])
    Wr = TP([128, 8, NE], BF16)
    b_Wr = Buf()
    P.dma("pool", Wr, router_w.rearrange("(k p) n -> p k n", p=128), writes=[b_Wr])
    rbT = TP([128, NE], F32)
    b_rb = Buf()
    P.dma("sync", rbT, router_b.partition_broadcast(128), writes=[b_rb])
    stats = TP([128, 64], F32)
    stat_i = [0]

    def ada_layer(l):
        c2raw = T([128, 2, 8], F32)
        b_c2 = Buf()
        for r in range(2):
            P.dma("sync", c2raw[:, r, :], c2[r].rearrange("(p k) -> p k", k=8), writes=[b_c2])
        sT = T([128, 8, 2], F32)
        b_sT = Buf()
        P.op("act", lambda e: e.activation(out=sT.rearrange("p k r -> p r k"), in_=c2raw, func=AF.Silu),
             reads=[b_c2], writes=[b_sT])
        CW_ = 256
        wA = Rot(2, [128, 8, CW_], F32)
        adabs = Rot(2, [2, CW_], F32)
        msbs = Rot(2, [2, CW_], F32)
        awv = ada_w[l].rearrange("(p k) n -> p k n", k=8)
        for j in range(6 * D // CW_):
            cs_ = slice(j * CW_, (j + 1) * CW_)
            wt, bw = wA.next()
            P.dma("sync", wt, awv[:, :, cs_], writes=[bw])
            ab, bab = adabs.next()
            P.dma("sync", ab, ada_b[l, cs_].partition_broadcast(2), writes=[bab])
            pb_i = j % 2

            def mm(e, wt=wt, pb_i=pb_i):
                for k in range(8):
                    ins = e.matmul(psb[pb_i][0:2, 0:CW_], lhsT=sT[:, k, :], rhs=wt[:, k, :],
                                   start=(k == 0), stop=(k == 7))
                return ins
            P.op("pe", mm, reads=[b_sT, bw], writes=[bps[pb_i]])
            mb, bmb = msbs.next()
            P.op("dve", lambda e, mb=mb, ab=ab, pb_i=pb_i: e.tensor_tensor(
                out=mb[0:2, :], in0=psb[pb_i][0:2, 0:CW_], in1=ab[0:2, :], op=ALU.add),
                reads=[bps[pb_i], bab], writes=[bmb])
            P.dma("act", Mscr[l][:, cs_], mb[0:2, :], reads=[bmb], writes=[bM[l]])
            if debug:
                dbg_ops.append(P.dma("act", dbg["d_M"][l][:, cs_], mb[0:2, :], reads=[bmb]))

    def phase0():
        ada_layer(0)

    dbg_ops = []
    phase0()

    _bregs = {}

    def breg(e, v):
        if v not in _bregs:
            _bregs[v] = e.to_reg(v)
        return _bregs[v]

    def conv_jobs():
        for l in range(2):
            for ex in range(NE):
                rows = slice(ex * 128, (ex + 1) * 128)
                gv = WGU[l][rows, :].rearrange("p (k n) -> p k n", k=8)
                yield (gv[:, :, 0:512], moe_w_gate[l, ex].rearrange("(k p) n -> p k n", p=128), bWGU[l])
                yield (gv[:, :, 512:1024], moe_w_up[l, ex].rearrange("(k p) n -> p k n", p=128), bWGU[l])
                yield (WD[l][rows, :].rearrange("p (k n) -> p k n", k=4),
                       moe_w_down[l, ex].rearrange("(k p) n -> p k n", p=128), bWD[l])
    conv_it = conv_jobs()
    conv_left = [96]

    def convert_upto(total):
        convert_some(max(0, conv_left[0] - (96 - total)))

    def convert_some(n):
        for _ in range(n):
            if conv_left[0] == 0:
                return
            o_, i_, _b = next(conv_it)
            conv_left[0] -= 1
            P.dma("pool", o_, i_, writes=[Buf()])

    def load_mod(l, row, which, dst, bdst):
        P.dma("sync", dst, Mscr[l, row, which * D:(which + 1) * D].partition_broadcast(128),
              reads=[bM[l]], writes=[bdst])

    def make_GS(l, gvec, shift_i, scale_i, rows=(0, 1)):
        gt = T([128, D], F32)
        bg = Buf()
        P.dma("sync", gt, gvec.partition_broadcast(128), writes=[bg])
        res = {}
        for row in rows:
            G = T([128, D], F32)
            S = T([128, D], F32)
            bG, bS = Buf(), Buf()
            load_mod(l, row, scale_i, G, bG)
            load_mod(l, row, shift_i, S, bS)
            P.op("dve", lambda e, G=G: e.scalar_tensor_tensor(out=G, in0=G, scalar=1.0, in1=gt,
                                                               op0=ALU.add, op1=ALU.mult),
                 reads=[bG, bg], writes=[bG])
            res[row] = (G, bG, S, bS)
        return res

    def norm_mod(ht, bh, G, bG, S, bS, tmp, btmp, a_out, ba):
        c = stat_i[0] % 32
        stat_i[0] += 1
        ss = stats[:, 2 * c:2 * c + 1]
        rs = stats[:, 2 * c + 1:2 * c + 2]
        bs = Buf()
        P.op("act", lambda e: e.activation(out=tmp, in_=ht, func=AF.Square, accum_out=ss),
             reads=[bh], writes=[btmp, bs])
        P.op("act", lambda e: e.activation(out=rs, in_=ss, func=AF.Ln, bias=epsT[:, 0:1], scale=1.0 / D),
             reads=[bs, b_eps], writes=[bs])
        P.op("act", lambda e: e.activation(out=rs, in_=rs, func=AF.Exp, scale=-0.5), reads=[bs], writes=[bs])
        if G is None:
            return rs, bs
        P.op("dve", lambda e: e.scalar_tensor_tensor(out=tmp, in0=ht, scalar=rs, in1=G,
                                                     op0=ALU.mult, op1=ALU.mult),
             reads=[bh, bs, bG], writes=[btmp])
        P.op("dve", lambda e: e.tensor_tensor(out=a_out, in0=tmp, in1=S, op=ALU.add),
             reads=[btmp, bS], writes=[ba])
        return rs, bs

    def transpose8(a_bf, ba, pbank, dstT, bdst, col0):
        pv = ps_bf(pbank).rearrange("p (k c) -> p k c", k=8)

        def tr(e):
            for k in range(8):
                ins = e.transpose(out=pv[:, k, :], in_=a_bf[:, k * 128:(k + 1) * 128], identity=ident)
            return ins
        P.op("pe", tr, reads=[ba, b_ident], writes=[bps[pbank]])
        P.op("act", lambda e: e.copy(out=dstT[:, :, col0:col0 + 128], in_=pv),
             reads=[bps[pbank]], writes=[bdst])

    def src_rows(i):
        if i < 32:
            return x[i * 128:(i + 1) * 128, :]
        return ctx[(i - 32) * 128:(i - 31) * 128, :]

    def routing(psR_bank, ntile, cwt, bcw, sc, bsc, aff_ready=False):
        n = ntile
        lg = psb[psR_bank][:, 0:n * 16]
        aff = sc[:, 0:n, 0:16]
        sel = sc[:, 0:n, 16:32]
        prs = sc[:, 0:n, 32:56]
        gs = sc[:, 0:n, 56:60]
        gmx = sc[:, 0:n, 60:61]
        gmk = sc[:, 0:n, 61:65]
        msel = sc[:, 0:n, 65:81]
        m1 = sc[:, 0:n, 81:82]
        tmp = sc[:, 0:n, 82:98]
        m2 = sc[:, 0:n, 98:99]
        wsum = sc[:, 0:n, 99:100]
        rd, wr = [bps[psR_bank], bsc, b_rb], [bsc]
        if not aff_ready:
            P.op("act", lambda e: e.activation(out=aff, in_=lg.rearrange("p (n e) -> p n e", e=16), func=AF.Sigmoid),
                 reads=rd, writes=wr)
        else:
            rd = [bsc, b_rb]
        P.op("dve", lambda e: e.tensor_tensor(out=sel, in0=aff, in1=rbT.unsqueeze(1).to_broadcast([128, n, 16]),
                                              op=ALU.add), reads=rd, writes=wr)
        sel4 = sel.rearrange("p n (g k) -> p n g k", k=4)
        prs4 = prs.rearrange("p n (g k) -> p n g k", k=6)
        pi = 0
        for a in range(4):
            for b in range(a + 1, 4):
                P.op("dve", lambda e, a=a, b=b, pi=pi: e.tensor_tensor(
                    out=prs4[:, :, :, pi], in0=sel4[:, :, :, a], in1=sel4[:, :, :, b], op=ALU.add),
                    reads=[bsc], writes=wr)
                pi += 1
        P.op("dve", lambda e: e.tensor_reduce(out=gs, in_=prs4, axis=AX.X, op=ALU.max), reads=[bsc], writes=wr)
        P.op("dve", lambda e: e.tensor_reduce(out=sc[:, 0:n, 60], in_=gs, axis=AX.X, op=ALU.max), reads=[bsc], writes=wr)
        P.op("dve", lambda e: e.tensor_tensor(out=gmk, in0=gs, in1=gmx.to_broadcast([128, n, 4]), op=ALU.is_ge),
             reads=[bsc], writes=wr)
        P.op("dve", lambda e: e.tensor_scalar(out=gmk, in0=gmk, scalar1=BIG, scalar2=-BIG, op0=ALU.mult, op1=ALU.add),
             reads=[bsc], writes=wr)
        P.op("dve", lambda e: e.tensor_tensor(out=msel.rearrange("p n (g k) -> p n g k", k=4), in0=sel4,
                                              in1=gmk.unsqueeze(3).to_broadcast([128, n, 4, 4]), op=ALU.add),
             reads=[bsc], writes=wr)
        P.op("dve", lambda e: e.tensor_reduce(out=sc[:, 0:n, 81], in_=msel, axis=AX.X, op=ALU.max), reads=[bsc], writes=wr)
        P.op("dve", lambda e: e.tensor_tensor(out=tmp, in0=msel, in1=m1.to_broadcast([128, n, 16]), op=ALU.is_ge),
             reads=[bsc], writes=wr)
        P.op("dve", lambda e: e.scalar_tensor_tensor(out=tmp, in0=tmp, scalar=-BIG, in1=msel,
                                                     op0=ALU.mult, op1=ALU.add), reads=[bsc], writes=wr)
        P.op("dve", lambda e: e.tensor_reduce(out=sc[:, 0:n, 98], in_=tmp, axis=AX.X, op=ALU.max), reads=[bsc], writes=wr)
        P.op("dve", lambda e: e.tensor_tensor(out=tmp, in0=msel, in1=m2.to_broadcast([128, n, 16]), op=ALU.is_ge),
             reads=[bsc], writes=wr)
        P.op("dve", lambda e: e.tensor_tensor(out=tmp, in0=tmp, in1=aff, op=ALU.mult), reads=[bsc], writes=wr)
        P.op("dve", lambda e: e.tensor_reduce(out=sc[:, 0:n, 99], in_=tmp, axis=AX.X, op=ALU.add), reads=[bsc], writes=wr)
        P.op("dve", lambda e: e.reciprocal(out=wsum, in_=wsum), reads=[bsc], writes=wr)
        P.op("dve", lambda e: e.tensor_tensor(out=cwt[:, 0:n, :], in0=tmp, in1=wsum.to_broadcast([128, n, 16]), op=ALU.mult),
             reads=[bsc], writes=[bcw])

    def layer0_A():
        new_phase()
        Vcs = T([128, NT, 1024], BF16)
        bV = Buf()
        keep = st["off"]
        Wcs = T([128, 8, 1024], BF16)
        bWcs = Buf()
        Wconv = T([128, 8, 1536], BF16)
        bWconv = Buf()
        P.dma("pool", Wconv, even_w_in[:, 512:2048].rearrange("(k p) n -> p k n", p=128), writes=[bWconv])
        Wf = T([128, 8, 512], BF16)
        bWf = Buf()
        P.dma("pool", Wf, even_w_in[:, 0:512].rearrange("(k p) n -> p k n", p=128), writes=[bWf])
        dD = T([128, 256], BF16)
        bdD = Buf()
        P.dma("sync", dD, dftD, writes=[bdD])
        WfT = T([128, 1024], BF16)
        bWfT = Buf()
        for h in range(4):
            pv = ps_bf(0).rearrange("p (k c) -> p k c", k=8)

            def tr(e, h=h, pv=pv):
                for k in range(8):
                    ins = e.transpose(out=pv[:, k, :], in_=Wf[:, k, h * 128:(h + 1) * 128], identity=ident)
                return ins
            P.op("pe", tr, reads=[bWf, b_ident], writes=[bps[0]])
            P.op("act", lambda e: e.copy(out=WfT, in_=ps_bf(0)), reads=[bps[0]], writes=[bWfT])
            for k in range(8):
                pbk = 1 + (k % 2)
                P.op("pe", lambda e, k=k, pbk=pbk: e.matmul(psb[pbk][:, 0:256], lhsT=WfT[:, k * 128:(k + 1) * 128],
                                                            rhs=dD, start=True, stop=True),
                     reads=[bWfT, bdD], writes=[bps[pbk]])
                P.op("dve", lambda e, k=k, h=h, pbk=pbk: e.tensor_copy(
                    out=Wcs[:, k, :].rearrange("p (two hh c) -> p two hh c", two=2, hh=4)[:, :, h, :],
                    in_=psb[pbk][:, 0:256].rearrange("p (two c) -> p two c", two=2)),
                    reads=[bps[pbk]], writes=[bWcs])
        GS = make_GS(0, norm_mix_g[0], 0, 1)
        hts = Rot(3, [128, D], F32)
        tmps = Rot(2, [128, D], F32)
        abf = Rot(2, [128, D], BF16)
        aT4s = Rot(2, [128, 8, 512], BF16)
        zts = Rot(2, [128, 4, 512], BF16)
        gbs = Rot(2, [128, 4, 512], BF16)
        gcs = Rot(2, [128, 512], F32)
        def l0_stage1(i):
            row = 0 if i < 32 else 1
            G, bG, S, bS = GS[row]
            ht, bh = hts.next()
            P.dma("sync", ht, src_rows(i), writes=[bh])
            tmp, btmp = tmps.next()
            a, ba = abf.next()
            norm_mod(ht, bh, G, bG, S, bS, tmp, btmp, a, ba)
            return a, ba

        def l0_stage2(i, j, a, ba, aT4, baT4):
            transpose8(a, ba, i % 2, aT4, baT4, j * 128)
            pb0 = 2 + 2 * (i % 2)

            def mmv(e):
                for hf in range(2):
                    for k in range(8):
                        ins = e.matmul(psb[pb0 + hf], lhsT=aT4[:, k, j * 128:(j + 1) * 128],
                                       rhs=Wcs[:, k, hf * 512:(hf + 1) * 512], start=(k == 0), stop=(k == 7))
                return ins
            P.op("pe", mmv, reads=[baT4, bWcs], writes=[bps[pb0], bps[pb0 + 1]])
            P.op("act", lambda e: e.copy(out=Vcs[:, i, 0:512], in_=psb[pb0]), reads=[bps[pb0]], writes=[bV])
            P.op("dve", lambda e: e.tensor_copy(out=Vcs[:, i, 512:1024], in_=psb[pb0 + 1]), reads=[bps[pb0 + 1]], writes=[bV])

        ngroups = 9
        pend = l0_stage1(0)
        for g in range(ngroups):
            ntile = 4 if g < 8 else 2
            gw = ntile * 128
            t0 = g * 512
            aT4, baT4 = aT4s.next()
            for j in range(ntile):
                i = g * 4 + j
                nxt = l0_stage1(i + 1) if i + 1 < NT else None
                l0_stage2(i, j, pend[0], pend[1], aT4, baT4)
                pend = nxt
            zt, bz = zts.next()
            gb, bgb = gbs.next()
            for cc in range(4):
                for part in (1, 0, 2):
                    c = part * 4 + cc
                    pbk = 6 + (c % 2)

                    def mmc(e, c=c, pbk=pbk, aT4=aT4, gw=gw):
                        for k in range(8):
                            ins = e.matmul(psb[pbk][:, 0:gw], lhsT=Wconv[:, k, c * 128:(c + 1) * 128],
                                           rhs=aT4[:, k, 0:gw], start=(k == 0), stop=(k == 7))
                        return ins
                    P.op("pe", mmc, reads=[baT4, bWconv], writes=[bps[pbk]])
                    if part == 1:
                        gc, bgc = gcs.next()
                        P.op("act", lambda e, gc=gc, pbk=pbk, gw=gw: e.copy(out=gc[:, 0:gw], in_=psb[pbk][:, 0:gw]),
                             reads=[bps[pbk]], writes=[bgc])
                    elif part == 0:
                        P.op("act", lambda e, gb=gb, cc=cc, pbk=pbk, gw=gw: e.copy(out=gb[:, cc, 0:gw], in_=psb[pbk][:, 0:gw]),
                             reads=[bps[pbk]], writes=[bgb])
                    else:
                        P.op("dve", lambda e, zt=zt, cc=cc, pbk=pbk, gc=gc, gw=gw: e.tensor_tensor(
                            out=zt[:, cc, 0:gw], in0=psb[pbk][:, 0:gw], in1=gc[:, 0:gw], op=ALU.mult),
                            reads=[bps[pbk], bgc], writes=[bz])
            convert_some(3)
            P.dma("act", ZT[:, :, t0:t0 + gw].rearrange("c p t -> p c t"), zt[:, :, 0:gw], reads=[bz], writes=[bZT])
            P.dma("act", GBT[:, :, t0:t0 + gw].rearrange("c p t -> p c t"), gb[:, :, 0:gw], reads=[bgb], writes=[bGBT])
        return Vcs, bV, keep

    def mixer_tail(l, g, ntile, yT, byT, Wout, bWout, gate1, GS2, hts, tmps, fbf, fT4, bfT4, hsrc_fn, psY0, psT_bank,
                   psR_bank, cwt, bcw, rsc, brsc, dbg_hm=None, filler=None, deferred=False):
        gw = ntile * 128
        t0 = g * 512

        def stA(j):
            i = g * 4 + j
            row = 0 if i < 32 else 1

            def mmo(e):
                for hf in range(2):
                    for k in range(8):
                        ins = e.matmul(psb[psY0 + hf], lhsT=yT[:, k, j * 128:(j + 1) * 128],
                                       rhs=Wout[:, k, hf * 512:(hf + 1) * 512], start=(k == 0), stop=(k == 7))
                return ins
            P.op("pe", mmo, reads=[byT, bWout], writes=[bps[psY0], bps[psY0 + 1]])
            ht, bh = hts.next()
            hsrc, hreads = hsrc_fn(i)
            P.dma("sync", ht, hsrc, reads=hreads, writes=[bh])
            tmp, btmp = tmps.next()
            g1, bg1 = gate1[row]
            for hf in range(2):
                P.op("dve", lambda e, hf=hf: e.tensor_tensor(
                    out=tmp[:, hf * 512:(hf + 1) * 512], in0=psb[psY0 + hf], in1=g1[:, hf * 512:(hf + 1) * 512],
                    op=ALU.mult), reads=[bps[psY0 + hf], bg1], writes=[btmp])
            P.op("dve", lambda e: e.tensor_tensor(out=ht, in0=tmp, in1=ht, op=ALU.add), reads=[btmp, bh], writes=[bh])
            P.dma("pool", Hm[i * 128:(i + 1) * 128, :], ht, reads=[bh], writes=[bHm])
            if dbg_hm is not None:
                dbg_ops.append(P.dma("pool", dbg_hm[i * 128:(i + 1) * 128, :], ht, reads=[bh]))
            G, bG, S, bS = GS2[row]
            f, bf_ = fbf.next()
            norm_mod(ht, bh, G, bG, S, bS, tmp, btmp, f, bf_)
            P.dma("act", Fd[i * 128:(i + 1) * 128, :], f, reads=[bf_], writes=[Buf()])
            return f, bf_

        def stB(j, f, bf_):
            transpose8(f, bf_, psT_bank, fT4, bfT4, j * 128)

            def mmr(e):
                for k in range(8):
                    ins = e.matmul(psb[psR_bank][:, j * 16:(j + 1) * 16], lhsT=fT4[:, k, j * 128:(j + 1) * 128],
                                   rhs=Wr[:, k, :], start=(k == 0), stop=(k == 7))
                return ins
            if deferred:
                def mmr0(e):
                    for k in range(8):
                        ins = e.matmul(psb[psR_bank][:, 0:16], lhsT=fT4[:, k, j * 128:(j + 1) * 128],
                                       rhs=Wr[:, k, :], start=(k == 0), stop=(k == 7))
                    return ins
                P.op("pe", mmr0, reads=[bfT4, b_Wr], writes=[bps[psR_bank]])
                P.op("act", lambda e: e.activation(out=rsc[:, j, 0:16], in_=psb[psR_bank][:, 0:16], func=AF.Sigmoid),
                     reads=[bps[psR_bank]], writes=[brsc])
            else:
                P.op("pe", mmr, reads=[bfT4, b_Wr], writes=[bps[psR_bank]])

        def finish():
            routing(psR_bank, ntile, cwt, bcw, rsc, brsc, aff_ready=deferred)
            P.dma("act", CW[t0:t0 + gw, :].rearrange("(n p) e -> p n e", p=128), cwt[:, 0:ntile, :], reads=[bcw], writes=[bCW])

        if deferred:
            return stA, stB, finish

        pend = stA(0)
        for j in range(ntile):
            if filler is not None:
                filler(1)
            nxt = stA(j + 1) if j + 1 < ntile else None
            if filler is not None:
                filler(1)
            stB(j, pend[0], pend[1])
            pend = nxt
        finish()

    def layer0_B(Vcs, bV, keep):
        P.barrier()
        st["off"] = keep
        Wout = T([128, 8, D], BF16)
        bWout = Buf()
        P.dma("pool", Wout, even_w_out.rearrange("(k p) n -> p k n", p=128), writes=[bWout])
        cwc = T([128, 3, 4], F32)
        bcwc = Buf()
        for kk in range(3):
            P.dma("sync", cwc[:, kk, :], even_conv_w[kk].rearrange("(c p) -> p c", p=128), writes=[bcwc],
                  allow_slow_non_contiguous=True)
        dC = T([128, 4, 256], BF16)
        bdC = Buf()
        P.dma("sync", dC, dftC, writes=[bdC])
        GS2 = make_GS(0, norm_ffn_g[0], 3, 4)
        gate1 = []
        for row in range(2):
            gt = T([128, D], F32)
            bg = Buf()
            load_mod(0, row, 2, gt, bg)
            gate1.append((gt, bg))
        ring = Rot(3, [128, 8, 512], BF16)
        yTs = Rot(2, [128, 8, 512], BF16)
        zin = Rot(1, [128, 4, 514], BF16)
        gbin = Rot(1, [128, 4, 512], BF16)
        cacc = Rot(2, [128, 512], F32)
        hts = Rot(2, [128, D], F32)
        tmps = Rot(2, [128, D], F32)
        fbf = Rot(2, [128, D], BF16)
        fT4s = Rot(1, [128, 8, 512], BF16)
        cws = Rot(2, [128, 4, NE], F32)
        rsc = T([128, 4, 128], F32)
        brsc = Buf()
        def fourier_ops(g, yT, byT):
            ops_ = []
            gw_ = 512 if g < 8 else 256
            if g < 8:
                for piece in range(8):
                    def one(piece=piece):
                        rt, brt = ring.next()
                        P.dma("sync", rt, dftN[g, :, piece * 8:(piece + 1) * 8, :], writes=[brt])

                        def mmf(e):
                            for s8_ in range(8):
                                stt = piece * 8 + s8_
                                base = 0 if stt < 32 else 512
                                for h in range(4):
                                    ins = e.matmul(psb[h], lhsT=Vcs[:, stt % 32, base + h * 128: base + (h + 1) * 128],
                                                   rhs=rt[:, s8_, :], start=(stt == 0), stop=(stt == 63))
                            return ins
                        P.op("pe", mmf, reads=[brt, bV], writes=[bps[0], bps[1], bps[2], bps[3]])
                    ops_.append(one)
            else:
                def onec():
                    def mmfc(e):
                        for jj in range(4):
                            base = 0 if jj < 2 else 512
                            for h in range(4):
                                ins = e.matmul(psb[h][:, 0:256], lhsT=Vcs[:, 32 + (jj % 2), base + h * 128: base + (h + 1) * 128],
                                               rhs=dC[:, jj, :], start=(jj == 0), stop=(jj == 3))
                        return ins
                    P.op("pe", mmfc, reads=[bdC, bV], writes=[bps[0], bps[1], bps[2], bps[3]])
                ops_.append(onec)

            def evac():
                for h in range(4):
                    P.op("act", lambda e, h=h: e.copy(out=yT[:, h, 0:gw_], in_=psb[h][:, 0:gw_]), reads=[bps[h]], writes=[byT])
            ops_.append(evac)
            return ops_

        yT_next = yTs.next()
        pending_f = fourier_ops(0, *yT_next)
        for g in range(9):
            ntile = 4 if g < 8 else 2
            gw = ntile * 128
            t0 = g * 512
            yT, byT = yT_next
            while pending_f:
                pending_f.pop(0)()
            if g + 1 < 9:
                yT_next = yTs.next()
                pending_f = fourier_ops(g + 1, *yT_next)

            def filler(n, pending_f=pending_f):
                for _ in range(n):
                    if pending_f:
                        pending_f.pop(0)()
            zt, bz = zin.next()
            gb, bgb = gbin.next()
            first = g in (0, 8)
            last = g in (7, 8)
            lo = t0 - (0 if first else 1)
            hi = t0 + gw + (0 if last else 1)
            c0 = 1 if first else 0
            if first:
                P.op("pool", lambda e, zt=zt: e.memset(zt[:, :, 0:1], 0.0), writes=[bz])
            if last:
                P.op("pool", lambda e, zt=zt, gw=gw: e.memset(zt[:, :, gw + 1:gw + 2], 0.0), writes=[bz])
            P.dma("sync", zt[:, :, c0:c0 + (hi - lo)], ZT[:, :, lo:hi].rearrange("c p t -> p c t"), reads=[bZT], writes=[bz])
            P.dma("sync", gb[:, :, 0:gw], GBT[:, :, t0:t0 + gw].rearrange("c p t -> p c t"), reads=[bGBT], writes=[bgb])
            for cc in range(4):
                ac, bac = cacc.next()
                P.op("dve", lambda e, ac=ac, zt=zt, cc=cc, gw=gw: e.tensor_scalar(
                    out=ac[:, 0:gw], in0=zt[:, cc, 0:gw], scalar1=cwc[:, 0, cc:cc + 1], scalar2=None, op0=ALU.mult),
                    reads=[bz, bcwc], writes=[bac])
                for kk in (1, 2):
                    P.op("dve", lambda e, ac=ac, zt=zt, cc=cc, gw=gw, kk=kk: e.scalar_tensor_tensor(
                        out=ac[:, 0:gw], in0=zt[:, cc, kk:kk + gw], scalar=cwc[:, kk, cc:cc + 1], in1=ac[:, 0:gw],
                        op0=ALU.mult, op1=ALU.add), reads=[bz, bcwc, bac], writes=[bac])
                P.op("dve", lambda e, ac=ac, gb=gb, cc=cc, gw=gw, yT=yT: e.tensor_tensor(
                    out=yT[:, 4 + cc, 0:gw], in0=ac[:, 0:gw], in1=gb[:, cc, 0:gw], op=ALU.mult),
                    reads=[bac, bgb], writes=[byT])
            convert_some(3)
            fT4, bfT4 = fT4s.next()
            cwt, bcw = cws.next()
            mixer_tail(0, g, ntile, yT, byT, Wout, bWout, gate1, GS2, hts, tmps, fbf, fT4, bfT4,
                       lambda i: (src_rows(i), []), 4, 6, 7, cwt, bcw, rsc, brsc, dbg.get("d_hm0"), filler=filler)

    def moe(l, ntok, Hout, bHout, final):
        new_phase()
        sgs = [(0, 2048), (2048, ntok - 2048)]
        accmax = max(n for _, n in sgs) // 128
        acc = T([128, accmax, D], F32)
        bacc = [Buf() for _ in range(accmax)]
        fTs = T([128, 8, accmax * 128], BF16)
        bfTs = Buf()
        cws = T([128, accmax, NE], F32)
        bcws = Buf()
        wgs = Rot(2, [128, 8, 512], BF16)
        wus = Rot(2, [128, 8, 512], BF16)
        wds = Rot(2, [128, 4, D], BF16)
        hid = Rot(2, [128, 4, 512], BF16)
        sgt = Rot(2, [128, 512], F32)
        g2 = []
        for row in range(2):
            gt = T([128, D], F32)
            bg = Buf()
            load_mod(l, row, 5, gt, bg)
            g2.append((gt, bg))
        hts = Rot(2, [128, D], F32)
        tmps = Rot(2, [128, D], F32)
        if final:
            fg = T([128, D], F32)
            bfg = Buf()
            P.dma("sync", fg, final_g.partition_broadcast(128), writes=[bfg])
        outs = []
        for (s0, sn) in sgs:
            ntl = sn // 128
            P.dma("sync", fTs[:, :, 0:sn], FT[:, :, s0:s0 + sn].rearrange("k p t -> p k t"), reads=[bFT], writes=[bfTs])
            P.dma("sync", cws[:, 0:ntl, :], CW[s0:s0 + sn, :].rearrange("(n p) e -> p n e", p=128), reads=[bCW], writes=[bcws])
            groups = [(q, min(512, sn - q)) for q in range(0, sn, 512)]
            for ex in range(NE):
                wg, bwg = wgs.next()
                wu, bwu = wus.next()
                wd, bwd = wds.next()
                P.dma("pool", wg, moe_w_gate[l, ex].rearrange("(k p) n -> p k n", p=128), writes=[bwg])
                P.dma("pool", wu, moe_w_up[l, ex].rearrange("(k p) n -> p k n", p=128), writes=[bwu])
                P.dma("pool", wd, moe_w_down[l, ex].rearrange("(k p) n -> p k n", p=128), writes=[bwd])
                for (q0, qn) in groups:
                    hd, bhd = hid.next()
                    for jc in range(4):
                        pg, pu = (0, 1) if jc % 2 == 0 else (2, 3)

                        def mmg(e, jc=jc, pg=pg, pu=pu, wg=wg, wu=wu, q0=q0, qn=qn):
                            for (pb_, w_) in ((pg, wg), (pu, wu)):
                                for k in range(8):
                                    ins = e.matmul(psb[pb_][:, 0:qn], lhsT=w_[:, k, jc * 128:(jc + 1) * 128],
                                                   rhs=fTs[:, k, q0:q0 + qn], start=(k == 0), stop=(k == 7))
                            return ins
                        P.op("pe", mmg, reads=[bwg, bwu, bfTs], writes=[bps[pg], bps[pu]])
                        sg, bsg = sgt.next()
                        P.op("act", lambda e, sg=sg, pg=pg, qn=qn: e.activation(out=sg[:, 0:qn], in_=psb[pg][:, 0:qn], func=AF.Silu),
                             reads=[bps[pg]], writes=[bsg])
                        P.op("dve", lambda e, sg=sg, pu=pu, hd=hd, jc=jc, qn=qn: e.tensor_tensor(
                            out=hd[:, jc, 0:qn], in0=psb[pu][:, 0:qn], in1=sg[:, 0:qn], op=ALU.mult),
                            reads=[bps[pu], bsg], writes=[bhd])
                    for jt in range(qn // 128):
                        tl = (q0 // 128) + jt
                        for hf in range(2):
                            pd = 4 + ((jt * 2 + hf) % 4)

                            def mmd(e, pd=pd, hd=hd, jt=jt, hf=hf, wd=wd):
                                for jc in range(4):
                                    ins = e.matmul(psb[pd], lhsT=hd[:, jc, jt * 128:(jt + 1) * 128],
                                                   rhs=wd[:, jc, hf * 512:(hf + 1) * 512], start=(jc == 0), stop=(jc == 3))
                                return ins
                            P.op("pe", mmd, reads=[bhd, bwd], writes=[bps[pd]])
                            av = acc[:, tl, hf * 512:(hf + 1) * 512]
                            if ex == 0:
                                P.op("dve", lambda e, av=av, pd=pd, tl=tl, ex=ex: e.tensor_scalar(
                                    out=av, in0=psb[pd], scalar1=cws[:, tl, ex:ex + 1], scalar2=None, op0=ALU.mult),
                                    reads=[bps[pd], bcws], writes=[bacc[tl]])
                            else:
                                P.op("dve", lambda e, av=av, pd=pd, tl=tl, ex=ex: e.scalar_tensor_tensor(
                                    out=av, in0=psb[pd], scalar=cws[:, tl, ex:ex + 1], in1=av,
                                    op0=ALU.mult, op1=ALU.add), reads=[bps[pd], bcws, bacc[tl]], writes=[bacc[tl]])
            for tl in range(ntl):
                i = s0 // 128 + tl
                row = 0 if i < 32 else 1
                ht, bh = hts.next()
                P.dma("sync", ht, Hm[i * 128:(i + 1) * 128, :], reads=[bHm], writes=[bh])
                gt, bg = g2[row]
                P.op("pool", lambda e, tl=tl, gt=gt: e.tensor_tensor(out=acc[:, tl, :], in0=acc[:, tl, :], in1=gt, op=ALU.mult),
                     reads=[bacc[tl], bg], writes=[bacc[tl]])
                P.op("pool", lambda e, tl=tl, ht=ht: e.tensor_tensor(out=ht, in0=acc[:, tl, :], in1=ht, op=ALU.add),
                     reads=[bacc[tl], bh], writes=[bh])
                if not final:
                    P.dma("pool", Hout[i * 128:(i + 1) * 128, :], ht, reads=[bh], writes=[bHout])
                    if debug:
                        dbg_ops.append(P.dma("pool", dbg["d_h1"][i * 128:(i + 1) * 128, :], ht, reads=[bh]))
                else:
                    tmp, btmp = tmps.next()
                    rs, bs = norm_mod(ht, bh, None, None, None, None, tmp, btmp, None, None)
                    P.op("dve", lambda e, tmp=tmp, ht=ht, rs=rs: e.scalar_tensor_tensor(
                        out=tmp, in0=ht, scalar=rs, in1=fg, op0=ALU.mult, op1=ALU.mult),
                        reads=[bh, bs, bfg], writes=[btmp])
                    outs.append(P.dma("act", out[i * 128:(i + 1) * 128, :], tmp, reads=[btmp]))
        return outs


    def layer1_A():
        new_phase()
        Up = T([128, 32, 512], BF16)
        qT = T([128, 4, SEQ], BF16)
        kT = T([128, 2, NTOK], BF16)
        Vt = T([128, NT, 128], BF16)
        bUp, bqT, bkT, bVt = Buf(), Buf(), Buf(), Buf()
        keep = st["off"]
        Wp = T([128, 8, 512], BF16)
        Wq = T([128, 8, 512], BF16)
        Wqr = T([128, 8, 512], BF16)
        Wk = T([128, 8, 256], BF16)
        Wkr = T([128, 8, 256], BF16)
        Wv = T([128, 8, 128], BF16)
        bWp, bWq, bWqr, bWk, bWkr, bWv = Buf(), Buf(), Buf(), Buf(), Buf(), Buf()
        wv = odd_w_in.rearrange("(k p) n -> p k n", p=128)
        P.dma("pool", Wp, wv[:, :, 0:512], writes=[bWp])
        P.dma("pool", Wq, wv[:, :, 512:1024], writes=[bWq])
        for a in range(4):
            P.dma("pool", Wk[:, :, a * 64:(a + 1) * 64], wv[:, :, 1024 + (a // 2) * 64:1024 + (a // 2 + 1) * 64], writes=[bWk])
        P.dma("pool", Wv, wv[:, :, 1152:1280], writes=[bWv])
        for (src, bsrc, dst, bdst, nblk) in ((Wq, bWq, Wqr, bWqr, 16), (Wk, bWk, Wkr, bWkr, 8)):
            sv = src.rearrange("p k (blk two i) -> p (k blk) two i", two=2, i=16)
            dv = dst.rearrange("p k (blk two i) -> p (k blk) two i", two=2, i=16)
            P.op("dve", lambda e, sv=sv, dv=dv: e.tensor_scalar(out=dv[:, :, 0, :], in0=sv[:, :, 1, :], scalar1=-1.0,
                                                              scalar2=None, op0=ALU.mult), reads=[bsrc], writes=[bdst])
            P.op("dve", lambda e, sv=sv, dv=dv: e.tensor_copy(out=dv[:, :, 1, :], in_=sv[:, :, 0, :]), reads=[bsrc], writes=[bdst])
        GS = make_GS(1, norm_mix_g[1], 0, 1)
        hts = Rot(2, [128, D], F32)
        tmps = Rot(2, [128, D], F32)
        abf = Rot(2, [128, D], BF16)
        aT4s = Rot(2, [128, 8, 512], BF16)
        ropes = Rot(2, [128, 2, 512], F32)
        rt1 = Rot(2, [128, 512], F32)
        rt2 = Rot(2, [128, 512], F32)
        fm_i = [0]

        def l1_stage1(i):
            row = 0 if i < 32 else 1
            G, bG, S, bS = GS[row]
            ht, bh = hts.next()
            P.dma("sync", ht, H1[i * 128:(i + 1) * 128, :], reads=[bH1], writes=[bh])
            tmp, btmp = tmps.next()
            a, ba = abf.next()
            norm_mod(ht, bh, G, bG, S, bS, tmp, btmp, a, ba)
            return a, ba

        def l1_stage2(i, j, a, ba, aT4, baT4):
            transpose8(a, ba, i % 2, aT4, baT4, j * 128)
            if i < 32:
                def mmp(e):
                    for k in range(8):
                        ins = e.matmul(psb[2], lhsT=aT4[:, k, j * 128:(j + 1) * 128], rhs=Wp[:, k, :],
                                       start=(k == 0), stop=(k == 7))
                    return ins
                P.op("pe", mmp, reads=[baT4, bWp], writes=[bps[2]])
                P.op("act", lambda e: e.copy(out=Up[:, i, :], in_=psb[2]), reads=[bps[2]], writes=[bUp])

            def mmvv(e):
                for k in range(8):
                    ins = e.matmul(psb[3][:, 0:128], lhsT=aT4[:, k, j * 128:(j + 1) * 128], rhs=Wv[:, k, :],
                                   start=(k == 0), stop=(k == 7))
                return ins
            P.op("pe", mmvv, reads=[baT4, bWv], writes=[bps[3]])
            P.op("dve", lambda e: e.tensor_copy(out=Vt[:, i, :], in_=psb[3][:, 0:128]), reads=[bps[3]], writes=[bVt])

        pend1 = [l1_stage1(0)]
        for g in range(9):
            ntile = 4 if g < 8 else 2
            gw = ntile * 128
            t0 = g * 512
            aT4, baT4 = aT4s.next()
            for j in range(ntile):
                i = g * 4 + j
                nxt1 = l1_stage1(i + 1) if i + 1 < NT else None
                l1_stage2(i, j, pend1[0][0], pend1[0][1], aT4, baT4)
                pend1[0] = nxt1
            convert_some(3)
            if g < 8:
                rp, brp = ropes.next()
                P.dma("sync", rp, rope[:, :, t0:t0 + 512], writes=[brp])
            jobs = [("k", jj) for jj in range(2)]
            if g < 8:
                jobs = [("q", cc) for cc in range(4)] + jobs
            for (kind, cc) in jobs:
                W, bW, Wr_, bWr_ = (Wq, bWq, Wqr, bWqr) if kind == "q" else (Wk, bWk, Wkr, bWkr)
                pA, pB = (4, 5) if fm_i[0] % 2 == 0 else (6, 7)
                fm_i[0] += 1
                rot = g < 8

                def mmq(e, W=W, Wr_=Wr_, cc=cc, pA=pA, pB=pB, aT4=aT4, gw=gw, rot=rot):
                    for (pb_, w_) in ((pA, W), (pB, Wr_)) if rot else ((pA, W),):
                        for k in range(8):
                            ins = e.matmul(psb[pb_][:, 0:gw], lhsT=w_[:, k, cc * 128:(cc + 1) * 128], rhs=aT4[:, k, 0:gw],
                                           start=(k == 0), stop=(k == 7))
                    return ins
                P.op("pe", mmq, reads=[baT4, bW, bWr_], writes=[bps[pA], bps[pB]])
                dst = qT[:, cc, t0:t0 + gw] if kind == "q" else kT[:, cc, t0:t0 + gw]
                bdst = bqT if kind == "q" else bkT
                if rot:
                    t1, bt1 = rt1.next()
                    t2, bt2 = rt2.next()
                    P.op("dve", lambda e, t1=t1, pA=pA, rp=rp: e.tensor_tensor(out=t1, in0=psb[pA], in1=rp[:, 0, :], op=ALU.mult),
                         reads=[bps[pA], brp], writes=[bt1])
                    P.op("dve", lambda e, t2=t2, pB=pB, rp=rp: e.tensor_tensor(out=t2, in0=psb[pB], in1=rp[:, 1, :], op=ALU.mult),
                         reads=[bps[pB], brp], writes=[bt2])
                    P.op("dve", lambda e, t1=t1, t2=t2, dst=dst: e.tensor_tensor(out=dst, in0=t1, in1=t2, op=ALU.add),
                         reads=[bt1, bt2], writes=[bdst])
                else:
                    P.op("act", lambda e, dst=dst, pA=pA, gw=gw: e.copy(out=dst, in_=psb[pA][:, 0:gw]), reads=[bps[pA]], writes=[bdst])
        return (Up, bUp, qT, bqT, kT, bkT, Vt, bVt), keep

    def layer1_B(pk, keep):
        Up, bUp, qT, bqT, kT, bkT, Vt, bVt = pk
        P.barrier()
        st["off"] = keep
        Wout = T([128, 8, D], BF16)
        bWout = Buf()
        P.dma("pool", Wout, odd_w_out.rearrange("(k p) n -> p k n", p=128), writes=[bWout])
        pB = T([128, 20, 128], BF16)
        bpB = Buf()
        P.dma("sync", pB, poolB, writes=[bpB])
        PW = T([128, 4, 128], BF16)
        bPW = Buf()
        P.dma("pool", PW, odd_pool_w.rearrange("g c d -> c g d"), writes=[bPW])
        psc = T([128, 4], F32)
        bpsc = Buf()
        P.dma("sync", psc, odd_pool_scale.rearrange("(g p) -> p g", p=128), writes=[bpsc], allow_slow_non_contiguous=True)
        mk = T([128, 2, 128], BF16)
        bmk = Buf()
        P.dma("pool", mk, amask, writes=[bmk])
        sk8 = T([128, 8], F32)
        bsk8 = Buf()
        P.dma("sync", sk8, odd_sink.partition_broadcast(128), writes=[bsk8])
        P.op("dve", lambda e: e.tensor_scalar(out=sk8, in0=sk8, scalar1=8.0, scalar2=None, op0=ALU.mult), reads=[bsk8], writes=[bsk8])
        GS2 = make_GS(1, norm_ffn_g[1], 3, 4, rows=(0,))
        gt = T([128, D], F32)
        bg = Buf()
        load_mod(1, 0, 2, gt, bg)
        gate1 = {0: (gt, bg)}
        yTs = Rot(2, [128, 8, 512], BF16)
        Ps = Rot(3, [128, 640], BF16)
        PTs = Rot(3, [128, 5, 128], BF16)
        pTs = Rot(1, [128, 4, 128], BF16)
        Osb = Rot(2, [128, 8, 64], BF16)
        st8 = Rot(2, [128, 8, 8], F32)
        st8h = [[Buf() for _ in range(8)] for _ in range(2)]
        st8b = [Buf(), Buf()]
        hts = Rot(2, [128, D], F32)
        tmps = Rot(2, [128, D], F32)
        fbf = Rot(2, [128, D], BF16)
        fT4s = Rot(1, [128, 8, 512], BF16)
        cws = Rot(2, [128, 4, NE], F32)
        rsc = T([128, 4, 128], F32)
        brsc = Buf()
        hcount = [0]
        def attn_block(i, cs, yT, byT):
            wb = [b_ for b_ in (i - 1, i, i + 1) if 0 <= b_ < 32]
            nw = len(wb) * 128
            w0 = wb[0] * 128
            c_lo = 512 - nw
            s8, bs8 = st8.next()
            osb, bosb = Osb.next()
            pO = psb[6].rearrange("p (h d) -> p h d", h=8)
            slot8 = (st8.i - 1) % 2
            hb = st8h[slot8]

            def st_qk(h):
                c, half, kvj = h // 2, h % 2, h // 4
                pr = slice(half * 64, (half + 1) * 64)
                pi = hcount[0] % 2
                hcount[0] += 1
                pair = pairs[pi]
                bpair = [bps[2 * pi], bps[2 * pi + 1]]

                def mmqk(e, pair=pair, pr=pr, c=c, kvj=kvj):
                    qv = qT[pr, c, i * 128:(i + 1) * 128]
                    e.matmul(pair[:, c_lo:512], lhsT=qv, rhs=kT[pr, kvj, w0:w0 + nw], start=True, stop=False,
                             skip_group_check=True)
                    if wb[0] == i - 1:
                        e.matmul(pair[:, c_lo:c_lo + 128], lhsT=ident, rhs=mk[:, 0, :], start=False, stop=False,
                                 skip_group_check=True)
                    if wb[-1] == i + 1:
                        e.matmul(pair[:, 384:512], lhsT=ident, rhs=mk[:, 1, :], start=False, stop=False,
                                 skip_group_check=True)
                    ins = e.matmul(pair[:, 512:768], lhsT=qv, rhs=kT[pr, kvj, SEQ:SEQ + 256], start=True, stop=True,
                                   skip_group_check=True)
                    return ins
                P.op("pe", mmqk, reads=[bqT, bkT, bmk, b_ident], writes=bpair)
                sv = pair[:, c_lo:768]
                nk = 768 - c_lo
                P.op("dve", lambda e: e.tensor_reduce(out=s8[:, 0, h:h + 1], in_=sv, axis=AX.X, op=ALU.max),
                     reads=bpair, writes=[hb[h]])
                P.op("dve", lambda e: e.tensor_scalar(out=s8[:, 2, h:h + 1], in0=s8[:, 0, h:h + 1], scalar1=sk8[:, h:h + 1],
                                                      scalar2=-0.125, op0=ALU.max, op1=ALU.mult),
                     reads=[hb[h], bsk8], writes=[hb[h]])
                Pt, bPt = Ps.next()
                P.op("act", lambda e: e.activation(
                    out=Pt[:, 0:nk], in_=sv, func=AF.Exp, bias=s8[:, 2, h:h + 1], scale=0.125, accum_out=s8[:, 3, h:h + 1]),
                    reads=bpair + [hb[h]], writes=[bPt, hb[h]])
                return (h, kvj, Pt, bPt, nk)

            def st_tr(stt):
                h, kvj, Pt, bPt, nk = stt
                nch = nk // 128
                ptb = 4 + (h % 2)
                ptv = ps_bf(ptb)[:, 0:640].rearrange("p (n q) -> p n q", n=5)

                def trp(e):
                    for n_ in range(nch):
                        ins = e.transpose(out=ptv[:, n_, :], in_=Pt[:, n_ * 128:(n_ + 1) * 128], identity=ident)
                    return ins
                P.op("pe", trp, reads=[bPt, b_ident], writes=[bps[ptb]])
                PT_, bPT = PTs.next()
                if h % 2 == 0:
                    P.op("act", lambda e: e.copy(out=PT_[:, 0:nch, :], in_=ptv[:, 0:nch, :]), reads=[bps[ptb]], writes=[bPT])
                else:
                    P.op("dve", lambda e: e.tensor_copy(out=PT_[:, 0:nch, :], in_=ptv[:, 0:nch, :]), reads=[bps[ptb]], writes=[bPT])
                return (h, kvj, PT_, bPT)

            def st_pv(stt):
                h, kvj, PT_, bPT = stt
                vt = wb + [32, 33]

                def mmpv(e):
                    for n_, tl in enumerate(vt):
                        ins = e.matmul(pO[:, h, :], lhsT=PT_[:, n_, :], rhs=Vt[:, tl, kvj * 64:(kvj + 1) * 64],
                                       start=(n_ == 0), stop=(n_ == len(vt) - 1))
                    return ins
                P.op("pe", mmpv, reads=[bPT, bVt], writes=[bps[6]])

            q_st = {0: st_qk(0)}
            t_st = {}
            for h in range(8):
                if h + 1 < 8:
                    q_st[h + 1] = st_qk(h + 1)
                t_st[h] = st_tr(q_st[h])
                if h >= 1:
                    st_pv(t_st[h - 1])
            st_pv(t_st[7])
            bs8 = st8b[slot8]
            P.op("dve", lambda e, s8=s8: e.scalar_tensor_tensor(out=s8[:, 4, :], in0=s8[:, 2, :], scalar=8.0, in1=sk8,
                                                                op0=ALU.mult, op1=ALU.add),
                 reads=[bs8, bsk8] + hb, writes=[bs8])
            P.op("act", lambda e, s8=s8: e.activation(out=s8[:, 4, :], in_=s8[:, 4, :], func=AF.Exp, scale=0.125),
                 reads=[bs8], writes=[bs8])
            P.op("dve", lambda e, s8=s8: e.tensor_tensor(out=s8[:, 5, :], in0=s8[:, 4, :], in1=s8[:, 3, :], op=ALU.add),
                 reads=[bs8] + hb, writes=[bs8])
            P.op("dve", lambda e, s8=s8: e.reciprocal(out=s8[:, 5, :], in_=s8[:, 5, :]), reads=[bs8], writes=[bs8])
            P.op("dve", lambda e, s8=s8, osb=osb: e.tensor_tensor(out=osb, in0=pO, in1=s8[:, 5, :].unsqueeze(2).to_broadcast([128, 8, 64]),
                                                                  op=ALU.mult), reads=[bps[6], bs8], writes=[bosb])
            otv = ps_bf(7)[:, 0:512].rearrange("p (n q) -> p n q", n=4)
            of = osb.rearrange("p h d -> p (h d)")

            def post():
                def tro(e):
                    for n_ in range(4):
                        ins = e.transpose(out=otv[:, n_, :], in_=of[:, n_ * 128:(n_ + 1) * 128], identity=ident)
                    return ins
                P.op("pe", tro, reads=[bosb, b_ident], writes=[bps[7]])
                P.op("act", lambda e: e.copy(out=yT[:, 4:8, cs], in_=otv), reads=[bps[7]], writes=[byT])
            return post

        prev_post = [None]
        prev_yT = None

        tail = [None]
        tail_f = {}

        def do_tail(g, yT, byT):
            convert_some(3)
            fT4, bfT4 = fT4s.next()
            cwt, bcw = cws.next()
            return mixer_tail(1, g, 4, yT, byT, Wout, bWout, gate1, GS2, hts, tmps, fbf, fT4, bfT4,
                              lambda i: (H1[i * 128:(i + 1) * 128, :], [bH1]), 0, 2, 3, cwt, bcw, rsc, brsc,
                              dbg.get("d_hm1"), deferred=True)

        for g in range(8):
            yT, byT = yTs.next()
            for j in range(4):
                i = g * 4 + j
                cs = slice(j * 128, (j + 1) * 128)
                srcs = []
                if i > 0:
                    srcs.append((i - 1, 0))
                srcs.append((i, 3 if i == 0 else (4 if i == 31 else 1)))
                if i < 31:
                    srcs.append((i + 1, 2))
                pv4 = psb[7].rearrange("p (g t) -> p g t", g=4)

                def mmpool(e, srcs=srcs):
                    for gi in range(4):
                        for n_, (s_, kind) in enumerate(srcs):
                            ins = e.matmul(pv4[:, gi, :], lhsT=Up[:, s_, gi * 128:(gi + 1) * 128], rhs=pB[:, gi * 5 + kind, :],
                                           start=(n_ == 0), stop=(n_ == len(srcs) - 1))
                    return ins
                P.op("pe", mmpool, reads=[bUp, bpB], writes=[bps[7]])
                pT_, bpT = pTs.next()
                P.op("act", lambda e, pT_=pT_: e.copy(out=pT_, in_=pv4), reads=[bps[7]], writes=[bpT])

                def mmpw(e, pT_=pT_):
                    for gi in range(4):
                        ins = e.matmul(pv4[:, gi, :], lhsT=PW[:, gi, :], rhs=pT_[:, gi, :], start=True, stop=True)
                    return ins
                P.op("pe", mmpw, reads=[bpT, bPW], writes=[bps[7]])
                P.op("dve", lambda e, yT=yT, cs=cs: e.tensor_tensor(out=yT[:, 0:4, cs], in0=pv4,
                                                                   in1=psc.unsqueeze(2).to_broadcast([128, 4, 128]), op=ALU.mult),
                     reads=[bps[7], bpsc], writes=[byT])
                if prev_post[0] is not None:
                    prev_post[0]()
                    prev_post[0] = None
                if g > 0:
                    if j == 0:
                        if tail[0] is not None:
                            tA, tB, tF = tail[0]
                            tB(3, *tail_f[3])
                            tF()
                        tail[0] = do_tail(g - 1, prev_yT[0], prev_yT[1])
                    tA, tB, tF = tail[0]
                    tail_f[j] = tA(j)
                    if j > 0:
                        tB(j - 1, *tail_f[j - 1])
                prev_post[0] = attn_block(i, cs, yT, byT)
            prev_yT = (yT, byT)
        prev_post[0]()
        if tail[0] is not None:
            tA, tB, tF = tail[0]
            tB(3, *tail_f[3])
            tF()
        tA, tB, tF = do_tail(7, prev_yT[0], prev_yT[1])
        pend = tA(0)
        for j in range(4):
            nxt = tA(j + 1) if j + 1 < 4 else None
            tB(j, *pend)
            pend = nxt
        tF()


    def moe_sparse(l, ntok, Hout, bHout, final):
        convert_upto(48 * (l + 1))
        new_phase()
        NTl = ntok // 128
        NTS = (2 * ntok) // 256 + NE - 1
        nsl = NTS * 256
        cwa = T([128, NTl, NE], F32)
        mka = T([128, NTl, NE], F32)
        mkb = T([128, NTl, NE], BF16)
        sp = T([128, NTl, NE], F32)
        v1 = T([128, NTl, NE], F32)
        v2 = T([128, NTl, NE], F32)
        tm = T([128, NTl, NE], F32)
        bR = Buf()
        Lt = T([128, 128], BF16)
        ones = T([128, 128], BF16)
        sm = T([128, 16, NE], F32)
        smi = T([128, NE], I32)
        slo = T([128, NTl], F32)
        shi = T([128, NTl], F32)
        clo = T([128, NTl], F32)
        chi = T([128, NTl], F32)
        sloi = sloi_t[:, 0:NTl]
        shii = shii_t[:, 0:NTl]
        tau = T([128, 64], F32)
        pix = T([128, 1], F32)
        cmpt = T([128, NTS, NE], F32)
        ecn = T([128, NTS], F32)
        widx = widx_t[:, 0:NTS]
        P.dma("sync", cwa, CW[0:ntok, :].rearrange("(n p) e -> p n e", p=128), reads=[bCW], writes=[bR])
        P.dma("sync", tau, tauc, writes=[bR])
        P.dma("sync", pix, pidx, writes=[bR])
        P.op("pool", lambda e: e.memset(ones, 1.0), writes=[bR])
        P.op("pool", lambda e: e.memset(Lt, 1.0), writes=[bR])
        P.op("pool", lambda e: e.affine_select(out=Lt, in_=Lt, pattern=[[1, 128]], compare_op=ALU.is_gt, fill=0.0,
                                               base=0, channel_multiplier=-1), reads=[bR], writes=[bR])
        P.op("dve", lambda e: e.tensor_scalar(out=mka, in0=cwa, scalar1=0.0, scalar2=None, op0=ALU.is_gt), reads=[bR], writes=[bR])
        P.op("dve", lambda e: e.tensor_copy(out=mkb, in_=mka), reads=[bR], writes=[bR])
        half = (NTl + 1) // 2

        def mmpos(e):
            for i in range(NTl):
                pbk, col = (0, i) if i < half else (1, i - half)
                o_ = psb[pbk][:, col * 16:(col + 1) * 16]
                for i2 in range(i):
                    e.matmul(o_, lhsT=ones, rhs=mkb[:, i2, :], start=(i2 == 0), stop=False)
                e.matmul(o_, lhsT=Lt, rhs=mkb[:, i, :], start=(i == 0), stop=True)
            for i in range(NTl):
                ins = e.matmul(psb[2][:, 0:16], lhsT=ones, rhs=mkb[:, i, :], start=(i == 0), stop=(i == NTl - 1))
            return ins
        P.op("pe", mmpos, reads=[bR], writes=[bps[0], bps[1], bps[2]])
        rd = [bR, bps[0], bps[1], bps[2]]
        P.op("dve", lambda e: e.tensor_scalar(out=sm[:, 0, :], in0=psb[2][:, 0:16], scalar1=255.0, scalar2=None, op0=ALU.add),
             reads=rd, writes=[bR])
        P.op("dve", lambda e: e.tensor_copy(out=smi, in_=sm[:, 0, :]), reads=[bR], writes=[bR])
        P.op("dve", lambda e: e.tensor_scalar(out=smi, in0=smi, scalar1=8, scalar2=8, op0=ALU.arith_shift_right,
                                              op1=ALU.logical_shift_left), reads=[bR], writes=[bR])
        P.op("dve", lambda e: e.tensor_copy(out=sm[:, 1, :], in_=smi), reads=[bR], writes=[bR])
        P.op("dve", lambda e: e.tensor_copy(out=sm[:, 2, :], in_=sm[:, 1, :]), reads=[bR], writes=[bR])
        cur, nxt = 2, 3
        for d_ in (1, 2, 4, 8):
            P.op("dve", lambda e, cur=cur, nxt=nxt, d_=d_: e.tensor_tensor(
                out=sm[:, nxt, d_:16], in0=sm[:, cur, d_:16], in1=sm[:, cur, 0:16 - d_], op=ALU.add), reads=[bR], writes=[bR])
            P.op("dve", lambda e, cur=cur, nxt=nxt, d_=d_: e.tensor_copy(out=sm[:, nxt, 0:d_], in_=sm[:, cur, 0:d_]),
                 reads=[bR], writes=[bR])
            cur, nxt = nxt, cur
        P.op("dve", lambda e, cur=cur: e.tensor_tensor(out=sm[:, 4, :], in0=sm[:, cur, :], in1=sm[:, 1, :], op=ALU.subtract),
             reads=[bR], writes=[bR])
        start = sm[:, 4, :]
        P.op("dve", lambda e: e.tensor_tensor(out=sp[:, 0:half, :], in0=psb[0][:, 0:half * 16].rearrange("p (n e) -> p n e", e=16),
                                              in1=start.unsqueeze(1).to_broadcast([128, half, NE]), op=ALU.add), reads=rd, writes=[bR])
        P.op("dve", lambda e: e.tensor_tensor(out=sp[:, half:NTl, :],
                                              in0=psb[1][:, 0:(NTl - half) * 16].rearrange("p (n e) -> p n e", e=16),
                                              in1=start.unsqueeze(1).to_broadcast([128, NTl - half, NE]), op=ALU.add), reads=rd, writes=[bR])
        P.op("dve", lambda e: e.tensor_scalar(out=tm, in0=mka, scalar1=-1.0e6, scalar2=1.0e6, op0=ALU.mult, op1=ALU.add),
             reads=[bR], writes=[bR])
        P.op("dve", lambda e: e.tensor_tensor(out=sp, in0=sp, in1=mka, op=ALU.mult), reads=[bR], writes=[bR])
        P.op("dve", lambda e: e.tensor_tensor(out=v1, in0=sp, in1=tm, op=ALU.add), reads=[bR], writes=[bR])
        P.op("dve", lambda e: e.tensor_tensor(out=v2, in0=sp, in1=tm, op=ALU.subtract), reads=[bR], writes=[bR])
        P.op("dve", lambda e: e.tensor_reduce(out=slo, in_=v1, axis=AX.X, op=ALU.min), reads=[bR], writes=[bR])
        P.op("dve", lambda e: e.tensor_reduce(out=shi, in_=v2, axis=AX.X, op=ALU.max), reads=[bR], writes=[bR])
        P.op("dve", lambda e: e.tensor_tensor(out=v1, in0=v1, in1=slo.unsqueeze(2).to_broadcast([128, NTl, NE]), op=ALU.is_equal),
             reads=[bR], writes=[bR])
        P.op("dve", lambda e: e.tensor_tensor(out=v1, in0=v1, in1=cwa, op=ALU.mult), reads=[bR], writes=[bR])
        P.op("dve", lambda e: e.tensor_reduce(out=clo, in_=v1, axis=AX.X, op=ALU.add), reads=[bR], writes=[bR])
        P.op("dve", lambda e: e.tensor_tensor(out=v2, in0=v2, in1=shi.unsqueeze(2).to_broadcast([128, NTl, NE]), op=ALU.is_equal),
             reads=[bR], writes=[bR])
        P.op("dve", lambda e: e.tensor_tensor(out=v2, in0=v2, in1=cwa, op=ALU.mult), reads=[bR], writes=[bR])
        P.op("dve", lambda e: e.tensor_reduce(out=chi, in_=v2, axis=AX.X, op=ALU.add), reads=[bR], writes=[bR])
        P.op("dve", lambda e: e.tensor_copy(out=sloi, in_=slo), reads=[bR], writes=[bR])
        P.op("dve", lambda e: e.tensor_copy(out=shii, in_=shi), reads=[bR], writes=[bR])
        P.op("dve", lambda e: e.tensor_tensor(out=cmpt, in0=start.unsqueeze(1).to_broadcast([128, NTS, NE]),
                                              in1=tau[:, 0:NTS].unsqueeze(2).to_broadcast([128, NTS, NE]), op=ALU.is_le),
             reads=[bR], writes=[bR])
        P.op("dve", lambda e: e.tensor_reduce(out=ecn, in_=cmpt, axis=AX.X, op=ALU.add), reads=[bR], writes=[bR])
        P.op("dve", lambda e: e.tensor_scalar(out=ecn, in0=ecn, scalar1=128.0, scalar2=-128.0, op0=ALU.mult, op1=ALU.add),
             reads=[bR], writes=[bR])
        P.op("dve", lambda e: e.tensor_scalar(out=ecn, in0=ecn, scalar1=pix[:, 0:1], scalar2=None, op0=ALU.add), reads=[bR], writes=[bR])
        P.op("dve", lambda e: e.tensor_copy(out=widx, in_=ecn), reads=[bR], writes=[bR])
        if l == 0:
            ada_layer(1)
        fts = Rot(3, [128, D], BF16)
        for i in range(NTl):
            ft, bft = fts.next()
            P.dma("sync", ft, Fd[i * 128:(i + 1) * 128, :], writes=[bft])
            for idx in (sloi, shii):
                P.op("pool", lambda e, ft=ft, idx=idx, i=i: e.indirect_dma_start(
                    out=Xs[:, :], out_offset=bass.IndirectOffsetOnAxis(ap=idx[:, i:i + 1], axis=0),
                    in_=ft, in_offset=None, bounds_check=breg(e, nsl - 1), oob_is_err=False),
                    reads=[bft, bR], writes=[Buf()], dma=True)
        P.barrier()
        mark_e = st["off"]
        xss = Rot(3, [128, 2, D], BF16)
        xTs = Rot(3, [128, 8, 256], BF16)
        nwb = 3
        wgus = Rot(nwb, [128, 8, 1024], BF16)
        wds = Rot(nwb, [128, 4, 1024], BF16)
        hids = Rot(2, [128, 4, 256], BF16)
        sgs_ = Rot(2, [128, 256], F32)
        yss = Rot(3, [128, 2, D], F32)
        for tq in range(NTS):
            xs_, bxs = xss.next()
            P.dma("sync", xs_, Xs[tq * 256:(tq + 1) * 256, :].rearrange("(j p) d -> p j d", p=128), writes=[bxs])
            wgu, bwgu = wgus.next()
            wd, bwd = wds.next()
            P.op("pool", lambda e, wgu=wgu, tq=tq: e.indirect_dma_start(
                out=wgu.rearrange("p k n -> p (k n)"), out_offset=None, in_=WGU[l][:, :],
                in_offset=bass.IndirectOffsetOnAxis(ap=widx[:, tq:tq + 1], axis=0), bounds_check=breg(e, NE * 128 - 1), oob_is_err=False),
                reads=[bR], writes=[bwgu], dma=True)
            P.op("pool", lambda e, wd=wd, tq=tq: e.indirect_dma_start(
                out=wd.rearrange("p k n -> p (k n)"), out_offset=None, in_=WD[l][:, :],
                in_offset=bass.IndirectOffsetOnAxis(ap=widx[:, tq:tq + 1], axis=0), bounds_check=breg(e, NE * 128 - 1), oob_is_err=False),
                reads=[bR], writes=[bwd], dma=True)
            xT, bxT = xTs.next()
            for j in range(2):
                transpose8(xs_[:, j, :], bxs, j, xT, bxT, j * 128)
            hd, bhd = hids.next()
            for jc in range(4):
                pg, pu = (2, 3) if jc % 2 == 0 else (4, 5)

                def mmg(e, jc=jc, pg=pg, pu=pu, wgu=wgu, xT=xT):
                    for (pb_, off) in ((pg, 0), (pu, 512)):
                        for k in range(8):
                            ins = e.matmul(psb[pb_][:, 0:256], lhsT=wgu[:, k, off + jc * 128: off + (jc + 1) * 128],
                                           rhs=xT[:, k, :], start=(k == 0), stop=(k == 7))
                    return ins
                P.op("pe", mmg, reads=[bwgu, bxT], writes=[bps[pg], bps[pu]])
                sg, bsg = sgs_.next()
                P.op("act", lambda e, sg=sg, pg=pg: e.activation(out=sg, in_=psb[pg][:, 0:256], func=AF.Silu),
                     reads=[bps[pg]], writes=[bsg])
                P.op("dve", lambda e, sg=sg, pu=pu, hd=hd, jc=jc: e.tensor_tensor(out=hd[:, jc, :], in0=psb[pu][:, 0:256], in1=sg, op=ALU.mult),
                     reads=[bps[pu], bsg], writes=[bhd])
            ys_, bys = yss.next()
            for j in range(2):
                for hf in range(2):
                    pd = 6 + ((j * 2 + hf) % 2)

                    def mmd(e, pd=pd, hd=hd, j=j, hf=hf, wd=wd):
                        for jc in range(4):
                            ins = e.matmul(psb[pd], lhsT=hd[:, jc, j * 128:(j + 1) * 128],
                                           rhs=wd[:, jc, hf * 512:(hf + 1) * 512], start=(jc == 0), stop=(jc == 3))
                        return ins
                    P.op("pe", mmd, reads=[bhd, bwd], writes=[bps[pd]])
                    if hf == 0:
                        P.op("act", lambda e, ys_=ys_, j=j, hf=hf, pd=pd: e.copy(out=ys_[:, j, hf * 512:(hf + 1) * 512], in_=psb[pd]),
                             reads=[bps[pd]], writes=[bys])
                    else:
                        P.op("dve", lambda e, ys_=ys_, j=j, hf=hf, pd=pd: e.tensor_copy(out=ys_[:, j, hf * 512:(hf + 1) * 512], in_=psb[pd]),
                             reads=[bps[pd]], writes=[bys])
            P.dma("act", Ys[tq * 256:(tq + 1) * 256, :].rearrange("(j p) d -> p j d", p=128), ys_, reads=[bys], writes=[Buf()])
        P.barrier()
        st["off"] = mark_e
        g2 = {}
        for row in ((0, 1) if ntok > SEQ else (0,)):
            gt = T([128, D], F32)
            bg = Buf()
            load_mod(l, row, 5, gt, bg)
            g2[row] = (gt, bg)
        if final:
            fg = T([128, D], F32)
            bfg = Buf()
            P.dma("sync", fg, final_g.partition_broadcast(128), writes=[bfg])
        ylos = Rot(3, [128, D], F32)
        yhis = Rot(3, [128, D], F32)
        hts = Rot(3, [128, D], F32)
        tmps = Rot(2, [128, D], F32)
        outs = []

        def cb_load(i):
            ylo, bylo = ylos.next()
            yhi, byhi = yhis.next()
            for (yt_, byt, idx) in ((ylo, bylo, sloi), (yhi, byhi, shii)):
                P.op("pool", lambda e, yt_=yt_, idx=idx: e.indirect_dma_start(
                    out=yt_, out_offset=None, in_=Ys[:, :], in_offset=bass.IndirectOffsetOnAxis(ap=idx[:, i:i + 1], axis=0),
                    bounds_check=breg(e, nsl - 1), oob_is_err=False), reads=[bR], writes=[byt], dma=True)
            ht, bh = hts.next()
            P.dma("sync", ht, Hm[i * 128:(i + 1) * 128, :], reads=[bHm], writes=[bh])
            return ylo, bylo, yhi, byhi, ht, bh

        def cb_comp(i, ylo, bylo, yhi, byhi, ht, bh):
            row = 0 if i < 32 else 1
            gt, bg = g2[row]
            P.op("dve", lambda e: e.tensor_scalar(out=ylo, in0=ylo, scalar1=clo[:, i:i + 1], scalar2=None, op0=ALU.mult),
                 reads=[bylo, bR], writes=[bylo])
            P.op("dve", lambda e: e.scalar_tensor_tensor(out=ylo, in0=yhi, scalar=chi[:, i:i + 1], in1=ylo,
                                                         op0=ALU.mult, op1=ALU.add), reads=[bylo, byhi, bR], writes=[bylo])
            P.op("dve", lambda e: e.tensor_tensor(out=ylo, in0=ylo, in1=gt, op=ALU.mult), reads=[bylo, bg], writes=[bylo])
            P.op("dve", lambda e: e.tensor_tensor(out=ht, in0=ylo, in1=ht, op=ALU.add),
                 reads=[bylo, bh], writes=[bh])
            if not final:
                P.dma("act", Hout[i * 128:(i + 1) * 128, :], ht, reads=[bh], writes=[bHout])
                if debug:
                    dbg_ops.append(P.dma("act", dbg["d_h1"][i * 128:(i + 1) * 128, :], ht, reads=[bh]))
            else:
                tmp, btmp = tmps.next()
                rs, bs = norm_mod(ht, bh, None, None, None, None, tmp, btmp, None, None)
                P.op("dve", lambda e: e.scalar_tensor_tensor(
                    out=tmp, in0=ht, scalar=rs, in1=fg, op0=ALU.mult, op1=ALU.mult), reads=[bh, bs, bfg], writes=[btmp])
                outs.append(P.dma("act", out[i * 128:(i + 1) * 128, :], tmp, reads=[btmp]))

        pend = [cb_load(0), cb_load(1)]
        for i in range(NTl):
            if i + 2 < NTl:
                pend.append(cb_load(i + 2))
            cb_comp(i, *pend.pop(0))
        return outs

    Vcs, bV, keep = layer0_A()
    layer0_B(Vcs, bV, keep)
    if debug:
        new_phase()
        cwd = T([128, NT, NE], F32)
        bb = Buf()
        P.dma("sync", cwd, CW.rearrange("(n p) e -> p n e", p=128), reads=[bCW], writes=[bb])
        dbg_ops.append(P.dma("sync", dbg["d_cw0"].rearrange("(n p) e -> p n e", p=128), cwd, reads=[bb]))
    finals = []
    if stop_after != "mix0":
        moe_sparse(0, NTOK, H1, bH1, False)
    if stop_after not in ("mix0", "moe0"):
        pk, keep1 = layer1_A()
        layer1_B(pk, keep1)
        if debug:
            new_phase()
            cwd = T([128, 32, NE], F32)
            bb = Buf()
            P.dma("sync", cwd, CW[0:SEQ, :].rearrange("(n p) e -> p n e", p=128), reads=[bCW], writes=[bb])
            dbg_ops.append(P.dma("sync", dbg["d_cw1"][0:SEQ, :].rearrange("(n p) e -> p n e", p=128), cwd, reads=[bb]))
        if stop_after != "mix1":
            finals = moe_sparse(1, SEQ, None, None, True)
    P.finalize(finals + dbg_ops)
    return nc


_NC = {}


def kernel(**inputs):
    inp = {k: np.asarray(v) for k, v in inputs.items()}
    c = _consts()
    if "nc" not in _NC:
        _NC["nc"] = build(debug=False)
    nc = _NC["nc"]
    shared = {}
    for k in ("ada_w", "ada_b", "norm_mix_g", "norm_ffn_g", "router_w", "router_b",
              "moe_w_gate", "moe_w_up", "moe_w_down", "final_g"):
        shared[k] = np.ascontiguousarray(inp[k], dtype=np.float32)
    for k in ("even_w_in", "even_conv_w", "even_w_out", "odd_w_in", "odd_pool_w", "odd_pool_scale",
              "odd_sink", "odd_w_out"):
        shared[k] = np.ascontiguousarray(inp[k][0], dtype=np.float32)
    for k in ("dftN", "dftC", "dftD", "rope", "poolB", "amask", "tauc", "pidx"):
        shared[k] = c[k]
    nb = inp["x"].shape[0]
    in_maps = []
    for b in range(nb):
        m = dict(shared)
        m["x"] = np.ascontiguousarray(inp["x"][b], dtype=np.float32)
        m["ctx"] = np.ascontiguousarray(inp["ctx"][b], dtype=np.float32)
        m["c2"] = np.ascontiguousarray(np.stack([inp["c"][b], inp["c_ctx"]], 0), dtype=np.float32)
        in_maps.append(m)
    res = run_bass_kernel_spmd(nc, in_maps, core_ids=list(range(nb)))
    return np.stack([np.asarray(r["out"], dtype=np.float32) for r in res.results], 0)
```

```python
import numpy as np
import ml_dtypes
import concourse.bass as bass
import concourse.mybir as mybir
from concourse.bass_utils import run_bass_kernel_spmd

F32 = mybir.dt.float32
BF16 = mybir.dt.bfloat16
AF = mybir.ActivationFunctionType
ALU = mybir.AluOpType
AX = mybir.AxisListType

D = 1024
SEQ = 4096
LCTX = 256
NTOK = SEQ + LCTX
NT = NTOK // 128
NE = 16
EPS = 1e-6
BIG = 1.0e4


class Buf:
    __slots__ = ("name", "last_w", "readers")

    def __init__(self, name=""):
        self.name = name
        self.last_w = None
        self.readers = []


class Op:
    __slots__ = ("eng", "emit", "deps", "is_dma", "signal", "sem", "val")

    def __init__(self, eng, emit, is_dma):
        self.eng = eng
        self.emit = emit
        self.is_dma = is_dma
        self.deps = []
        self.signal = False
        self.sem = None
        self.val = None


ENGS = ("sync", "act", "dve", "pool", "pe")
NDMA_SEMS = {"sync": 16, "act": 8, "pool": 32}


class Prog:
    def __init__(self, nc):
        self.nc = nc
        self.ops = {e: [] for e in ENGS}
        self.ctx = []
        self.last_compute = {}
        self.dma_since = []
        self.pending = {}

    def enter(self, cm):
        v = cm.__enter__()
        self.ctx.append(cm)
        return v

    def sbuf(self, name, shape, dt):
        return self.enter(self.nc.sbuf_tensor(name, list(shape), dt))

    def psum(self, name, shape, dt):
        return self.enter(self.nc.psum_tensor(name, list(shape), dt))

    def close(self):
        for cm in reversed(self.ctx):
            cm.__exit__(None, None, None)
        self.ctx = []

    def op(self, eng, emit, reads=(), writes=(), dma=False):
        o = Op(eng, emit, dma)
        deps = {}
        for b in reads:
            if b.last_w is not None:
                deps[id(b.last_w)] = (b.last_w, True)
        for b in writes:
            if b.last_w is not None and id(b.last_w) not in deps:
                deps[id(b.last_w)] = (b.last_w, False)
            for r in b.readers:
                if id(r) not in deps:
                    deps[id(r)] = (r, False)
        if eng in self.pending:
            for d in self.pending.pop(eng):
                if id(d) not in deps and not (d.eng == eng and not d.is_dma):
                    deps[id(d)] = (d, True)
        for d, raw in deps.values():
            if d is o:
                continue
            if d.eng == o.eng and not d.is_dma:
                if raw and o.eng != "pe" and not o.is_dma:
                    o.deps.append(d)
                    d.signal = True
                elif o.is_dma:
                    o.deps.append(d)
                    d.signal = True
                continue
            o.deps.append(d)
            d.signal = True
        for b in reads:
            if not dma:
                b.readers = [r for r in b.readers if r.is_dma or r.eng != eng]
            b.readers.append(o)
        for b in writes:
            b.last_w = o
            b.readers = []
        self.ops[eng].append(o)
        if dma:
            self.dma_since.append(o)
        else:
            self.last_compute[eng] = o
        return o

    def barrier(self):
        pend = list(self.last_compute.values()) + list(self.dma_since)
        self.dma_since = []
        for e in ENGS:
            self.pending[e] = list(self.pending.get(e, [])) + pend

    def dma(self, eng, out, in_, reads=(), writes=(), **kw):
        return self.op(eng, lambda e: e.dma_start(out=out, in_=in_, **kw), reads, writes, dma=True)

    def finalize(self, final_wait_ops=()):
        nc = self.nc
        for o in final_wait_ops:
            o.signal = True
        eng_sem = {e: self.enter(nc.semaphore("c_" + e)) for e in ("act", "dve", "pool", "pe")}
        dma_sems = {e: [self.enter(nc.semaphore(f"d_{e}{i}")) for i in range(n)]
                    for e, n in NDMA_SEMS.items()}
        for e in ENGS:
            cnt = 0
            dcnt = 0
            per_sem_val = {}
            prev_on_sem = {}
            for o in self.ops[e]:
                if o.is_dma:
                    pool = dma_sems[e]
                    k = dcnt % len(pool)
                    dcnt += 1
                    o.sem = pool[k]
                    per_sem_val[k] = per_sem_val.get(k, 0) + 16
                    o.val = per_sem_val[k]
                    if k in prev_on_sem:
                        o.deps.append(prev_on_sem[k])
                    prev_on_sem[k] = o
                    o.signal = True
                elif o.signal:
                    cnt += 1
                    o.sem = eng_sem[e]
                    o.val = cnt
        block = self.enter(nc.Block())
        handles = {"sync": block.sync, "act": block.scalar, "dve": block.vector,
                   "pool": block.gpsimd, "pe": block.tensor}

        def make(e):
            ops = self.ops[e]

            def body(eng):
                waited = {}
                for o in ops:
                    need = {}
                    for d in o.deps:
                        key = id(d.sem)
                        if waited.get(key, 0) >= d.val:
                            continue
                        if key not in need or need[key][1] < d.val:
                            need[key] = (d.sem, d.val)
                    for key, (s, v) in need.items():
                        eng.wait_ge(s, v)
                        waited[key] = v
                    ins = o.emit(eng)
                    if o.signal:
                        ins.then_inc(o.sem, 16 if o.is_dma else 1)
                if e == "sync":
                    for o in final_wait_ops:
                        eng.wait_ge(o.sem, o.val)
            return body

        for e in ENGS:
            if self.ops[e] or e == "sync":
                handles[e](make(e))
        self.close()


_CONST = {}


def _consts():
    if _CONST:
        return _CONST
    bf = ml_dtypes.bfloat16
    N = SEQ
    s = np.arange(N, dtype=np.int64)
    prod = (s[:, None] * s[None, :]) % N
    ang = prod.astype(np.float64) * (2 * np.pi / N)
    cosm = (np.cos(ang) / 64.0).astype(np.float32)
    nsin = (-np.sin(ang) / 64.0).astype(np.float32)
    both = np.stack([cosm, nsin], 0).reshape(2, 32, 128, 8, 512)
    dftN = np.ascontiguousarray(both.transpose(3, 2, 0, 1, 4)).reshape(8, 128, 64, 512)
    _CONST["dftN"] = dftN.astype(bf)
    del prod, ang, cosm, nsin, both, dftN
    sc = np.arange(LCTX)
    angc = ((sc[:, None] * sc[None, :]) % LCTX) * (2 * np.pi / LCTX)
    cb = np.stack([np.cos(angc) / 16.0, -np.sin(angc) / 16.0], 0).reshape(2, 2, 128, 256)
    _CONST["dftC"] = np.ascontiguousarray(cb.transpose(2, 0, 1, 3)).reshape(128, 4, 256).astype(bf)
    j = np.arange(128)
    angd = ((j[:, None] * j[None, :]) % 128) * (2 * np.pi / 128)
    _CONST["dftD"] = np.concatenate([np.cos(angd), np.sin(angd)], 1).astype(np.float32) / np.sqrt(128.0)
    _CONST["dftD"] = _CONST["dftD"].astype(bf)
    quarter = 16
    inv = 10000.0 ** (-np.arange(quarter, dtype=np.float32) / quarter)
    t = np.arange(N)
    pos = np.stack([t // 64, t % 64], 0).astype(np.float32)
    p = np.arange(128)
    d = p % 64
    axis = d // 32
    i = d % 16
    angr = pos[axis, :] * inv[i][:, None]
    _CONST["rope"] = np.stack([np.cos(angr), np.sin(angr)], 1).astype(np.float32)
    half = (d % 32) // 16
    _CONST["rot_src"] = np.where(half == 0, d + 16, d - 16)[:64]
    _CONST["rot_sign"] = np.where(half == 0, -1.0, 1.0)[:64].astype(np.float32)
    pb = np.zeros((128, 20, 128), np.float32)
    for gi, w in enumerate((2, 4, 8, 16)):
        r = w // 2
        for kind in range(5):
            if kind == 0:
                tt, ss = np.arange(128, 256), np.arange(0, 128)
                base_t = 1280
            elif kind == 1:
                tt, ss = np.arange(128, 256), np.arange(128, 256)
                base_t = 1280
            elif kind == 2:
                tt, ss = np.arange(128, 256), np.arange(256, 384)
                base_t = 1280
            elif kind == 3:
                tt, ss = np.arange(0, 128), np.arange(0, 128)
                base_t = 0
            else:
                tt, ss = np.arange(N - 128, N), np.arange(N - 128, N)
                base_t = 0
            if kind < 3:
                tt = tt + base_t
                ss = ss + base_t
            lo = np.clip(tt - r, 0, N)
            hi = np.clip(tt + r + 1, 0, N)
            cnt = (hi - lo).astype(np.float32)
            m = ((ss[:, None] >= lo[None, :]) & (ss[:, None] < hi[None, :])).astype(np.float32) / cnt[None, :]
            m = m - (ss[:, None] == tt[None, :]).astype(np.float32)
            pb[:, gi * 5 + kind, :] = m
    _CONST["poolB"] = pb.astype(bf)
    q = np.arange(128)
    am = np.zeros((128, 2, 128), np.float32)
    am[:, 0, :] = np.where(q[None, :] >= q[:, None], 0.0, -30000.0)
    am[:, 1, :] = np.where(q[None, :] <= q[:, None], 0.0, -30000.0)
    _CONST["amask"] = am
    _CONST["tauc"] = np.tile((np.arange(64, dtype=np.float32) * 256.0)[None, :], (128, 1))
    _CONST["pidx"] = np.arange(128, dtype=np.float32).reshape(128, 1)
    return _CONST


def build(debug=False, stop_after=None):
    nc = bass.Bass("TRN2", target_bir_lowering=False)

    def din(name, shape, dt=F32):
        return nc.dram_tensor(name, list(shape), dt, kind="ExternalInput").ap()

    def dscr(name, shape, dt=F32):
        return nc.dram_tensor(name, list(shape), dt, kind="Internal").ap()

    x = din("x", [SEQ, D])
    ctx = din("ctx", [LCTX, D])
    c2 = din("c2", [2, D])
    ada_w = din("ada_w", [2, D, 6 * D])
    ada_b = din("ada_b", [2, 6 * D])
    norm_mix_g = din("norm_mix_g", [2, D])
    norm_ffn_g = din("norm_ffn_g", [2, D])
    even_w_in = din("even_w_in", [D, 2048])
    even_conv_w = din("even_conv_w", [3, 512])
    even_w_out = din("even_w_out", [D, D])
    odd_w_in = din("odd_w_in", [D, 1280])
    odd_pool_w = din("odd_pool_w", [4, 128, 128])
    odd_pool_scale = din("odd_pool_scale", [512])
    odd_sink = din("odd_sink", [8])
    odd_w_out = din("odd_w_out", [D, D])
    router_w = din("router_w", [D, NE])
    router_b = din("router_b", [NE])
    moe_w_gate = din("moe_w_gate", [2, NE, D, 512])
    moe_w_up = din("moe_w_up", [2, NE, D, 512])
    moe_w_down = din("moe_w_down", [2, NE, 512, D])
    final_g = din("final_g", [D])
    dftN = din("dftN", [8, 128, 64, 512], BF16)
    dftC = din("dftC", [128, 4, 256], BF16)
    dftD = din("dftD", [128, 256], BF16)
    rope = din("rope", [128, 2, SEQ])
    poolB = din("poolB", [128, 20, 128], BF16)
    amask = din("amask", [128, 2, 128])
    tauc = din("tauc", [128, 64])
    pidx = din("pidx", [128, 1])
    out = nc.dram_tensor("out", [SEQ, D], F32, kind="ExternalOutput").ap()

    Mscr = dscr("Mscr", [2, 2, 6 * D])
    Hm = dscr("Hm", [NTOK, D])
    H1 = dscr("H1", [NTOK, D])
    ZT = dscr("ZT", [4, 128, NTOK], BF16)
    GBT = dscr("GBT", [4, 128, NTOK], BF16)
    FT = dscr("FT", [8, 128, NTOK], BF16)
    CW = dscr("CW", [NTOK, NE])
    Fd = dscr("Fd", [NTOK, D], BF16)
    NSLOT = 50 * 256
    Xs = dscr("Xs", [NSLOT, D], BF16)
    Ys = dscr("Ys", [NSLOT, D])
    WGU = [dscr(f"WGU{l}", [NE * 128, 8 * 1024], BF16) for l in range(2)]
    WD = [dscr(f"WD{l}", [NE * 128, 4 * 1024], BF16) for l in range(2)]
    bFd = Buf()
    bWGU = [Buf(), Buf()]
    bWD = [Buf(), Buf()]
    I32 = mybir.dt.int32
    bM = [Buf(), Buf()]
    bHm, bH1, bZT, bGBT, bFT, bCW = Buf(), Buf(), Buf(), Buf(), Buf(), Buf()
    dbg = {}
    if debug:
        for nm, shp in (("d_hm0", [NTOK, D]), ("d_cw0", [NTOK, NE]), ("d_h1", [NTOK, D]),
                        ("d_hm1", [NTOK, D]), ("d_cw1", [NTOK, NE]), ("d_M", [2, 2, 6 * D])):
            dbg[nm] = nc.dram_tensor(nm, shp, F32, kind="ExternalOutput").ap()

    P = Prog(nc)
    ARENA_ELEMS = 101 * 1024
    arena = P.sbuf("arena", [128, ARENA_ELEMS], BF16)
    pers = P.sbuf("pers", [128, 2048], BF16)
    st = {"off": 0, "poff": 0}

    def _carve(base, off, shape, dt):
        n = int(np.prod(shape[1:]))
        esz = 2 if dt == BF16 else 4
        nb = n * esz
        ap = base[0:shape[0], off // 2:(off + nb) // 2]
        if dt != BF16:
            ap = ap.bitcast(dt)
        if len(shape) == 3:
            ap = ap.rearrange("p (a b) -> p a b", a=shape[1], b=shape[2])
        elif len(shape) == 4:
            ap = ap.rearrange("p (a b c) -> p a b c", a=shape[1], b=shape[2], c=shape[3])
        return ap, (nb + 31) // 32 * 32

    def T(shape, dt):
        ap, nb = _carve(arena, st["off"], shape, dt)
        st["off"] += nb
        assert st["off"] <= ARENA_ELEMS * 2, st["off"]
        return ap

    def TP(shape, dt):
        ap, nb = _carve(pers, st["poff"], shape, dt)
        st["poff"] += nb
        assert st["poff"] <= 4096, st["poff"]
        return ap

    def new_phase():
        P.barrier()
        st["off"] = 0

    class Rot:
        def __init__(self, n, shape, dt):
            self.t = [T(shape, dt) for _ in range(n)]
            self.b = [Buf() for _ in range(n)]
            self.i = 0

        def next(self):
            k = self.i % len(self.t)
            self.i += 1
            return self.t[k], self.b[k]

    pairs = [P.psum(f"pp{i}", [128, 1024], F32) for i in range(4)]
    sloi_t = P.sbuf("sloi_t", [128, NT], mybir.dt.int32)
    shii_t = P.sbuf("shii_t", [128, NT], mybir.dt.int32)
    widx_t = P.sbuf("widx_t", [128, 64], mybir.dt.int32)
    psb = [pairs[i // 2][:, (i % 2) * 512:(i % 2 + 1) * 512] for i in range(8)]
    bps = [Buf() for _ in range(8)]

    def ps_bf(i):
        return psb[i].bitcast(BF16)

    ident = TP([128, 128], BF16)
    b_ident = Buf()
    epsT = TP([128, 1], F32)
    b_eps = Buf()
    P.op("pool", lambda e: e.memset(ident, 0.0), writes=[b_ident])
    P.op("pool", lambda e: e.affine_select(out=ident, in_=ident, pattern=[[-1, 128]],
                                           compare_op=ALU.not_equal, fill=1.0, base=0,
                                           channel_multiplier=1), reads=[b_ident], writes=[b_ident])
    P.op("pool", lambda e: e.memset(epsT, EPS), writes=[b_eps])
    Wr = TP([128, 8, NE], BF16)
    b_Wr = Buf()
    P.dma("pool", Wr, router_w.rearrange("(k p) n -> p k n", p=128), writes=[b_Wr])
    rbT = TP([128, NE], F32)
    b_rb = Buf()
    P.dma("sync", rbT, router_b.partition_broadcast(128), writes=[b_rb])
    stats = TP([128, 64], F32)
    stat_i = [0]

    def ada_layer(l):
        c2raw = T([128, 2, 8], F32)
        b_c2 = Buf()
        for r in range(2):
            P.dma("sync", c2raw[:, r, :], c2[r].rearrange("(p k) -> p k", k=8), writes=[b_c2])
        sT = T([128, 8, 2], F32)
        b_sT = Buf()
        P.op("act", lambda e: e.activation(out=sT.rearrange("p k r -> p r k"), in_=c2raw, func=AF.Silu),
             reads=[b_c2], writes=[b_sT])
        CW_ = 512 if l == 0 else 256
        wA = Rot(3 if l == 0 else 2, [128, 8, CW_], F32)
        adabs = Rot(2, [2, CW_], F32)
        msbs = Rot(2, [2, CW_], F32)
        awv = ada_w[l].rearrange("(p k) n -> p k n", k=8)
        for j in range(6 * D // CW_):
            cs_ = slice(j * CW_, (j + 1) * CW_)
            wt, bw = wA.next()
            P.dma("sync", wt, awv[:, :, cs_], writes=[bw])
            ab, bab = adabs.next()
            P.dma("sync", ab, ada_b[l, cs_].partition_broadcast(2), writes=[bab])
            pb_i = j % 2

            def mm(e, wt=wt, pb_i=pb_i):
                for k in range(8):
                    ins = e.matmul(psb[pb_i][0:2, 0:CW_], lhsT=sT[:, k, :], rhs=wt[:, k, :],
                                   start=(k == 0), stop=(k == 7))
                return ins
            P.op("pe", mm, reads=[b_sT, bw], writes=[bps[pb_i]])
            mb, bmb = msbs.next()
            P.op("dve", lambda e, mb=mb, ab=ab, pb_i=pb_i: e.tensor_tensor(
                out=mb[0:2, :], in0=psb[pb_i][0:2, 0:CW_], in1=ab[0:2, :], op=ALU.add),
                reads=[bps[pb_i], bab], writes=[bmb])
            P.dma("act", Mscr[l][:, cs_], mb[0:2, :], reads=[bmb], writes=[bM[l]])
            if debug:
                dbg_ops.append(P.dma("act", dbg["d_M"][l][:, cs_], mb[0:2, :], reads=[bmb]))

    def phase0():
        ada_layer(0)

    dbg_ops = []
    phase0()

    _bregs = {}

    def breg(e, v):
        if v not in _bregs:
            _bregs[v] = e.to_reg(v)
        return _bregs[v]

    def conv_jobs():
        for l in range(2):
            for ex in range(NE):
                rows = slice(ex * 128, (ex + 1) * 128)
                gv = WGU[l][rows, :].rearrange("p (k n) -> p k n", k=8)
                yield (gv[:, :, 0:512], moe_w_gate[l, ex].rearrange("(k p) n -> p k n", p=128), bWGU[l])
                yield (gv[:, :, 512:1024], moe_w_up[l, ex].rearrange("(k p) n -> p k n", p=128), bWGU[l])
                yield (WD[l][rows, :].rearrange("p (k n) -> p k n", k=4),
                       moe_w_down[l, ex].rearrange("(k p) n -> p k n", p=128), bWD[l])
    conv_it = conv_jobs()
    conv_left = [96]

    def convert_upto(total):
        convert_some(max(0, conv_left[0] - (96 - total)))

    def convert_some(n):
        for _ in range(n):
            if conv_left[0] == 0:
                return
            o_, i_, _b = next(conv_it)
            conv_left[0] -= 1
            P.dma("pool", o_, i_, writes=[Buf()])

    def load_mod(l, row, which, dst, bdst):
        P.dma("sync", dst, Mscr[l, row, which * D:(which + 1) * D].partition_broadcast(128),
              reads=[bM[l]], writes=[bdst])

    def make_GS(l, gvec, shift_i, scale_i, rows=(0, 1)):
        gt = T([128, D], F32)
        bg = Buf()
        P.dma("sync", gt, gvec.partition_broadcast(128), writes=[bg])
        res = {}
        for row in rows:
            G = T([128, D], F32)
            S = T([128, D], F32)
            bG, bS = Buf(), Buf()
            load_mod(l, row, scale_i, G, bG)
            load_mod(l, row, shift_i, S, bS)
            P.op("dve", lambda e, G=G: e.scalar_tensor_tensor(out=G, in0=G, scalar=1.0, in1=gt,
                                                               op0=ALU.add, op1=ALU.mult),
                 reads=[bG, bg], writes=[bG])
            res[row] = (G, bG, S, bS)
        return res

    def norm_mod(ht, bh, G, bG, S, bS, tmp, btmp, a_out, ba):
        c = stat_i[0] % 32
        stat_i[0] += 1
        ss = stats[:, 2 * c:2 * c + 1]
        rs = stats[:, 2 * c + 1:2 * c + 2]
        bs = Buf()
        P.op("act", lambda e: e.activation(out=tmp, in_=ht, func=AF.Square, accum_out=ss),
             reads=[bh], writes=[btmp, bs])
        P.op("act", lambda e: e.activation(out=rs, in_=ss, func=AF.Ln, bias=epsT[:, 0:1], scale=1.0 / D),
             reads=[bs, b_eps], writes=[bs])
        P.op("act", lambda e: e.activation(out=rs, in_=rs, func=AF.Exp, scale=-0.5), reads=[bs], writes=[bs])
        if G is None:
            return rs, bs
        P.op("dve", lambda e: e.scalar_tensor_tensor(out=tmp, in0=ht, scalar=rs, in1=G,
                                                     op0=ALU.mult, op1=ALU.mult),
             reads=[bh, bs, bG], writes=[btmp])
        P.op("dve", lambda e: e.tensor_tensor(out=a_out, in0=tmp, in1=S, op=ALU.add),
             reads=[btmp, bS], writes=[ba])
        return rs, bs

    def transpose8(a_bf, ba, pbank, dstT, bdst, col0):
        pv = ps_bf(pbank).rearrange("p (k c) -> p k c", k=8)

        def tr(e):
            for k in range(8):
                ins = e.transpose(out=pv[:, k, :], in_=a_bf[:, k * 128:(k + 1) * 128], identity=ident)
            return ins
        P.op("pe", tr, reads=[ba, b_ident], writes=[bps[pbank]])
        P.op("act", lambda e: e.copy(out=dstT[:, :, col0:col0 + 128], in_=pv),
             reads=[bps[pbank]], writes=[bdst])

    def src_rows(i):
        if i < 32:
            return x[i * 128:(i + 1) * 128, :]
        return ctx[(i - 32) * 128:(i - 31) * 128, :]

    def routing(psR_bank, ntile, cwt, bcw, sc, bsc, aff_ready=False):
        n = ntile
        lg = psb[psR_bank][:, 0:n * 16]
        aff = sc[:, 0:n, 0:16]
        sel = sc[:, 0:n, 16:32]
        prs = sc[:, 0:n, 32:56]
        gs = sc[:, 0:n, 56:60]
        gmx = sc[:, 0:n, 60:61]
        gmk = sc[:, 0:n, 61:65]
        msel = sc[:, 0:n, 65:81]
        m1 = sc[:, 0:n, 81:82]
        tmp = sc[:, 0:n, 82:98]
        m2 = sc[:, 0:n, 98:99]
        wsum = sc[:, 0:n, 99:100]
        rd, wr = [bps[psR_bank], bsc, b_rb], [bsc]
        if not aff_ready:
            P.op("act", lambda e: e.activation(out=aff, in_=lg.rearrange("p (n e) -> p n e", e=16), func=AF.Sigmoid),
                 reads=rd, writes=wr)
        else:
            rd = [bsc, b_rb]
        P.op("dve", lambda e: e.tensor_tensor(out=sel, in0=aff, in1=rbT.unsqueeze(1).to_broadcast([128, n, 16]),
                                              op=ALU.add), reads=rd, writes=wr)
        sel4 = sel.rearrange("p n (g k) -> p n g k", k=4)
        prs4 = prs.rearrange("p n (g k) -> p n g k", k=6)
        pi = 0
        for a in range(4):
            for b in range(a + 1, 4):
                P.op("dve", lambda e, a=a, b=b, pi=pi: e.tensor_tensor(
                    out=prs4[:, :, :, pi], in0=sel4[:, :, :, a], in1=sel4[:, :, :, b], op=ALU.add),
                    reads=[bsc], writes=wr)
                pi += 1
        P.op("dve", lambda e: e.tensor_reduce(out=gs, in_=prs4, axis=AX.X, op=ALU.max), reads=[bsc], writes=wr)
        P.op("dve", lambda e: e.tensor_reduce(out=sc[:, 0:n, 60], in_=gs, axis=AX.X, op=ALU.max), reads=[bsc], writes=wr)
        P.op("dve", lambda e: e.tensor_tensor(out=gmk, in0=gs, in1=gmx.to_broadcast([128, n, 4]), op=ALU.is_ge),
             reads=[bsc], writes=wr)
        P.op("dve", lambda e: e.tensor_scalar(out=gmk, in0=gmk, scalar1=BIG, scalar2=-BIG, op0=ALU.mult, op1=ALU.add),
             reads=[bsc], writes=wr)
        P.op("dve", lambda e: e.tensor_tensor(out=msel.rearrange("p n (g k) -> p n g k", k=4), in0=sel4,
                                              in1=gmk.unsqueeze(3).to_broadcast([128, n, 4, 4]), op=ALU.add),
             reads=[bsc], writes=wr)
        P.op("dve", lambda e: e.tensor_reduce(out=sc[:, 0:n, 81], in_=msel, axis=AX.X, op=ALU.max), reads=[bsc], writes=wr)
        P.op("dve", lambda e: e.tensor_tensor(out=tmp, in0=msel, in1=m1.to_broadcast([128, n, 16]), op=ALU.is_ge),
             reads=[bsc], writes=wr)
        P.op("dve", lambda e: e.scalar_tensor_tensor(out=tmp, in0=tmp, scalar=-BIG, in1=msel,
                                                     op0=ALU.mult, op1=ALU.add), reads=[bsc], writes=wr)
        P.op("dve", lambda e: e.tensor_reduce(out=sc[:, 0:n, 98], in_=tmp, axis=AX.X, op=ALU.max), reads=[bsc], writes=wr)
        P.op("dve", lambda e: e.tensor_tensor(out=tmp, in0=msel, in1=m2.to_broadcast([128, n, 16]), op=ALU.is_ge),
             reads=[bsc], writes=wr)
        P.op("dve", lambda e: e.tensor_tensor(out=tmp, in0=tmp, in1=aff, op=ALU.mult), reads=[bsc], writes=wr)
        P.op("dve", lambda e: e.tensor_reduce(out=sc[:, 0:n, 99], in_=tmp, axis=AX.X, op=ALU.add), reads=[bsc], writes=wr)
        P.op("dve", lambda e: e.reciprocal(out=wsum, in_=wsum), reads=[bsc], writes=wr)
        P.op("dve", lambda e: e.tensor_tensor(out=cwt[:, 0:n, :], in0=tmp, in1=wsum.to_broadcast([128, n, 16]), op=ALU.mult),
             reads=[bsc], writes=[bcw])

    def layer0_A():
        new_phase()
        Vcs = T([128, NT, 1024], BF16)
        bV = Buf()
        keep = st["off"]
        Wcs = T([128, 8, 1024], BF16)
        bWcs = Buf()
        Wconv = T([128, 8, 1536], BF16)
        bWconv = Buf()
        P.dma("pool", Wconv, even_w_in[:, 512:2048].rearrange("(k p) n -> p k n", p=128), writes=[bWconv])
        Wf = T([128, 8, 512], BF16)
        bWf = Buf()
        P.dma("pool", Wf, even_w_in[:, 0:512].rearrange("(k p) n -> p k n", p=128), writes=[bWf])
        dD = T([128, 256], BF16)
        bdD = Buf()
        P.dma("sync", dD, dftD, writes=[bdD])
        WfT = T([128, 1024], BF16)
        bWfT = Buf()
        for h in range(4):
            pv = ps_bf(0).rearrange("p (k c) -> p k c", k=8)

            def tr(e, h=h, pv=pv):
                for k in range(8):
                    ins = e.transpose(out=pv[:, k, :], in_=Wf[:, k, h * 128:(h + 1) * 128], identity=ident)
                return ins
            P.op("pe", tr, reads=[bWf, b_ident], writes=[bps[0]])
            P.op("act", lambda e: e.copy(out=WfT, in_=ps_bf(0)), reads=[bps[0]], writes=[bWfT])
            for k in range(8):
                pbk = 1 + (k % 2)
                P.op("pe", lambda e, k=k, pbk=pbk: e.matmul(psb[pbk][:, 0:256], lhsT=WfT[:, k * 128:(k + 1) * 128],
                                                            rhs=dD, start=True, stop=True),
                     reads=[bWfT, bdD], writes=[bps[pbk]])
                P.op("dve", lambda e, k=k, h=h, pbk=pbk: e.tensor_copy(
                    out=Wcs[:, k, :].rearrange("p (two hh c) -> p two hh c", two=2, hh=4)[:, :, h, :],
                    in_=psb[pbk][:, 0:256].rearrange("p (two c) -> p two c", two=2)),
                    reads=[bps[pbk]], writes=[bWcs])
        GS = make_GS(0, norm_mix_g[0], 0, 1)
        hts = Rot(3, [128, D], F32)
        tmps = Rot(2, [128, D], F32)
        abf = Rot(2, [128, D], BF16)
        aT4s = Rot(2, [128, 8, 512], BF16)
        zts = Rot(2, [128, 4, 512], BF16)
        gbs = Rot(2, [128, 4, 512], BF16)
        gcs = Rot(2, [128, 512], F32)
        def l0_stage1(i):
            row = 0 if i < 32 else 1
            G, bG, S, bS = GS[row]
            ht, bh = hts.next()
            P.dma("sync", ht, src_rows(i), writes=[bh])
            tmp, btmp = tmps.next()
            a, ba = abf.next()
            norm_mod(ht, bh, G, bG, S, bS, tmp, btmp, a, ba)
            return a, ba

        def l0_stage2(i, j, a, ba, aT4, baT4):
            transpose8(a, ba, i % 2, aT4, baT4, j * 128)
            pb0 = 2 + 2 * (i % 2)

            def mmv(e):
                for hf in range(2):
                    for k in range(8):
                        ins = e.matmul(psb[pb0 + hf], lhsT=aT4[:, k, j * 128:(j + 1) * 128],
                                       rhs=Wcs[:, k, hf * 512:(hf + 1) * 512], start=(k == 0), stop=(k == 7))
                return ins
            P.op("pe", mmv, reads=[baT4, bWcs], writes=[bps[pb0], bps[pb0 + 1]])
            P.op("act", lambda e: e.copy(out=Vcs[:, i, 0:512], in_=psb[pb0]), reads=[bps[pb0]], writes=[bV])
            P.op("dve", lambda e: e.tensor_copy(out=Vcs[:, i, 512:1024], in_=psb[pb0 + 1]), reads=[bps[pb0 + 1]], writes=[bV])

        ngroups = 9
        pend = l0_stage1(0)
        for g in range(ngroups):
            ntile = 4 if g < 8 else 2
            gw = ntile * 128
            t0 = g * 512
            aT4, baT4 = aT4s.next()
            for j in range(ntile):
                i = g * 4 + j
                nxt = l0_stage1(i + 1) if i + 1 < NT else None
                l0_stage2(i, j, pend[0], pend[1], aT4, baT4)
                pend = nxt
            zt, bz = zts.next()
            gb, bgb = gbs.next()
            for cc in range(4):
                for part in (1, 0, 2):
                    c = part * 4 + cc
                    pbk = 6 + (c % 2)

                    def mmc(e, c=c, pbk=pbk, aT4=aT4, gw=gw):
                        for k in range(8):
                            ins = e.matmul(psb[pbk][:, 0:gw], lhsT=Wconv[:, k, c * 128:(c + 1) * 128],
                                           rhs=aT4[:, k, 0:gw], start=(k == 0), stop=(k == 7))
                        return ins
                    P.op("pe", mmc, reads=[baT4, bWconv], writes=[bps[pbk]])
                    if part == 1:
                        gc, bgc = gcs.next()
                        P.op("act", lambda e, gc=gc, pbk=pbk, gw=gw: e.copy(out=gc[:, 0:gw], in_=psb[pbk][:, 0:gw]),
                             reads=[bps[pbk]], writes=[bgc])
                    elif part == 0:
                        P.op("act", lambda e, gb=gb, cc=cc, pbk=pbk, gw=gw: e.copy(out=gb[:, cc, 0:gw], in_=psb[pbk][:, 0:gw]),
                             reads=[bps[pbk]], writes=[bgb])
                    else:
                        P.op("dve", lambda e, zt=zt, cc=cc, pbk=pbk, gc=gc, gw=gw: e.tensor_tensor(
                            out=zt[:, cc, 0:gw], in0=psb[pbk][:, 0:gw], in1=gc[:, 0:gw], op=ALU.mult),
                            reads=[bps[pbk], bgc], writes=[bz])
            convert_some(3)
            P.dma("act", ZT[:, :, t0:t0 + gw].rearrange("c p t -> p c t"), zt[:, :, 0:gw], reads=[bz], writes=[bZT])
            P.dma("act", GBT[:, :, t0:t0 + gw].rearrange("c p t -> p c t"), gb[:, :, 0:gw], reads=[bgb], writes=[bGBT])
        return Vcs, bV, keep

    def mixer_tail(l, g, ntile, yT, byT, Wout, bWout, gate1, GS2, hts, tmps, fbf, fT4, bfT4, hsrc_fn, psY0, psT_bank,
                   psR_bank, cwt, bcw, rsc, brsc, dbg_hm=None, filler=None, deferred=False):
        gw = ntile * 128
        t0 = g * 512

        def stA(j):
            i = g * 4 + j
            row = 0 if i < 32 else 1

            def mmo(e):
                for hf in range(2):
                    for k in range(8):
                        ins = e.matmul(psb[psY0 + hf], lhsT=yT[:, k, j * 128:(j + 1) * 128],
                                       rhs=Wout[:, k, hf * 512:(hf + 1) * 512], start=(k == 0), stop=(k == 7))
                return ins
            P.op("pe", mmo, reads=[byT, bWout], writes=[bps[psY0], bps[psY0 + 1]])
            ht, bh = hts.next()
            hsrc, hreads = hsrc_fn(i)
            P.dma("sync", ht, hsrc, reads=hreads, writes=[bh])
            tmp, btmp = tmps.next()
            g1, bg1 = gate1[row]
            for hf in range(2):
                P.op("dve", lambda e, hf=hf: e.tensor_tensor(
                    out=tmp[:, hf * 512:(hf + 1) * 512], in0=psb[psY0 + hf], in1=g1[:, hf * 512:(hf + 1) * 512],
                    op=ALU.mult), reads=[bps[psY0 + hf], bg1], writes=[btmp])
            P.op("dve", lambda e: e.tensor_tensor(out=ht, in0=tmp, in1=ht, op=ALU.add), reads=[btmp, bh], writes=[bh])
            P.dma("pool", Hm[i * 128:(i + 1) * 128, :], ht, reads=[bh], writes=[bHm])
            if dbg_hm is not None:
                dbg_ops.append(P.dma("pool", dbg_hm[i * 128:(i + 1) * 128, :], ht, reads=[bh]))
            G, bG, S, bS = GS2[row]
            f, bf_ = fbf.next()
            norm_mod(ht, bh, G, bG, S, bS, tmp, btmp, f, bf_)
            P.dma("act", Fd[i * 128:(i + 1) * 128, :], f, reads=[bf_], writes=[Buf()])
            return f, bf_

        def stB(j, f, bf_):
            transpose8(f, bf_, psT_bank, fT4, bfT4, j * 128)

            def mmr(e):
                for k in range(8):
                    ins = e.matmul(psb[psR_bank][:, j * 16:(j + 1) * 16], lhsT=fT4[:, k, j * 128:(j + 1) * 128],
                                   rhs=Wr[:, k, :], start=(k == 0), stop=(k == 7))
                return ins
            if deferred:
                def mmr0(e):
                    for k in range(8):
                        ins = e.matmul(psb[psR_bank][:, 0:16], lhsT=fT4[:, k, j * 128:(j + 1) * 128],
                                       rhs=Wr[:, k, :], start=(k == 0), stop=(k == 7))
                    return ins
                P.op("pe", mmr0, reads=[bfT4, b_Wr], writes=[bps[psR_bank]])
                P.op("act", lambda e: e.activation(out=rsc[:, j, 0:16], in_=psb[psR_bank][:, 0:16], func=AF.Sigmoid),
                     reads=[bps[psR_bank]], writes=[brsc])
            else:
                P.op("pe", mmr, reads=[bfT4, b_Wr], writes=[bps[psR_bank]])

        def finish():
            routing(psR_bank, ntile, cwt, bcw, rsc, brsc, aff_ready=deferred)
            P.dma("act", CW[t0:t0 + gw, :].rearrange("(n p) e -> p n e", p=128), cwt[:, 0:ntile, :], reads=[bcw], writes=[bCW])

        if deferred:
            return stA, stB, finish

        pend = stA(0)
        for j in range(ntile):
            if filler is not None:
                filler(1)
            nxt = stA(j + 1) if j + 1 < ntile else None
            if filler is not None:
                filler(1)
            stB(j, pend[0], pend[1])
            pend = nxt
        finish()

    def layer0_B(Vcs, bV, keep):
        P.barrier()
        st["off"] = keep
        Wout = T([128, 8, D], BF16)
        bWout = Buf()
        P.dma("pool", Wout, even_w_out.rearrange("(k p) n -> p k n", p=128), writes=[bWout])
        cwc = T([128, 3, 4], F32)
        bcwc = Buf()
        for kk in range(3):
            P.dma("sync", cwc[:, kk, :], even_conv_w[kk].rearrange("(c p) -> p c", p=128), writes=[bcwc],
                  allow_slow_non_contiguous=True)
        dC = T([128, 4, 256], BF16)
        bdC = Buf()
        P.dma("sync", dC, dftC, writes=[bdC])
        GS2 = make_GS(0, norm_ffn_g[0], 3, 4)
        gate1 = []
        for row in range(2):
            gt = T([128, D], F32)
            bg = Buf()
            load_mod(0, row, 2, gt, bg)
            gate1.append((gt, bg))
        ring = Rot(3, [128, 8, 512], BF16)
        yTs = Rot(2, [128, 8, 512], BF16)
        zin = Rot(1, [128, 4, 514], BF16)
        gbin = Rot(1, [128, 4, 512], BF16)
        cacc = Rot(2, [128, 512], F32)
        hts = Rot(2, [128, D], F32)
        tmps = Rot(2, [128, D], F32)
        fbf = Rot(2, [128, D], BF16)
        fT4s = Rot(1, [128, 8, 512], BF16)
        cws = Rot(2, [128, 4, NE], F32)
        rsc = T([128, 4, 128], F32)
        brsc = Buf()
        def fourier_ops(g, yT, byT):
            ops_ = []
            gw_ = 512 if g < 8 else 256
            if g < 8:
                for piece in range(8):
                    def one(piece=piece):
                        rt, brt = ring.next()
                        P.dma("sync", rt, dftN[g, :, piece * 8:(piece + 1) * 8, :], writes=[brt])

                        def mmf(e):
                            for s8_ in range(8):
                                stt = piece * 8 + s8_
                                base = 0 if stt < 32 else 512
                                for h in range(4):
                                    ins = e.matmul(psb[h], lhsT=Vcs[:, stt % 32, base + h * 128: base + (h + 1) * 128],
                                                   rhs=rt[:, s8_, :], start=(stt == 0), stop=(stt == 63))
                            return ins
                        P.op("pe", mmf, reads=[brt, bV], writes=[bps[0], bps[1], bps[2], bps[3]])
                    ops_.append(one)
            else:
                def onec():
                    def mmfc(e):
                        for jj in range(4):
                            base = 0 if jj < 2 else 512
                            for h in range(4):
                                ins = e.matmul(psb[h][:, 0:256], lhsT=Vcs[:, 32 + (jj % 2), base + h * 128: base + (h + 1) * 128],
                                               rhs=dC[:, jj, :], start=(jj == 0), stop=(jj == 3))
                        return ins
                    P.op("pe", mmfc, reads=[bdC, bV], writes=[bps[0], bps[1], bps[2], bps[3]])
                ops_.append(onec)

            def evac():
                for h in range(4):
                    P.op("act", lambda e, h=h: e.copy(out=yT[:, h, 0:gw_], in_=psb[h][:, 0:gw_]), reads=[bps[h]], writes=[byT])
            ops_.append(evac)
            return ops_

        yT_next = yTs.next()
        pending_f = fourier_ops(0, *yT_next)
        for g in range(9):
            ntile = 4 if g < 8 else 2
            gw = ntile * 128
            t0 = g * 512
            yT, byT = yT_next
            while pending_f:
                pending_f.pop(0)()
            if g + 1 < 9:
                yT_next = yTs.next()
                pending_f = fourier_ops(g + 1, *yT_next)

            def filler(n, pending_f=pending_f):
                for _ in range(n):
                    if pending_f:
                        pending_f.pop(0)()
            zt, bz = zin.next()
            gb, bgb = gbin.next()
            first = g in (0, 8)
            last = g in (7, 8)
            lo = t0 - (0 if first else 1)
            hi = t0 + gw + (0 if last else 1)
            c0 = 1 if first else 0
            if first:
                P.op("pool", lambda e, zt=zt: e.memset(zt[:, :, 0:1], 0.0), writes=[bz])
            if last:
                P.op("pool", lambda e, zt=zt, gw=gw: e.memset(zt[:, :, gw + 1:gw + 2], 0.0), writes=[bz])
            P.dma("sync", zt[:, :, c0:c0 + (hi - lo)], ZT[:, :, lo:hi].rearrange("c p t -> p c t"), reads=[bZT], writes=[bz])
            P.dma("sync", gb[:, :, 0:gw], GBT[:, :, t0:t0 + gw].rearrange("c p t -> p c t"), reads=[bGBT], writes=[bgb])
            for cc in range(4):
                ac, bac = cacc.next()
                P.op("dve", lambda e, ac=ac, zt=zt, cc=cc, gw=gw: e.tensor_scalar(
                    out=ac[:, 0:gw], in0=zt[:, cc, 0:gw], scalar1=cwc[:, 0, cc:cc + 1], scalar2=None, op0=ALU.mult),
                    reads=[bz, bcwc], writes=[bac])
                for kk in (1, 2):
                    P.op("dve", lambda e, ac=ac, zt=zt, cc=cc, gw=gw, kk=kk: e.scalar_tensor_tensor(
                        out=ac[:, 0:gw], in0=zt[:, cc, kk:kk + gw], scalar=cwc[:, kk, cc:cc + 1], in1=ac[:, 0:gw],
                        op0=ALU.mult, op1=ALU.add), reads=[bz, bcwc, bac], writes=[bac])
                P.op("dve", lambda e, ac=ac, gb=gb, cc=cc, gw=gw, yT=yT: e.tensor_tensor(
                    out=yT[:, 4 + cc, 0:gw], in0=ac[:, 0:gw], in1=gb[:, cc, 0:gw], op=ALU.mult),
                    reads=[bac, bgb], writes=[byT])
            convert_some(3)
            fT4, bfT4 = fT4s.next()
            cwt, bcw = cws.next()
            mixer_tail(0, g, ntile, yT, byT, Wout, bWout, gate1, GS2, hts, tmps, fbf, fT4, bfT4,
                       lambda i: (src_rows(i), []), 4, 6, 7, cwt, bcw, rsc, brsc, dbg.get("d_hm0"), filler=filler)

    def moe(l, ntok, Hout, bHout, final):
        new_phase()
        sgs = [(0, 2048), (2048, ntok - 2048)]
        accmax = max(n for _, n in sgs) // 128
        acc = T([128, accmax, D], F32)
        bacc = [Buf() for _ in range(accmax)]
        fTs = T([128, 8, accmax * 128], BF16)
        bfTs = Buf()
        cws = T([128, accmax, NE], F32)
        bcws = Buf()
        wgs = Rot(2, [128, 8, 512], BF16)
        wus = Rot(2, [128, 8, 512], BF16)
        wds = Rot(2, [128, 4, D], BF16)
        hid = Rot(2, [128, 4, 512], BF16)
        sgt = Rot(2, [128, 512], F32)
        g2 = []
        for row in range(2):
            gt = T([128, D], F32)
            bg = Buf()
            load_mod(l, row, 5, gt, bg)
            g2.append((gt, bg))
        hts = Rot(2, [128, D], F32)
        tmps = Rot(2, [128, D], F32)
        if final:
            fg = T([128, D], F32)
            bfg = Buf()
            P.dma("sync", fg, final_g.partition_broadcast(128), writes=[bfg])
        outs = []
        for (s0, sn) in sgs:
            ntl = sn // 128
            P.dma("sync", fTs[:, :, 0:sn], FT[:, :, s0:s0 + sn].rearrange("k p t -> p k t"), reads=[bFT], writes=[bfTs])
            P.dma("sync", cws[:, 0:ntl, :], CW[s0:s0 + sn, :].rearrange("(n p) e -> p n e", p=128), reads=[bCW], writes=[bcws])
            groups = [(q, min(512, sn - q)) for q in range(0, sn, 512)]
            for ex in range(NE):
                wg, bwg = wgs.next()
                wu, bwu = wus.next()
                wd, bwd = wds.next()
                P.dma("pool", wg, moe_w_gate[l, ex].rearrange("(k p) n -> p k n", p=128), writes=[bwg])
                P.dma("pool", wu, moe_w_up[l, ex].rearrange("(k p) n -> p k n", p=128), writes=[bwu])
                P.dma("pool", wd, moe_w_down[l, ex].rearrange("(k p) n -> p k n", p=128), writes=[bwd])
                for (q0, qn) in groups:
                    hd, bhd = hid.next()
                    for jc in range(4):
                        pg, pu = (0, 1) if jc % 2 == 0 else (2, 3)

                        def mmg(e, jc=jc, pg=pg, pu=pu, wg=wg, wu=wu, q0=q0, qn=qn):
                            for (pb_, w_) in ((pg, wg), (pu, wu)):
                                for k in range(8):
                                    ins = e.matmul(psb[pb_][:, 0:qn], lhsT=w_[:, k, jc * 128:(jc + 1) * 128],
                                                   rhs=fTs[:, k, q0:q0 + qn], start=(k == 0), stop=(k == 7))
                            return ins
                        P.op("pe", mmg, reads=[bwg, bwu, bfTs], writes=[bps[pg], bps[pu]])
                        sg, bsg = sgt.next()
                        P.op("act", lambda e, sg=sg, pg=pg, qn=qn: e.activation(out=sg[:, 0:qn], in_=psb[pg][:, 0:qn], func=AF.Silu),
                             reads=[bps[pg]], writes=[bsg])
                        P.op("dve", lambda e, sg=sg, pu=pu, hd=hd, jc=jc, qn=qn: e.tensor_tensor(
                            out=hd[:, jc, 0:qn], in0=psb[pu][:, 0:qn], in1=sg[:, 0:qn], op=ALU.mult),
                            reads=[bps[pu], bsg], writes=[bhd])
                    for jt in range(qn // 128):
                        tl = (q0 // 128) + jt
                        for hf in range(2):
                            pd = 4 + ((jt * 2 + hf) % 4)

                            def mmd(e, pd=pd, hd=hd, jt=jt, hf=hf, wd=wd):
                                for jc in range(4):
                                    ins = e.matmul(psb[pd], lhsT=hd[:, jc, jt * 128:(jt + 1) * 128],
                                                   rhs=wd[:, jc, hf * 512:(hf + 1) * 512], start=(jc == 0), stop=(jc == 3))
                                return ins
                            P.op("pe", mmd, reads=[bhd, bwd], writes=[bps[pd]])
                            av = acc[:, tl, hf * 512:(hf + 1) * 512]
                            if ex == 0:
                                P.op("dve", lambda e, av=av, pd=pd, tl=tl, ex=ex: e.tensor_scalar(
                                    out=av, in0=psb[pd], scalar1=cws[:, tl, ex:ex + 1], scalar2=None, op0=ALU.mult),
                                    reads=[bps[pd], bcws], writes=[bacc[tl]])
                            else:
                                P.op("dve", lambda e, av=av, pd=pd, tl=tl, ex=ex: e.scalar_tensor_tensor(
                                    out=av, in0=psb[pd], scalar=cws[:, tl, ex:ex + 1], in1=av,
                                    op0=ALU.mult, op1=ALU.add), reads=[bps[pd], bcws, bacc[tl]], writes=[bacc[tl]])
            for tl in range(ntl):
                i = s0 // 128 + tl
                row = 0 if i < 32 else 1
                ht, bh = hts.next()
                P.dma("sync", ht, Hm[i * 128:(i + 1) * 128, :], reads=[bHm], writes=[bh])
                gt, bg = g2[row]
                P.op("pool", lambda e, tl=tl, gt=gt: e.tensor_tensor(out=acc[:, tl, :], in0=acc[:, tl, :], in1=gt, op=ALU.mult),
                     reads=[bacc[tl], bg], writes=[bacc[tl]])
                P.op("pool", lambda e, tl=tl, ht=ht: e.tensor_tensor(out=ht, in0=acc[:, tl, :], in1=ht, op=ALU.add),
                     reads=[bacc[tl], bh], writes=[bh])
                if not final:
                    P.dma("pool", Hout[i * 128:(i + 1) * 128, :], ht, reads=[bh], writes=[bHout])
                    if debug:
                        dbg_ops.append(P.dma("pool", dbg["d_h1"][i * 128:(i + 1) * 128, :], ht, reads=[bh]))
                else:
                    tmp, btmp = tmps.next()
                    rs, bs = norm_mod(ht, bh, None, None, None, None, tmp, btmp, None, None)
                    P.op("dve", lambda e, tmp=tmp, ht=ht, rs=rs: e.scalar_tensor_tensor(
                        out=tmp, in0=ht, scalar=rs, in1=fg, op0=ALU.mult, op1=ALU.mult),
                        reads=[bh, bs, bfg], writes=[btmp])
                    outs.append(P.dma("act", out[i * 128:(i + 1) * 128, :], tmp, reads=[btmp]))
        return outs


    def layer1_A():
        new_phase()
        Up = T([128, 32, 512], BF16)
        qT = T([128, 4, SEQ], BF16)
        kT = T([128, 2, NTOK], BF16)
        Vt = T([128, NT, 128], BF16)
        bUp, bqT, bkT, bVt = Buf(), Buf(), Buf(), Buf()
        keep = st["off"]
        Wp = T([128, 8, 512], BF16)
        Wq = T([128, 8, 512], BF16)
        Wqr = T([128, 8, 512], BF16)
        Wk = T([128, 8, 256], BF16)
        Wkr = T([128, 8, 256], BF16)
        Wv = T([128, 8, 128], BF16)
        bWp, bWq, bWqr, bWk, bWkr, bWv = Buf(), Buf(), Buf(), Buf(), Buf(), Buf()
        wv = odd_w_in.rearrange("(k p) n -> p k n", p=128)
        P.dma("pool", Wp, wv[:, :, 0:512], writes=[bWp])
        P.dma("pool", Wq, wv[:, :, 512:1024], writes=[bWq])
        for a in range(4):
            P.dma("pool", Wk[:, :, a * 64:(a + 1) * 64], wv[:, :, 1024 + (a // 2) * 64:1024 + (a // 2 + 1) * 64], writes=[bWk])
        P.dma("pool", Wv, wv[:, :, 1152:1280], writes=[bWv])
        for (src, bsrc, dst, bdst, nblk) in ((Wq, bWq, Wqr, bWqr, 16), (Wk, bWk, Wkr, bWkr, 8)):
            sv = src.rearrange("p k (blk two i) -> p (k blk) two i", two=2, i=16)
            dv = dst.rearrange("p k (blk two i) -> p (k blk) two i", two=2, i=16)
            P.op("dve", lambda e, sv=sv, dv=dv: e.tensor_scalar(out=dv[:, :, 0, :], in0=sv[:, :, 1, :], scalar1=-1.0,
                                                              scalar2=None, op0=ALU.mult), reads=[bsrc], writes=[bdst])
            P.op("dve", lambda e, sv=sv, dv=dv: e.tensor_copy(out=dv[:, :, 1, :], in_=sv[:, :, 0, :]), reads=[bsrc], writes=[bdst])
        GS = make_GS(1, norm_mix_g[1], 0, 1)
        hts = Rot(2, [128, D], F32)
        tmps = Rot(2, [128, D], F32)
        abf = Rot(2, [128, D], BF16)
        aT4s = Rot(2, [128, 8, 512], BF16)
        ropes = Rot(2, [128, 2, 512], F32)
        rt1 = Rot(2, [128, 512], F32)
        rt2 = Rot(2, [128, 512], F32)
        fm_i = [0]

        def l1_stage1(i):
            row = 0 if i < 32 else 1
            G, bG, S, bS = GS[row]
            ht, bh = hts.next()
            P.dma("sync", ht, H1[i * 128:(i + 1) * 128, :], reads=[bH1], writes=[bh])
            tmp, btmp = tmps.next()
            a, ba = abf.next()
            norm_mod(ht, bh, G, bG, S, bS, tmp, btmp, a, ba)
            return a, ba

        def l1_stage2(i, j, a, ba, aT4, baT4):
            transpose8(a, ba, i % 2, aT4, baT4, j * 128)
            if i < 32:
                def mmp(e):
                    for k in range(8):
                        ins = e.matmul(psb[2], lhsT=aT4[:, k, j * 128:(j + 1) * 128], rhs=Wp[:, k, :],
                                       start=(k == 0), stop=(k == 7))
                    return ins
                P.op("pe", mmp, reads=[baT4, bWp], writes=[bps[2]])
                P.op("act", lambda e: e.copy(out=Up[:, i, :], in_=psb[2]), reads=[bps[2]], writes=[bUp])

            def mmvv(e):
                for k in range(8):
                    ins = e.matmul(psb[3][:, 0:128], lhsT=aT4[:, k, j * 128:(j + 1) * 128], rhs=Wv[:, k, :],
                                   start=(k == 0), stop=(k == 7))
                return ins
            P.op("pe", mmvv, reads=[baT4, bWv], writes=[bps[3]])
            P.op("dve", lambda e: e.tensor_copy(out=Vt[:, i, :], in_=psb[3][:, 0:128]), reads=[bps[3]], writes=[bVt])

        pend1 = [l1_stage1(0)]
        for g in range(9):
            ntile = 4 if g < 8 else 2
            gw = ntile * 128
            t0 = g * 512
            aT4, baT4 = aT4s.next()
            for j in range(ntile):
                i = g * 4 + j
                nxt1 = l1_stage1(i + 1) if i + 1 < NT else None
                l1_stage2(i, j, pend1[0][0], pend1[0][1], aT4, baT4)
                pend1[0] = nxt1
            convert_some(3)
            if g < 8:
                rp, brp = ropes.next()
                P.dma("sync", rp, rope[:, :, t0:t0 + 512], writes=[brp])
            jobs = [("k", jj) for jj in range(2)]
            if g < 8:
                jobs = [("q", cc) for cc in range(4)] + jobs
            for (kind, cc) in jobs:
                W, bW, Wr_, bWr_ = (Wq, bWq, Wqr, bWqr) if kind == "q" else (Wk, bWk, Wkr, bWkr)
                pA, pB = (4, 5) if fm_i[0] % 2 == 0 else (6, 7)
                fm_i[0] += 1
                rot = g < 8

                def mmq(e, W=W, Wr_=Wr_, cc=cc, pA=pA, pB=pB, aT4=aT4, gw=gw, rot=rot):
                    for (pb_, w_) in ((pA, W), (pB, Wr_)) if rot else ((pA, W),):
                        for k in range(8):
                            ins = e.matmul(psb[pb_][:, 0:gw], lhsT=w_[:, k, cc * 128:(cc + 1) * 128], rhs=aT4[:, k, 0:gw],
                                           start=(k == 0), stop=(k == 7))
                    return ins
                P.op("pe", mmq, reads=[baT4, bW, bWr_], writes=[bps[pA], bps[pB]])
                dst = qT[:, cc, t0:t0 + gw] if kind == "q" else kT[:, cc, t0:t0 + gw]
                bdst = bqT if kind == "q" else bkT
                if rot:
                    t1, bt1 = rt1.next()
                    t2, bt2 = rt2.next()
                    P.op("dve", lambda e, t1=t1, pA=pA, rp=rp: e.tensor_tensor(out=t1, in0=psb[pA], in1=rp[:, 0, :], op=ALU.mult),
                         reads=[bps[pA], brp], writes=[bt1])
                    P.op("dve", lambda e, t2=t2, pB=pB, rp=rp: e.tensor_tensor(out=t2, in0=psb[pB], in1=rp[:, 1, :], op=ALU.mult),
                         reads=[bps[pB], brp], writes=[bt2])
                    P.op("dve", lambda e, t1=t1, t2=t2, dst=dst: e.tensor_tensor(out=dst, in0=t1, in1=t2, op=ALU.add),
                         reads=[bt1, bt2], writes=[bdst])
                else:
                    P.op("act", lambda e, dst=dst, pA=pA, gw=gw: e.copy(out=dst, in_=psb[pA][:, 0:gw]), reads=[bps[pA]], writes=[bdst])
        return (Up, bUp, qT, bqT, kT, bkT, Vt, bVt), keep

    def layer1_B(pk, keep):
        Up, bUp, qT, bqT, kT, bkT, Vt, bVt = pk
        P.barrier()
        st["off"] = keep
        Wout = T([128, 8, D], BF16)
        bWout = Buf()
        P.dma("pool", Wout, odd_w_out.rearrange("(k p) n -> p k n", p=128), writes=[bWout])
        pB = T([128, 20, 128], BF16)
        bpB = Buf()
        P.dma("sync", pB, poolB, writes=[bpB])
        PW = T([128, 4, 128], BF16)
        bPW = Buf()
        P.dma("pool", PW, odd_pool_w.rearrange("g c d -> c g d"), writes=[bPW])
        psc = T([128, 4], F32)
        bpsc = Buf()
        P.dma("sync", psc, odd_pool_scale.rearrange("(g p) -> p g", p=128), writes=[bpsc], allow_slow_non_contiguous=True)
        mk = T([128, 2, 128], BF16)
        bmk = Buf()
        P.dma("pool", mk, amask, writes=[bmk])
        sk8 = T([128, 8], F32)
        bsk8 = Buf()
        P.dma("sync", sk8, odd_sink.partition_broadcast(128), writes=[bsk8])
        P.op("dve", lambda e: e.tensor_scalar(out=sk8, in0=sk8, scalar1=8.0, scalar2=None, op0=ALU.mult), reads=[bsk8], writes=[bsk8])
        GS2 = make_GS(1, norm_ffn_g[1], 3, 4, rows=(0,))
        gt = T([128, D], F32)
        bg = Buf()
        load_mod(1, 0, 2, gt, bg)
        gate1 = {0: (gt, bg)}
        yTs = Rot(2, [128, 8, 512], BF16)
        Ps = Rot(3, [128, 640], BF16)
        PTs = Rot(3, [128, 5, 128], BF16)
        pTs = Rot(1, [128, 4, 128], BF16)
        Osb = Rot(2, [128, 8, 64], BF16)
        st8 = Rot(2, [128, 8, 8], F32)
        st8h = [[Buf() for _ in range(8)] for _ in range(2)]
        st8b = [Buf(), Buf()]
        hts = Rot(2, [128, D], F32)
        tmps = Rot(2, [128, D], F32)
        fbf = Rot(2, [128, D], BF16)
        fT4s = Rot(1, [128, 8, 512], BF16)
        cws = Rot(2, [128, 4, NE], F32)
        rsc = T([128, 4, 128], F32)
        brsc = Buf()
        hcount = [0]
        def attn_block(i, cs, yT, byT):
            wb = [b_ for b_ in (i - 1, i, i + 1) if 0 <= b_ < 32]
            nw = len(wb) * 128
            w0 = wb[0] * 128
            c_lo = 512 - nw
            s8, bs8 = st8.next()
            osb, bosb = Osb.next()
            pO = psb[6].rearrange("p (h d) -> p h d", h=8)
            slot8 = (st8.i - 1) % 2
            hb = st8h[slot8]

            def st_qk(h):
                c, half, kvj = h // 2, h % 2, h // 4
                pr = slice(half * 64, (half + 1) * 64)
                pi = hcount[0] % 2
                hcount[0] += 1
                pair = pairs[pi]
                bpair = [bps[2 * pi], bps[2 * pi + 1]]

                def mmqk(e, pair=pair, pr=pr, c=c, kvj=kvj):
                    qv = qT[pr, c, i * 128:(i + 1) * 128]
                    e.matmul(pair[:, c_lo:512], lhsT=qv, rhs=kT[pr, kvj, w0:w0 + nw], start=True, stop=False,
                             skip_group_check=True)
                    if wb[0] == i - 1:
                        e.matmul(pair[:, c_lo:c_lo + 128], lhsT=ident, rhs=mk[:, 0, :], start=False, stop=False,
                                 skip_group_check=True)
                    if wb[-1] == i + 1:
                        e.matmul(pair[:, 384:512], lhsT=ident, rhs=mk[:, 1, :], start=False, stop=False,
                                 skip_group_check=True)
                    ins = e.matmul(pair[:, 512:768], lhsT=qv, rhs=kT[pr, kvj, SEQ:SEQ + 256], start=True, stop=True,
                                   skip_group_check=True)
                    return ins
                P.op("pe", mmqk, reads=[bqT, bkT, bmk, b_ident], writes=bpair)
                sv = pair[:, c_lo:768]
                nk = 768 - c_lo
                P.op("dve", lambda e: e.tensor_reduce(out=s8[:, 0, h:h + 1], in_=sv, axis=AX.X, op=ALU.max),
                     reads=bpair, writes=[hb[h]])
                P.op("dve", lambda e: e.tensor_scalar(out=s8[:, 2, h:h + 1], in0=s8[:, 0, h:h + 1], scalar1=sk8[:, h:h + 1],
                                                      scalar2=-0.125, op0=ALU.max, op1=ALU.mult),
                     reads=[hb[h], bsk8], writes=[hb[h]])
                Pt, bPt = Ps.next()
                P.op("act", lambda e: e.activation(
                    out=Pt[:, 0:nk], in_=sv, func=AF.Exp, bias=s8[:, 2, h:h + 1], scale=0.125, accum_out=s8[:, 3, h:h + 1]),
                    reads=bpair + [hb[h]], writes=[bPt, hb[h]])
                return (h, kvj, Pt, bPt, nk)

            def st_tr(stt):
                h, kvj, Pt, bPt, nk = stt
                nch = nk // 128
                ptb = 4 + (h % 2)
                ptv = ps_bf(ptb)[:, 0:640].rearrange("p (n q) -> p n q", n=5)

                def trp(e):
                    for n_ in range(nch):
                        ins = e.transpose(out=ptv[:, n_, :], in_=Pt[:, n_ * 128:(n_ + 1) * 128], identity=ident)
                    return ins
                P.op("pe", trp, reads=[bPt, b_ident], writes=[bps[ptb]])
                PT_, bPT = PTs.next()
                if h % 2 == 0:
                    P.op("act", lambda e: e.copy(out=PT_[:, 0:nch, :], in_=ptv[:, 0:nch, :]), reads=[bps[ptb]], writes=[bPT])
                else:
                    P.op("dve", lambda e: e.tensor_copy(out=PT_[:, 0:nch, :], in_=ptv[:, 0:nch, :]), reads=[bps[ptb]], writes=[bPT])
                return (h, kvj, PT_, bPT)

            def st_pv(stt):
                h, kvj, PT_, bPT = stt
                vt = wb + [32, 33]

                def mmpv(e):
                    for n_, tl in enumerate(vt):
                        ins = e.matmul(pO[:, h, :], lhsT=PT_[:, n_, :], rhs=Vt[:, tl, kvj * 64:(kvj + 1) * 64],
                                       start=(n_ == 0), stop=(n_ == len(vt) - 1))
                    return ins
                P.op("pe", mmpv, reads=[bPT, bVt], writes=[bps[6]])

            q_st = {0: st_qk(0)}
            t_st = {}
            for h in range(8):
                if h + 1 < 8:
                    q_st[h + 1] = st_qk(h + 1)
                t_st[h] = st_tr(q_st[h])
                if h >= 1:
                    st_pv(t_st[h - 1])
            st_pv(t_st[7])
            bs8 = st8b[slot8]
            P.op("dve", lambda e, s8=s8: e.scalar_tensor_tensor(out=s8[:, 4, :], in0=s8[:, 2, :], scalar=8.0, in1=sk8,
                                                                op0=ALU.mult, op1=ALU.add),
                 reads=[bs8, bsk8] + hb, writes=[bs8])
            P.op("act", lambda e, s8=s8: e.activation(out=s8[:, 4, :], in_=s8[:, 4, :], func=AF.Exp, scale=0.125),
                 reads=[bs8], writes=[bs8])
            P.op("dve", lambda e, s8=s8: e.tensor_tensor(out=s8[:, 5, :], in0=s8[:, 4, :], in1=s8[:, 3, :], op=ALU.add),
                 reads=[bs8] + hb, writes=[bs8])
            P.op("dve", lambda e, s8=s8: e.reciprocal(out=s8[:, 5, :], in_=s8[:, 5, :]), reads=[bs8], writes=[bs8])
            P.op("dve", lambda e, s8=s8, osb=osb: e.tensor_tensor(out=osb, in0=pO, in1=s8[:, 5, :].unsqueeze(2).to_broadcast([128, 8, 64]),
                                                                  op=ALU.mult), reads=[bps[6], bs8], writes=[bosb])
            otv = ps_bf(7)[:, 0:512].rearrange("p (n q) -> p n q", n=4)
            of = osb.rearrange("p h d -> p (h d)")

            def post():
                def tro(e):
                    for n_ in range(4):
                        ins = e.transpose(out=otv[:, n_, :], in_=of[:, n_ * 128:(n_ + 1) * 128], identity=ident)
                    return ins
                P.op("pe", tro, reads=[bosb, b_ident], writes=[bps[7]])
                P.op("act", lambda e: e.copy(out=yT[:, 4:8, cs], in_=otv), reads=[bps[7]], writes=[byT])
            return post

        prev_post = [None]
        prev_yT = None

        tail = [None]
        tail_f = {}

        def do_tail(g, yT, byT):
            convert_some(3)
            fT4, bfT4 = fT4s.next()
            cwt, bcw = cws.next()
            return mixer_tail(1, g, 4, yT, byT, Wout, bWout, gate1, GS2, hts, tmps, fbf, fT4, bfT4,
                              lambda i: (H1[i * 128:(i + 1) * 128, :], [bH1]), 0, 2, 3, cwt, bcw, rsc, brsc,
                              dbg.get("d_hm1"), deferred=True)

        for g in range(8):
            yT, byT = yTs.next()
            for j in range(4):
                i = g * 4 + j
                cs = slice(j * 128, (j + 1) * 128)
                srcs = []
                if i > 0:
                    srcs.append((i - 1, 0))
                srcs.append((i, 3 if i == 0 else (4 if i == 31 else 1)))
                if i < 31:
                    srcs.append((i + 1, 2))
                pv4 = psb[7].rearrange("p (g t) -> p g t", g=4)

                def mmpool(e, srcs=srcs):
                    for gi in range(4):
                        for n_, (s_, kind) in enumerate(srcs):
                            ins = e.matmul(pv4[:, gi, :], lhsT=Up[:, s_, gi * 128:(gi + 1) * 128], rhs=pB[:, gi * 5 + kind, :],
                                           start=(n_ == 0), stop=(n_ == len(srcs) - 1))
                    return ins
                P.op("pe", mmpool, reads=[bUp, bpB], writes=[bps[7]])
                pT_, bpT = pTs.next()
                P.op("act", lambda e, pT_=pT_: e.copy(out=pT_, in_=pv4), reads=[bps[7]], writes=[bpT])

                def mmpw(e, pT_=pT_):
                    for gi in range(4):
                        ins = e.matmul(pv4[:, gi, :], lhsT=PW[:, gi, :], rhs=pT_[:, gi, :], start=True, stop=True)
                    return ins
                P.op("pe", mmpw, reads=[bpT, bPW], writes=[bps[7]])
                P.op("dve", lambda e, yT=yT, cs=cs: e.tensor_tensor(out=yT[:, 0:4, cs], in0=pv4,
                                                                   in1=psc.unsqueeze(2).to_broadcast([128, 4, 128]), op=ALU.mult),
                     reads=[bps[7], bpsc], writes=[byT])
                if prev_post[0] is not None:
                    prev_post[0]()
                    prev_post[0] = None
                if g > 0:
                    if j == 0:
                        if tail[0] is not None:
                            tA, tB, tF = tail[0]
                            tB(3, *tail_f[3])
                            tF()
                        tail[0] = do_tail(g - 1, prev_yT[0], prev_yT[1])
                    tA, tB, tF = tail[0]
                    tail_f[j] = tA(j)
                    if j > 0:
                        tB(j - 1, *tail_f[j - 1])
                prev_post[0] = attn_block(i, cs, yT, byT)
            prev_yT = (yT, byT)
        prev_post[0]()
        if tail[0] is not None:
            tA, tB, tF = tail[0]
            tB(3, *tail_f[3])
            tF()
        tA, tB, tF = do_tail(7, prev_yT[0], prev_yT[1])
        pend = tA(0)
        for j in range(4):
            nxt = tA(j + 1) if j + 1 < 4 else None
            tB(j, *pend)
            pend = nxt
        tF()


    def moe_sparse(l, ntok, Hout, bHout, final):
        convert_upto(48 * (l + 1))
        new_phase()
        NTl = ntok // 128
        NTS = (2 * ntok) // 256 + NE - 1
        nsl = NTS * 256
        cwa = T([128, NTl, NE], F32)
        mka = T([128, NTl, NE], F32)
        mkb = T([128, NTl, NE], BF16)
        sp = T([128, NTl, NE], F32)
        v1 = T([128, NTl, NE], F32)
        v2 = T([128, NTl, NE], F32)
        tm = T([128, NTl, NE], F32)
        bR = Buf()
        Lt = T([128, 128], BF16)
        ones = T([128, 128], BF16)
        sm = T([128, 16, NE], F32)
        smi = T([128, NE], I32)
        slo = T([128, NTl], F32)
        shi = T([128, NTl], F32)
        clo = T([128, NTl], F32)
        chi = T([128, NTl], F32)
        sloi = sloi_t[:, 0:NTl]
        shii = shii_t[:, 0:NTl]
        tau = T([128, 64], F32)
        pix = T([128, 1], F32)
        cmpt = T([128, NTS, NE], F32)
        ecn = T([128, NTS], F32)
        widx = widx_t[:, 0:NTS]
        P.dma("sync", cwa, CW[0:ntok, :].rearrange("(n p) e -> p n e", p=128), reads=[bCW], writes=[bR])
        P.dma("sync", tau, tauc, writes=[bR])
        P.dma("sync", pix, pidx, writes=[bR])
        P.op("pool", lambda e: e.memset(ones, 1.0), writes=[bR])
        P.op("pool", lambda e: e.memset(Lt, 1.0), writes=[bR])
        P.op("pool", lambda e: e.affine_select(out=Lt, in_=Lt, pattern=[[1, 128]], compare_op=ALU.is_gt, fill=0.0,
                                               base=0, channel_multiplier=-1), reads=[bR], writes=[bR])
        P.op("dve", lambda e: e.tensor_scalar(out=mka, in0=cwa, scalar1=0.0, scalar2=None, op0=ALU.is_gt), reads=[bR], writes=[bR])
        P.op("dve", lambda e: e.tensor_copy(out=mkb, in_=mka), reads=[bR], writes=[bR])
        half = (NTl + 1) // 2

        def mmpos(e):
            for i in range(NTl):
                pbk, col = (0, i) if i < half else (1, i - half)
                o_ = psb[pbk][:, col * 16:(col + 1) * 16]
                for i2 in range(i):
                    e.matmul(o_, lhsT=ones, rhs=mkb[:, i2, :], start=(i2 == 0), stop=False)
                e.matmul(o_, lhsT=Lt, rhs=mkb[:, i, :], start=(i == 0), stop=True)
            for i in range(NTl):
                ins = e.matmul(psb[2][:, 0:16], lhsT=ones, rhs=mkb[:, i, :], start=(i == 0), stop=(i == NTl - 1))
            return ins
        P.op("pe", mmpos, reads=[bR], writes=[bps[0], bps[1], bps[2]])
        rd = [bR, bps[0], bps[1], bps[2]]
        P.op("dve", lambda e: e.tensor_scalar(out=sm[:, 0, :], in0=psb[2][:, 0:16], scalar1=255.0, scalar2=None, op0=ALU.add),
             reads=rd, writes=[bR])
        P.op("dve", lambda e: e.tensor_copy(out=smi, in_=sm[:, 0, :]), reads=[bR], writes=[bR])
        P.op("dve", lambda e: e.tensor_scalar(out=smi, in0=smi, scalar1=8, scalar2=8, op0=ALU.arith_shift_right,
                                              op1=ALU.logical_shift_left), reads=[bR], writes=[bR])
        P.op("dve", lambda e: e.tensor_copy(out=sm[:, 1, :], in_=smi), reads=[bR], writes=[bR])
        P.op("dve", lambda e: e.tensor_copy(out=sm[:, 2, :], in_=sm[:, 1, :]), reads=[bR], writes=[bR])
        cur, nxt = 2, 3
        for d_ in (1, 2, 4, 8):
            P.op("dve", lambda e, cur=cur, nxt=nxt, d_=d_: e.tensor_tensor(
                out=sm[:, nxt, d_:16], in0=sm[:, cur, d_:16], in1=sm[:, cur, 0:16 - d_], op=ALU.add), reads=[bR], writes=[bR])
            P.op("dve", lambda e, cur=cur, nxt=nxt, d_=d_: e.tensor_copy(out=sm[:, nxt, 0:d_], in_=sm[:, cur, 0:d_]),
                 reads=[bR], writes=[bR])
            cur, nxt = nxt, cur
        P.op("dve", lambda e, cur=cur: e.tensor_tensor(out=sm[:, 4, :], in0=sm[:, cur, :], in1=sm[:, 1, :], op=ALU.subtract),
             reads=[bR], writes=[bR])
        start = sm[:, 4, :]
        P.op("dve", lambda e: e.tensor_tensor(out=sp[:, 0:half, :], in0=psb[0][:, 0:half * 16].rearrange("p (n e) -> p n e", e=16),
                                              in1=start.unsqueeze(1).to_broadcast([128, half, NE]), op=ALU.add), reads=rd, writes=[bR])
        P.op("dve", lambda e: e.tensor_tensor(out=sp[:, half:NTl, :],
                                              in0=psb[1][:, 0:(NTl - half) * 16].rearrange("p (n e) -> p n e", e=16),
                                              in1=start.unsqueeze(1).to_broadcast([128, NTl - half, NE]), op=ALU.add), reads=rd, writes=[bR])
        P.op("dve", lambda e: e.tensor_scalar(out=tm, in0=mka, scalar1=-1.0e6, scalar2=1.0e6, op0=ALU.mult, op1=ALU.add),
             reads=[bR], writes=[bR])
        P.op("dve", lambda e: e.tensor_tensor(out=sp, in0=sp, in1=mka, op=ALU.mult), reads=[bR], writes=[bR])
        P.op("dve", lambda e: e.tensor_tensor(out=v1, in0=sp, in1=tm, op=ALU.add), reads=[bR], writes=[bR])
        P.op("dve", lambda e: e.tensor_tensor(out=v2, in0=sp, in1=tm, op=ALU.subtract), reads=[bR], writes=[bR])
        P.op("dve", lambda e: e.tensor_reduce(out=slo, in_=v1, axis=AX.X, op=ALU.min), reads=[bR], writes=[bR])
        P.op("dve", lambda e: e.tensor_reduce(out=shi, in_=v2, axis=AX.X, op=ALU.max), reads=[bR], writes=[bR])
        P.op("dve", lambda e: e.tensor_tensor(out=v1, in0=v1, in1=slo.unsqueeze(2).to_broadcast([128, NTl, NE]), op=ALU.is_equal),
             reads=[bR], writes=[bR])
        P.op("dve", lambda e: e.tensor_tensor(out=v1, in0=v1, in1=cwa, op=ALU.mult), reads=[bR], writes=[bR])
        P.op("dve", lambda e: e.tensor_reduce(out=clo, in_=v1, axis=AX.X, op=ALU.add), reads=[bR], writes=[bR])
        P.op("dve", lambda e: e.tensor_tensor(out=v2, in0=v2, in1=shi.unsqueeze(2).to_broadcast([128, NTl, NE]), op=ALU.is_equal),
             reads=[bR], writes=[bR])
        P.op("dve", lambda e: e.tensor_tensor(out=v2, in0=v2, in1=cwa, op=ALU.mult), reads=[bR], writes=[bR])
        P.op("dve", lambda e: e.tensor_reduce(out=chi, in_=v2, axis=AX.X, op=ALU.add), reads=[bR], writes=[bR])
        P.op("dve", lambda e: e.tensor_copy(out=sloi, in_=slo), reads=[bR], writes=[bR])
        P.op("dve", lambda e: e.tensor_copy(out=shii, in_=shi), reads=[bR], writes=[bR])
        P.op("dve", lambda e: e.tensor_tensor(out=cmpt, in0=start.unsqueeze(1).to_broadcast([128, NTS, NE]),
                                              in1=tau[:, 0:NTS].unsqueeze(2).to_broadcast([128, NTS, NE]), op=ALU.is_le),
             reads=[bR], writes=[bR])
        P.op("dve", lambda e: e.tensor_reduce(out=ecn, in_=cmpt, axis=AX.X, op=ALU.add), reads=[bR], writes=[bR])
        P.op("dve", lambda e: e.tensor_scalar(out=ecn, in0=ecn, scalar1=128.0, scalar2=-128.0, op0=ALU.mult, op1=ALU.add),
             reads=[bR], writes=[bR])
        P.op("dve", lambda e: e.tensor_scalar(out=ecn, in0=ecn, scalar1=pix[:, 0:1], scalar2=None, op0=ALU.add), reads=[bR], writes=[bR])
        P.op("dve", lambda e: e.tensor_copy(out=widx, in_=ecn), reads=[bR], writes=[bR])
        if l == 0:
            ada_layer(1)
        fts = Rot(3, [128, D], BF16)
        for i in range(NTl):
            ft, bft = fts.next()
            P.dma("sync", ft, Fd[i * 128:(i + 1) * 128, :], writes=[bft])
            for idx in (sloi, shii):
                P.op("pool", lambda e, ft=ft, idx=idx, i=i: e.indirect_dma_start(
                    out=Xs[:, :], out_offset=bass.IndirectOffsetOnAxis(ap=idx[:, i:i + 1], axis=0),
                    in_=ft, in_offset=None, bounds_check=breg(e, nsl - 1), oob_is_err=False),
                    reads=[bft, bR], writes=[Buf()], dma=True)
        P.barrier()
        mark_e = st["off"]
        xss = Rot(2, [128, 2, D], BF16)
        xTs = Rot(2, [128, 8, 256], BF16)
        nwb = 3
        wgus = Rot(nwb, [128, 8, 1024], BF16)
        wds = Rot(nwb, [128, 4, 1024], BF16)
        hids = Rot(2, [128, 4, 256], BF16)
        sgs_ = Rot(2, [128, 256], F32)
        yss = Rot(2, [128, 2, D], F32)
        for tq in range(NTS):
            xs_, bxs = xss.next()
            P.dma("sync", xs_, Xs[tq * 256:(tq + 1) * 256, :].rearrange("(j p) d -> p j d", p=128), writes=[bxs])
            wgu, bwgu = wgus.next()
            wd, bwd = wds.next()
            P.op("pool", lambda e, wgu=wgu, tq=tq: e.indirect_dma_start(
                out=wgu.rearrange("p k n -> p (k n)"), out_offset=None, in_=WGU[l][:, :],
                in_offset=bass.IndirectOffsetOnAxis(ap=widx[:, tq:tq + 1], axis=0), bounds_check=breg(e, NE * 128 - 1), oob_is_err=False),
                reads=[bR], writes=[bwgu], dma=True)
            P.op("pool", lambda e, wd=wd, tq=tq: e.indirect_dma_start(
                out=wd.rearrange("p k n -> p (k n)"), out_offset=None, in_=WD[l][:, :],
                in_offset=bass.IndirectOffsetOnAxis(ap=widx[:, tq:tq + 1], axis=0), bounds_check=breg(e, NE * 128 - 1), oob_is_err=False),
                reads=[bR], writes=[bwd], dma=True)
            xT, bxT = xTs.next()
            for j in range(2):
                transpose8(xs_[:, j, :], bxs, j, xT, bxT, j * 128)
            hd, bhd = hids.next()
            for jc in range(4):
                pg, pu = (2, 3) if jc % 2 == 0 else (4, 5)

                def mmg(e, jc=jc, pg=pg, pu=pu, wgu=wgu, xT=xT):
                    for (pb_, off) in ((pg, 0), (pu, 512)):
                        for k in range(8):
                            ins = e.matmul(psb[pb_][:, 0:256], lhsT=wgu[:, k, off + jc * 128: off + (jc + 1) * 128],
                                           rhs=xT[:, k, :], start=(k == 0), stop=(k == 7))
                    return ins
                P.op("pe", mmg, reads=[bwgu, bxT], writes=[bps[pg], bps[pu]])
                sg, bsg = sgs_.next()
                P.op("act", lambda e, sg=sg, pg=pg: e.activation(out=sg, in_=psb[pg][:, 0:256], func=AF.Silu),
                     reads=[bps[pg]], writes=[bsg])
                P.op("dve", lambda e, sg=sg, pu=pu, hd=hd, jc=jc: e.tensor_tensor(out=hd[:, jc, :], in0=psb[pu][:, 0:256], in1=sg, op=ALU.mult),
                     reads=[bps[pu], bsg], writes=[bhd])
            ys_, bys = yss.next()
            for j in range(2):
                for hf in range(2):
                    pd = 6 + ((j * 2 + hf) % 2)

                    def mmd(e, pd=pd, hd=hd, j=j, hf=hf, wd=wd):
                        for jc in range(4):
                            ins = e.matmul(psb[pd], lhsT=hd[:, jc, j * 128:(j + 1) * 128],
                                           rhs=wd[:, jc, hf * 512:(hf + 1) * 512], start=(jc == 0), stop=(jc == 3))
                        return ins
                    P.op("pe", mmd, reads=[bhd, bwd], writes=[bps[pd]])
                    if hf == 0:
                        P.op("act", lambda e, ys_=ys_, j=j, hf=hf, pd=pd: e.copy(out=ys_[:, j, hf * 512:(hf + 1) * 512], in_=psb[pd]),
                             reads=[bps[pd]], writes=[bys])
                    else:
                        P.op("dve", lambda e, ys_=ys_, j=j, hf=hf, pd=pd: e.tensor_copy(out=ys_[:, j, hf * 512:(hf + 1) * 512], in_=psb[pd]),
                             reads=[bps[pd]], writes=[bys])
            P.dma("act", Ys[tq * 256:(tq + 1) * 256, :].rearrange("(j p) d -> p j d", p=128), ys_, reads=[bys], writes=[Buf()])
        P.barrier()
        st["off"] = mark_e
        g2 = {}
        for row in ((0, 1) if ntok > SEQ else (0,)):
            gt = T([128, D], F32)
            bg = Buf()
            load_mod(l, row, 5, gt, bg)
            g2[row] = (gt, bg)
        if final:
            fg = T([128, D], F32)
            bfg = Buf()
            P.dma("sync", fg, final_g.partition_broadcast(128), writes=[bfg])
        ylos = Rot(3, [128, D], F32)
        yhis = Rot(3, [128, D], F32)
        hts = Rot(3, [128, D], F32)
        tmps = Rot(2, [128, D], F32)
        outs = []

        def cb_load(i):
            ylo, bylo = ylos.next()
            yhi, byhi = yhis.next()
            for (yt_, byt, idx) in ((ylo, bylo, sloi), (yhi, byhi, shii)):
                P.op("pool", lambda e, yt_=yt_, idx=idx: e.indirect_dma_start(
                    out=yt_, out_offset=None, in_=Ys[:, :], in_offset=bass.IndirectOffsetOnAxis(ap=idx[:, i:i + 1], axis=0),
                    bounds_check=breg(e, nsl - 1), oob_is_err=False), reads=[bR], writes=[byt], dma=True)
            ht, bh = hts.next()
            P.dma("sync", ht, Hm[i * 128:(i + 1) * 128, :], reads=[bHm], writes=[bh])
            return ylo, bylo, yhi, byhi, ht, bh

        def cb_comp(i, ylo, bylo, yhi, byhi, ht, bh):
            row = 0 if i < 32 else 1
            gt, bg = g2[row]
            P.op("dve", lambda e: e.tensor_scalar(out=ylo, in0=ylo, scalar1=clo[:, i:i + 1], scalar2=None, op0=ALU.mult),
                 reads=[bylo, bR], writes=[bylo])
            P.op("dve", lambda e: e.scalar_tensor_tensor(out=ylo, in0=yhi, scalar=chi[:, i:i + 1], in1=ylo,
                                                         op0=ALU.mult, op1=ALU.add), reads=[bylo, byhi, bR], writes=[bylo])
            P.op("dve", lambda e: e.tensor_tensor(out=ylo, in0=ylo, in1=gt, op=ALU.mult), reads=[bylo, bg], writes=[bylo])
            P.op("dve", lambda e: e.tensor_tensor(out=ht, in0=ylo, in1=ht, op=ALU.add),
                 reads=[bylo, bh], writes=[bh])
            if not final:
                P.dma("act", Hout[i * 128:(i + 1) * 128, :], ht, reads=[bh], writes=[bHout])
                if debug:
                    dbg_ops.append(P.dma("act", dbg["d_h1"][i * 128:(i + 1) * 128, :], ht, reads=[bh]))
            else:
                tmp, btmp = tmps.next()
                rs, bs = norm_mod(ht, bh, None, None, None, None, tmp, btmp, None, None)
                P.op("dve", lambda e: e.scalar_tensor_tensor(
                    out=tmp, in0=ht, scalar=rs, in1=fg, op0=ALU.mult, op1=ALU.mult), reads=[bh, bs, bfg], writes=[btmp])
                outs.append(P.dma("act", out[i * 128:(i + 1) * 128, :], tmp, reads=[btmp]))

        pend = [cb_load(0), cb_load(1)]
        for i in range(NTl):
            if i + 2 < NTl:
                pend.append(cb_load(i + 2))
            cb_comp(i, *pend.pop(0))
        return outs

    Vcs, bV, keep = layer0_A()
    layer0_B(Vcs, bV, keep)
    if debug:
        new_phase()
        cwd = T([128, NT, NE], F32)
        bb = Buf()
        P.dma("sync", cwd, CW.rearrange("(n p) e -> p n e", p=128), reads=[bCW], writes=[bb])
        dbg_ops.append(P.dma("sync", dbg["d_cw0"].rearrange("(n p) e -> p n e", p=128), cwd, reads=[bb]))
    finals = []
    if stop_after != "mix0":
        moe_sparse(0, NTOK, H1, bH1, False)
    if stop_after not in ("mix0", "moe0"):
        pk, keep1 = layer1_A()
        layer1_B(pk, keep1)
        if debug:
            new_phase()
            cwd = T([128, 32, NE], F32)
            bb = Buf()
            P.dma("sync", cwd, CW[0:SEQ, :].rearrange("(n p) e -> p n e", p=128), reads=[bCW], writes=[bb])
            dbg_ops.append(P.dma("sync", dbg["d_cw1"][0:SEQ, :].rearrange("(n p) e -> p n e", p=128), cwd, reads=[bb]))
        if stop_after != "mix1":
            finals = moe_sparse(1, SEQ, None, None, True)
    P.finalize(finals + dbg_ops)
    return nc


_NC = {}


def kernel(**inputs):
    inp = {k: np.asarray(v) for k, v in inputs.items()}
    c = _consts()
    if "nc" not in _NC:
        _NC["nc"] = build(debug=False)
    nc = _NC["nc"]
    shared = {}
    for k in ("ada_w", "ada_b", "norm_mix_g", "norm_ffn_g", "router_w", "router_b",
              "moe_w_gate", "moe_w_up", "moe_w_down", "final_g"):
        shared[k] = np.ascontiguousarray(inp[k], dtype=np.float32)
    for k in ("even_w_in", "even_conv_w", "even_w_out", "odd_w_in", "odd_pool_w", "odd_pool_scale",
              "odd_sink", "odd_w_out"):
        shared[k] = np.ascontiguousarray(inp[k][0], dtype=np.float32)
    for k in ("dftN", "dftC", "dftD", "rope", "poolB", "amask", "tauc", "pidx"):
        shared[k] = c[k]
    nb = inp["x"].shape[0]
    in_maps = []
    for b in range(nb):
        m = dict(shared)
        m["x"] = np.ascontiguousarray(inp["x"][b], dtype=np.float32)
        m["ctx"] = np.ascontiguousarray(inp["ctx"][b], dtype=np.float32)
        m["c2"] = np.ascontiguousarray(np.stack([inp["c"][b], inp["c_ctx"]], 0), dtype=np.float32)
        in_maps.append(m)
    res = run_bass_kernel_spmd(nc, in_maps, core_ids=list(range(nb)))
    return np.stack([np.asarray(r["out"], dtype=np.float32) for r in res.results], 0)
```
